# Optimizing a Trainium2 kernel written in Bass

```python
import jax
import jax.numpy as jnp
from jax import lax
import numpy as np

D_MODEL = 1024
BATCH = 8
SEQ = 4096
DEPTH = 1

HEAD_DIM = 64
N_FOX_HEADS = 8
N_SB_HEADS = 8
FOX_WIDTH = N_FOX_HEADS * HEAD_DIM
SB_WIDTH = N_SB_HEADS * HEAD_DIM
N_GROUPS = 4
EXPERTS_PER_GROUP = 4
N_EXPERTS = N_GROUPS * EXPERTS_PER_GROUP
TOP_K_EXPERTS = 2
D_FF_EXPERT = D_MODEL // 2
Q_BLOCK = 128
RMS_EPS = 1e-6
N_MOD = 6
IN_SPLITS = (
    FOX_WIDTH, 2 * FOX_WIDTH, 3 * FOX_WIDTH,
    3 * FOX_WIDTH + N_FOX_HEADS,
    3 * FOX_WIDTH + N_FOX_HEADS + SB_WIDTH,
    3 * FOX_WIDTH + N_FOX_HEADS + 2 * SB_WIDTH,
    3 * FOX_WIDTH + N_FOX_HEADS + 3 * SB_WIDTH,
    3 * FOX_WIDTH + N_FOX_HEADS + 3 * SB_WIDTH + D_MODEL,
)
N_IN = 3 * FOX_WIDTH + N_FOX_HEADS + 3 * SB_WIDTH + 2 * D_MODEL

kernel_name = "hybrid_fox_stickbreak_hmoe"


def rmsnorm(x, g):
    xf = x.astype(jnp.float32)
    xf = xf * lax.rsqrt(jnp.mean(xf * xf, axis=-1, keepdims=True) + RMS_EPS)
    return xf.astype(x.dtype) * g


def to_heads(t, n_heads):
    b, s, _ = t.shape
    return t.reshape(b, s, n_heads, HEAD_DIM).transpose(0, 2, 1, 3)


def from_heads(t):
    b, h, s, d = t.shape
    return t.transpose(0, 2, 1, 3).reshape(b, s, h * d)


def sweep_query_blocks(block_fn, seq):
    n_blocks = seq // Q_BLOCK
    out = lax.map(block_fn, jnp.arange(n_blocks))
    nb, b, h, qb, d = out.shape
    return out.transpose(1, 2, 0, 3, 4).reshape(b, h, nb * qb, d)


def forgetting_attention(q, k, v, log_f):
    seq = q.shape[2]
    scale = HEAD_DIM ** -0.5
    F = jnp.cumsum(log_f, axis=-1)
    kpos = jnp.arange(seq)

    def block(i):
        start = i * Q_BLOCK
        qs = lax.dynamic_slice_in_dim(q, start, Q_BLOCK, axis=2)
        Fq = lax.dynamic_slice_in_dim(F, start, Q_BLOCK, axis=2)
        logits = jnp.einsum("bhqd,bhkd->bhqk", qs, k).astype(jnp.float32) * scale
        logits = logits + Fq[..., :, None] - F[..., None, :]
        qpos = start + jnp.arange(Q_BLOCK)
        mask = kpos[None, :] <= qpos[:, None]
        logits = jnp.where(mask, logits, -jnp.inf)
        p = jax.nn.softmax(logits, axis=-1)
        return jnp.einsum("bhqk,bhkd->bhqd", p.astype(v.dtype), v)

    return sweep_query_blocks(block, seq)


def stick_breaking_attention(q, k, v):
    seq = q.shape[2]
    scale = HEAD_DIM ** -0.5
    kpos = jnp.arange(seq)

    def block(i):
        start = i * Q_BLOCK
        qs = lax.dynamic_slice_in_dim(q, start, Q_BLOCK, axis=2)
        z = jnp.einsum("bhqd,bhkd->bhqk", qs, k).astype(jnp.float32) * scale
        qpos = start + jnp.arange(Q_BLOCK)
        mask = kpos[None, :] < qpos[:, None]
        log_beta = jax.nn.log_sigmoid(z)
        log_one_minus = jnp.where(mask, jax.nn.log_sigmoid(-z), 0.0)
        suffix = lax.cumsum(log_one_minus, axis=3, reverse=True) - log_one_minus
        A = jnp.where(mask, jnp.exp(log_beta + suffix), 0.0)
        return jnp.einsum("bhqk,bhkd->bhqd", A.astype(v.dtype), v)

    return sweep_query_blocks(block, seq)


def hierarchical_moe(h, rg_w, rg_b, re_w, re_b, w_gate, w_up, w_down):
    b, s, d = h.shape
    hf = h.reshape(b * s, d)
    g_prob = jax.nn.softmax((hf @ rg_w + rg_b).astype(jnp.float32), axis=-1)
    g_top_p, g_idx = lax.top_k(g_prob, 1)
    e_logits = (hf @ re_w + re_b).astype(jnp.float32).reshape(b * s, N_GROUPS, EXPERTS_PER_GROUP)
    e_sel = jnp.take_along_axis(e_logits, g_idx[:, :, None], axis=1)[:, 0, :]
    e_prob = jax.nn.softmax(e_sel, axis=-1)
    e_top_p, e_idx = lax.top_k(e_prob, TOP_K_EXPERTS)
    e_top_p = e_top_p / jnp.sum(e_top_p, axis=-1, keepdims=True)
    within = jnp.sum(jax.nn.one_hot(e_idx, EXPERTS_PER_GROUP, dtype=jnp.float32)
                     * e_top_p[..., None], axis=1)
    combine = (jax.nn.one_hot(g_idx[:, 0], N_GROUPS, dtype=jnp.float32)[:, :, None]
               * within[:, None, :] * g_top_p[:, :, None])
    combine = combine.reshape(b * s, N_EXPERTS).astype(h.dtype)
    out = jnp.zeros_like(hf)
    for e in range(N_EXPERTS):
        act = jax.nn.silu(hf @ w_gate[e]) * (hf @ w_up[e])
        out = out + combine[:, e:e + 1] * (act @ w_down[e])
    return out.reshape(b, s, d)


def setup_inputs(seed: int = 0) -> dict:
    key = jax.random.key(seed)
    ks = jax.random.split(key, 20)
    D, L = D_MODEL, DEPTH

    def nrm(k, shape, scale):
        return jax.random.normal(k, shape, jnp.float32) * scale

    return {
        "x": nrm(ks[0], (BATCH, SEQ, D), 1.0),
        "c": nrm(ks[1], (BATCH, D), 1.0),
        "ada_w": nrm(ks[2], (L, D, N_MOD * D), 0.5 * D ** -0.5),
        "ada_b": nrm(ks[3], (L, N_MOD * D), 0.02),
        "norm1_g": 1.0 + nrm(ks[4], (L, D), 0.02),
        "w_in": nrm(ks[5], (L, D, N_IN), D ** -0.5),
        "b_forget": 2.0 + nrm(ks[6], (L, N_FOX_HEADS), 0.1),
        "w_branch_fox": nrm(ks[7], (L, FOX_WIDTH, D), FOX_WIDTH ** -0.5),
        "w_branch_sb": nrm(ks[8], (L, SB_WIDTH, D), SB_WIDTH ** -0.5),
        "w_out": nrm(ks[9], (L, D, D), D ** -0.5),
        "norm2_g": 1.0 + nrm(ks[10], (L, D), 0.02),
        "router_group_w": nrm(ks[11], (L, D, N_GROUPS), D ** -0.5),
        "router_group_b": nrm(ks[12], (L, N_GROUPS), 0.01),
        "router_expert_w": nrm(ks[13], (L, D, N_EXPERTS), D ** -0.5),
        "router_expert_b": nrm(ks[14], (L, N_EXPERTS), 0.01),
        "expert_w_gate": nrm(ks[15], (L, N_EXPERTS, D, D_FF_EXPERT), D ** -0.5),
        "expert_w_up": nrm(ks[16], (L, N_EXPERTS, D, D_FF_EXPERT), D ** -0.5),
        "expert_w_down": nrm(ks[17], (L, N_EXPERTS, D_FF_EXPERT, D), D_FF_EXPERT ** -0.5),
        "final_g": 1.0 + nrm(ks[18], (D,), 0.02),
    }


def reference(x, c, ada_w, ada_b, norm1_g, w_in, b_forget, w_branch_fox, w_branch_sb, w_out,
              norm2_g, router_group_w, router_group_b, router_expert_w, router_expert_b,
              expert_w_gate, expert_w_up, expert_w_down, final_g):
    c_act = jax.nn.silu(c)
    for l in range(DEPTH):
        mod = c_act @ ada_w[l] + ada_b[l]
        shift1, scale1, gate1, shift2, scale2, gate2 = [m[:, None, :] for m in jnp.split(mod, N_MOD, axis=-1)]

        h = rmsnorm(x, norm1_g[l]) * (1.0 + scale1) + shift1
        proj = h @ w_in[l]
        qa, ka, va, fa, qb, kb, vb, ga, gb = jnp.split(proj, IN_SPLITS, axis=-1)
        log_f = jax.nn.log_sigmoid((fa + b_forget[l]).astype(jnp.float32)).transpose(0, 2, 1)
        o_fox = forgetting_attention(to_heads(qa, N_FOX_HEADS), to_heads(ka, N_FOX_HEADS),
                                     to_heads(va, N_FOX_HEADS), log_f)
        o_sb = stick_breaking_attention(to_heads(qb, N_SB_HEADS), to_heads(kb, N_SB_HEADS),
                                        to_heads(vb, N_SB_HEADS))
        merged = (jax.nn.sigmoid(ga) * (from_heads(o_fox) @ w_branch_fox[l])
                  + jax.nn.sigmoid(gb) * (from_heads(o_sb) @ w_branch_sb[l]))
        x = x + gate1 * (merged @ w_out[l])

        h2 = rmsnorm(x, norm2_g[l]) * (1.0 + scale2) + shift2
        y = hierarchical_moe(h2, router_group_w[l], router_group_b[l], router_expert_w[l],
                             router_expert_b[l], expert_w_gate[l], expert_w_up[l], expert_w_down[l])
        x = x + gate2 * y
    return rmsnorm(x, final_g)
```

```python
import contextlib
import numpy as np
import concourse.bass as bass
import concourse.mybir as mybir
from concourse.bass_utils import run_bass_kernel_spmd

F32 = mybir.dt.float32
BF16 = mybir.dt.bfloat16
U8 = mybir.dt.uint8
AF = mybir.ActivationFunctionType
ALU = mybir.AluOpType

S = 4096
D = 1024
NT = S // 128
NC_ = S // 512
NIN = 5128
NEG = -30000.0
EPS = 1e-6


class Prog:
    ENGS = ("pe", "act", "dve", "pool", "sp")

    def __init__(self, nc, n_dma_sems=12):
        self.nc = nc
        self.ops = []
        self.n_dma_sems = n_dma_sems

    def add(self, eng, fn, reads=(), writes=(), dma=False, barrier=False):
        self.ops.append(dict(eng=eng, fn=fn, reads=tuple(reads), writes=tuple(writes), dma=dma, barrier=barrier))

    def pe(self, fn, reads=(), writes=()): self.add("pe", fn, reads, writes)
    def act(self, fn, reads=(), writes=()): self.add("act", fn, reads, writes)
    def dve(self, fn, reads=(), writes=()): self.add("dve", fn, reads, writes)
    def pool(self, fn, reads=(), writes=()): self.add("pool", fn, reads, writes)
    def dma(self, eng, fn, reads=(), writes=()): self.add(eng, fn, reads, writes, dma=True)
    def barrier(self): self.add(None, None, barrier=True)

    def build(self):
        ops = self.ops
        n = len(ops)
        last_w, readers = {}, {}
        deps = [set() for _ in range(n)]
        last_on_eng = {}
        dma_since = []
        bar_deps = None
        for i, o in enumerate(ops):
            if o["barrier"]:
                bar_deps = set(last_on_eng.values()) | set(dma_since)
                last_w, readers = {}, {}
                continue
            if bar_deps is not None:
                pass
            for k in o["reads"]:
                if k in last_w:
                    deps[i].add((last_w[k], False))
                if isinstance(k, tuple) and k[0] == "bank":
                    for r in readers.get(k, ()):
                        if ops[r]["eng"] != o["eng"]:
                            deps[i].add((r, False))
            for k in o["writes"]:
                if k in last_w:
                    deps[i].add((last_w[k], False))
                for r in readers.get(k, ()):
                    if r != i:
                        deps[i].add((r, True))
            for k in o["reads"]:
                readers.setdefault(k, []).append(i)
            for k in o["writes"]:
                last_w[k] = i
                readers[k] = []
            o["bar"] = bar_deps
            if o["dma"]:
                dma_since.append(i)
            else:
                last_on_eng[o["eng"]] = i
        fdeps = [set() for _ in range(n)]
        first_after_bar = {}
        for i, o in enumerate(ops):
            if o["barrier"]:
                continue
            for d, war in deps[i]:
                p = ops[d]
                if (not p["dma"]) and (not o["dma"]) and p["eng"] == o["eng"]:
                    if o["eng"] == "pe":
                        continue
                fdeps[i].add(d)
            bd = o.get("bar")
            if bd is not None and first_after_bar.get(o["eng"]) is not bd:
                first_after_bar[o["eng"]] = bd
                for d in bd:
                    if d != i and not (ops[d]["eng"] == o["eng"] and not ops[d]["dma"] and not o["dma"]):
                        fdeps[i].add(d)
        needs_inc = [False] * n
        for i in range(n):
            for d in fdeps[i]:
                needs_inc[d] = True
        eng_count = {e: 0 for e in self.ENGS}
        ticket = [None] * n
        dma_rr = {e: 0 for e in self.ENGS}
        dma_tot = {}
        pre_wait = [None] * n
        for i, o in enumerate(ops):
            if o["barrier"]:
                continue
            if o["dma"]:
                e = o["eng"]
                s = (e, dma_rr[e] % self.n_dma_sems)
                dma_rr[e] += 1
                prev = dma_tot.get(s, 0)
                if prev > 0:
                    pre_wait[i] = (s, prev)
                dma_tot[s] = prev + 16
                ticket[i] = (s, prev + 16)
            elif needs_inc[i]:
                eng_count[o["eng"]] += 1
                ticket[i] = (o["eng"], eng_count[o["eng"]])
        self.final_dma = dict(dma_tot)
        progs = {e: [] for e in self.ENGS}
        waited = {e: {} for e in self.ENGS}
        for i, o in enumerate(ops):
            if o["barrier"]:
                continue
            e = o["eng"]
            cand = [ticket[d] for d in fdeps[i]]
            if pre_wait[i] is not None:
                cand.append(pre_wait[i])
            best = {}
            for (s, v) in cand:
                if v > best.get(s, 0):
                    best[s] = v
            ws = []
            for s, v in best.items():
                if waited[e].get(s, 0) >= v:
                    continue
                waited[e][s] = v
                ws.append((s, v))
            progs[e].append((ws, o["fn"], ticket[i], o["dma"]))
        self.stats = {e: len(progs[e]) for e in self.ENGS}
        return progs

    def emit(self):
        nc = self.nc
        progs = self.build()
        semkeys = set()
        for e in self.ENGS:
            for ws, fn, t, isdma in progs[e]:
                for s, v in ws:
                    semkeys.add(s)
                if t is not None:
                    semkeys.add(t[0])
        semkeys = sorted(semkeys, key=str)
        with contextlib.ExitStack() as st:
            sems = {}
            for k in semkeys:
                nm = "s_" + (k if isinstance(k, str) else f"{k[0]}{k[1]}")
                sems[k] = st.enter_context(nc.semaphore(nm))
            block = st.enter_context(nc.Block())
            final = self.final_dma

            def make(e):
                def body(eng):
                    for ws, fn, t, isdma in progs[e]:
                        for s, v in ws:
                            eng.wait_ge(sems[s], v)
                        ins = fn(eng)
                        if t is not None:
                            ins.then_inc(sems[t[0]], 16 if isdma else 1)
                    if e == "sp":
                        for s, v in final.items():
                            eng.wait_ge(sems[s], v)
                return body
            block.tensor(make("pe"))
            block.scalar(make("act"))
            block.vector(make("dve"))
            block.gpsimd(make("pool"))
            block.sync(make("sp"))


class Arena:
    def __init__(self, nc, nbytes):
        self.t = nc.alloc_sbuf_tensor("arena", [128, nbytes], U8)
        self.nbytes = nbytes
        self.off = 0
        self.top = nbytes
        self.marks = {}

    def mark(self, name):
        self.marks[name] = self.off

    def reset(self, name):
        self.off = self.marks[name]

    def alloc_top(self, shape, dt):
        esz = 2 if dt == BF16 else 4
        n = int(np.prod(shape[1:]))
        nb = (n * esz + 31) // 32 * 32
        self.top -= nb
        v = self.t[0:shape[0], self.top:self.top + n * esz].bitcast(dt)
        if len(shape) == 3:
            v = v.rearrange("p (a b) -> p a b", b=shape[2])
        return v

    def alloc(self, shape, dt):
        esz = 2 if dt == BF16 else 4
        n = int(np.prod(shape[1:]))
        nb = (n * esz + 31) // 32 * 32
        assert self.off + nb <= self.top, (self.off, nb, self.top)
        v = self.t[0:shape[0], self.off:self.off + n * esz].bitcast(dt)
        self.off += nb
        if len(shape) == 3:
            v = v.rearrange("p (a b) -> p a b", b=shape[2])
        elif len(shape) == 4:
            v = v.rearrange("p (a b c) -> p a b c", b=shape[2], c=shape[3])
        return v


def MM(out, lhsT, rhs, start, stop):
    return lambda e: e.matmul(out, lhsT, rhs, start=start, stop=stop)


def TR(out, in_, ident):
    return lambda e: e.transpose(out, in_, ident)


def ACT(out, in_, func, **kw):
    return lambda e: e.activation(out=out, in_=in_, func=func, **kw)


def TT(out, in0, in1, op):
    return lambda e: e.tensor_tensor(out=out, in0=in0, in1=in1, op=op)


def TS(out, in0, s1, s2, op0, op1=None):
    if op1 is None:
        return lambda e: e.tensor_scalar(out=out, in0=in0, scalar1=s1, scalar2=None, op0=op0)
    return lambda e: e.tensor_scalar(out=out, in0=in0, scalar1=s1, scalar2=s2, op0=op0, op1=op1)


def STT(out, in0, scalar, in1, op0, op1):
    return lambda e: e.scalar_tensor_tensor(out=out, in0=in0, scalar=scalar, in1=in1, op0=op0, op1=op1)


def CP(out, in_):
    return lambda e: e.tensor_copy(out=out, in_=in_)


def DMA(out, in_):
    return lambda e: e.dma_start(out=out, in_=in_)


def MEMSET(ap, v):
    return lambda e: e.memset(ap, v)


def RECIP(out, in_):
    return lambda e: e.reciprocal(out=out, in_=in_)


def build_program(stop_after=99, dbg=False):
    nc = bass.Bass("TRN2", target_bir_lowering=False)

    def din(name, shape, dt=F32):
        return nc.dram_tensor(name, shape, dt, kind="ExternalInput").ap()

    x_d = din("x", [S, D])
    cT_d = din("cT", [128, 8])
    adaw_d = din("ada_w", [D, 6 * D])
    adabT_d = din("ada_bT", [128, 48])
    adab_row_d = din("ada_b_row", [1, 6 * D])
    n1g_d = din("n1g", [128, 8])
    n2g_d = din("n2g", [128, 8])
    fg_row_d = din("fg_row", [1, D])
    win_d = din("w_in", [D, NIN])
    bfg_d = din("b_forget", [8, 1])
    wbf_d = din("w_bf", [512, D])
    wbs_d = din("w_bs", [512, D])
    wout_d = din("w_out", [D, D])
    wr_d = din("w_r", [D, 20])
    br_row_d = din("b_r_row", [1, 20])
    wg2_d = din("e_wg", [16 * 256, 2048])
    wu2_d = din("e_wu", [16 * 256, 2048])
    wd2_d = din("e_wd", [16 * 512, D])
    n2g_row_d = din("n2g_row", [1, D])
    rc_d = din("rconst", [128, 40])
    out_d = nc.dram_tensor("out", [S, D], F32, kind="ExternalOutput").ap()
    ot_scr = nc.dram_tensor("ot_scr", [8, 128, S], BF16, kind="ExternalOutput" if dbg else "Internal").ap()
    fsc = nc.dram_tensor("fsc", [8, 12, S], BF16, kind="Internal").ap()
    gsc = nc.dram_tensor("gsc", [4, 128, D], F32, kind="Internal").ap()
    NS = 12
    h2tok_d = nc.dram_tensor("h2tok", [S, D], BF16, kind="Internal").ap()
    h2s_d = nc.dram_tensor("h2s", [NS * 512, D], BF16, kind="Internal").ap()
    cws_d = nc.dram_tensor("cws", [NS * 512, 4], F32, kind="Internal").ap()
    ys_d = nc.dram_tensor("ys", [NS * 512, D], F32, kind="Internal").ap()
    wg2b_d = nc.dram_tensor("wg2b", [16 * 256, 2048], BF16, kind="Internal").ap()
    wu2b_d = nc.dram_tensor("wu2b", [16 * 256, 2048], BF16, kind="Internal").ap()
    wd2b_d = nc.dram_tensor("wd2b", [16 * 512, D], BF16, kind="Internal").ap()
    dbg_o = {}
    if dbg:
        dbg_o["hT"] = nc.dram_tensor("dbg_hT", [128, 8, S], BF16, kind="ExternalOutput").ap()
        dbg_o["mod"] = nc.dram_tensor("dbg_mod", [128, 48], F32, kind="ExternalOutput").ap()
        dbg_o["g1"] = nc.dram_tensor("dbg_g1", [128, D], F32, kind="ExternalOutput").ap()
        dbg_o["comb"] = nc.dram_tensor("dbg_comb", [128, NT, 4], F32, kind="ExternalOutput").ap()
        dbg_o["h2T"] = nc.dram_tensor("dbg_h2T", [128, 8, S], BF16, kind="ExternalOutput").ap()

    win_v = win_d.rearrange("(k p) n -> p k n", p=128)
    adaw_v = adaw_d.rearrange("(k p) n -> p k n", p=128)

    P = Prog(nc)
    A = Arena(nc, 206 * 1024)
    banks = [nc.alloc_psum_tensor(f"bank{i}", [128, 512], F32) for i in range(8)]

    def bank_bf(i):
        return banks[i][:, :].bitcast(BF16)

    ohg_all = A.alloc([128, NT, 4], F32)
    cw_all = A.alloc([128, NT, 4], F32)
    s1 = A.alloc([128, 8], F32)
    b1 = A.alloc([128, 8], F32)
    s2 = A.alloc([128, 8], F32)
    b2 = A.alloc([128, 8], F32)
    n1g = A.alloc([128, 8], F32)
    n2g = A.alloc([128, 8], F32)
    modT = A.alloc([128, 48], F32)
    small = A.alloc([128, 64], F32)
    identb = A.alloc([128, 128], BF16)
    identf = A.alloc([128, 128], F32)
    onesf = A.alloc([128, 128], F32)
    A.mark("pre_hT")
    hT = A.alloc([128, 8, S], BF16)
    A.mark("phase")
    wq = A.alloc_top([128, 8, 576], BF16)
    wk = A.alloc_top([128, 8, 576], BF16)
    wv = A.alloc_top([128, 8, 512], BF16)
    wfa = A.alloc_top([128, 8, 8], BF16)

    P.pool(MEMSET(onesf, 1.0), writes=["onesf"])
    P.pool(lambda e: e.affine_select(out=identf, in_=onesf, pattern=[[-1, 128]], compare_op=ALU.is_equal,
                                     fill=0.0, base=0, channel_multiplier=1), reads=["onesf"], writes=["identf"])
    P.pool(CP(identb, identf), reads=["identf"], writes=["identb"])
    P.pool(MEMSET(modT, 0.0), writes=["modT_a", "modT_b"])
    P.dma("sp", DMA(n1g, n1g_d), writes=["n1g"])
    P.dma("sp", DMA(n2g, n2g_d), writes=["n2g"])

    gate1_b = A.alloc([128, D], F32)
    gate2_b = A.alloc([128, D], F32)
    cT = A.alloc([128, 8], F32)
    c_bf = A.alloc([128, 8], BF16)
    c_rep = A.alloc([128, 8, 128], BF16)
    adabT = A.alloc([128, 48], F32)
    adab_g = A.alloc([128, 4, D], F32)
    n2g_b = A.alloc([128, D], F32)
    sb2_b = [A.alloc([128, D], F32) for _ in range(2)]
    adaw_sb = [A.alloc([128, 8, 512], BF16) for _ in range(3)]
    P.dma("sp", DMA(cT, cT_d), writes=["cT"])
    P.dma("sp", DMA(adabT, adabT_d), writes=["adabT"])
    P.dma("sp", DMA(adab_g[:, 0, :], adab_row_d[0:1, 2 * D:3 * D].partition_broadcast(128)), writes=["adab_g0"])
    P.dma("sp", DMA(adab_g[:, 1, :], adab_row_d[0:1, 5 * D:6 * D].partition_broadcast(128)), writes=["adab_g1"])
    P.dma("sp", DMA(adab_g[:, 2, :], adab_row_d[0:1, 3 * D:4 * D].partition_broadcast(128)), writes=["adab_g2"])
    P.dma("sp", DMA(adab_g[:, 3, :], adab_row_d[0:1, 4 * D:5 * D].partition_broadcast(128)), writes=["adab_g3"])
    P.dma("sp", DMA(n2g_b, n2g_row_d[0:1, :].partition_broadcast(128)), writes=["n2g_b"])
    P.act(ACT(c_bf, cT, AF.Silu), reads=["cT"], writes=["c_bf"])
    P.dve(CP(c_rep, c_bf[:, :].unsqueeze(2).to_broadcast([128, 8, 128])), reads=["c_bf"], writes=["c_rep"])
    order = [0, 1, 2, 3, 6, 7, 8, 9, 4, 5, 10, 11]
    for n_, q in enumerate(order):
        buf = adaw_sb[n_ % 3]
        bk = ("adaw", n_ % 3)
        P.dma("pool", DMA(buf, adaw_v[:, :, q * 512:(q + 1) * 512]), writes=[bk])
        v, half = q // 2, q % 2
        if v in (2, 5):
            gi = 0 if v == 2 else 1
            bnk = banks[gi * 2 + half]
            for k in range(8):
                P.pe(MM(bnk[:, :], c_rep[:, k, :], buf[:, k, :], k == 0, k == 7), reads=[bk, "c_rep"], writes=[("bank", gi * 2 + half)])
            dst = (gate1_b if gi == 0 else gate2_b)[:, half * 512:(half + 1) * 512]
            P.dve(TT(dst, bnk[:, :], adab_g[:, gi, half * 512:(half + 1) * 512], ALU.add),
                  reads=[("bank", gi * 2 + half), f"adab_g{gi}"], writes=[("gate", gi, half)])
            if half == 1:
                P.dma("sp", DMA(gsc[gi], gate1_b if gi == 0 else gate2_b), reads=[("gate", gi, 0), ("gate", gi, 1)], writes=[("gsc", gi)])
        else:
            if v in (3, 4):
                gi = v - 1
                bi = (v - 3) * 2 + half
                for k in range(8):
                    P.pe(MM(banks[bi][:, :], c_rep[:, k, :], buf[:, k, :], k == 0, k == 7), reads=[bk, "c_rep"], writes=[("bank", bi)])
                dst = sb2_b[v - 3][:, half * 512:(half + 1) * 512]
                P.dve(TT(dst, banks[bi][:, :], adab_g[:, gi, half * 512:(half + 1) * 512], ALU.add),
                      reads=[("bank", bi), f"adab_g{gi}"], writes=[("sb2", v - 3, half)])
                if v == 4:
                    P.dve(STT(dst, dst, 1.0, n2g_b[:, half * 512:(half + 1) * 512], ALU.add, ALU.mult),
                          reads=[("sb2", 1, half), "n2g_b"], writes=[("sb2", 1, half)])
                if half == 1:
                    P.dma("sp", DMA(gsc[2 + v - 3], sb2_b[v - 3]), reads=[("sb2", v - 3, 0), ("sb2", v - 3, 1)], writes=[("gsc", 2 + v - 3)])
            for cc in range(4):
                j = v * 8 + half * 4 + cc
                for k in range(8):
                    mb = 4 if v < 2 else 5
                    P.pe(MM(banks[mb][:, j:j + 1], buf[:, k, cc * 128:(cc + 1) * 128], c_bf[:, k:k + 1], k == 0, k == 7),
                         reads=[bk, "c_bf"], writes=[("bank", mb)])
        if n_ == 3:
            P.dve(TT(modT[:, 0:16], banks[4][:, 0:16], adabT[:, 0:16], ALU.add),
                  reads=[("bank", 4), "adabT"], writes=["modT_a"])
            P.dve(STT(s1, modT[:, 8:16], 1.0, n1g, ALU.add, ALU.mult), reads=["modT_a", "n1g"], writes=["s1"])
            P.dve(CP(b1, modT[:, 0:8]), reads=["modT_a"], writes=["b1"])
        if n_ == 7:
            P.dve(TT(modT[:, 24:40], banks[5][:, 24:40], adabT[:, 24:40], ALU.add),
                  reads=[("bank", 5), "adabT"], writes=["modT_b"])
            P.dve(STT(s2, modT[:, 32:40], 1.0, n2g, ALU.add, ALU.mult), reads=["modT_b", "n2g"], writes=["s2"])
            P.dve(CP(b2, modT[:, 24:32]), reads=["modT_b"], writes=["b2"])
    if dbg:
        P.dma("sp", DMA(dbg_o["mod"], modT), reads=["modT_a", "modT_b"], writes=["dbg_mod"])
        P.dma("sp", DMA(dbg_o["g1"], gate1_b), reads=[("gate", 0, 0), ("gate", 0, 1)], writes=["dbg_g1"])

    if stop_after <= 0:
        P.emit()
        return nc, P
    def hkeys(tc, k=None):
        ks = range(8) if k is None else [k]
        return [("hT", tb, kk) for tb in range(tc * 4, tc * 4 + 4) for kk in ks]

    def norm_block(xb, xkey, ssq, rstd, junk, xn, xnkey, tb_tag, xkey2=None):
        xk = [xkey] if xkey2 is None else [xkey, xkey2]
        P.act(ACT(junk, xb, AF.Square, accum_out=ssq), reads=xk, writes=[("ssq", tb_tag), ("junk", tb_tag % 2)])
        P.dve(TS(rstd, ssq, 1.0 / D, EPS, ALU.mult, ALU.add), reads=[("ssq", tb_tag)], writes=[("rs", tb_tag)])
        P.act(ACT(rstd, rstd, AF.Sqrt), reads=[("rs", tb_tag)], writes=[("rs", tb_tag)])
        P.dve(RECIP(rstd, rstd), reads=[("rs", tb_tag)], writes=[("rs", tb_tag)])
        P.dve(TS(xn, xb, rstd, None, ALU.mult), reads=xk + [("rs", tb_tag)], writes=[xnkey])

    def load_branch_weights(br):
        base = 0 if br == 0 else 1544
        P.dma("pool", DMA(wq, win_v[:, :, base:base + 576]), writes=["wq"])
        P.dma("pool", DMA(wk, win_v[:, :, base + 512:base + 1088]), writes=["wk"])
        P.dma("pool", DMA(wv, win_v[:, :, base + 1024:base + 1536]), writes=["wv"])

    load_branch_weights(0)
    P.dma("pool", DMA(wfa, win_v[:, :, 1536:1544]), writes=["wfa"])
    xbuf = [A.alloc([128, D], F32) for _ in range(2)]
    junkb = [A.alloc([128, D], BF16) for _ in range(2)]
    xnb = [A.alloc([128, D], BF16) for _ in range(2)]
    ssq_t = A.alloc([128, NT], F32)
    rstd_t = A.alloc([128, NT], F32)
    import os as _os
    for tb in range(int(_os.environ.get('S1_N', NT))):
        xb = xbuf[tb % 2]
        P.dma("sp", DMA(xb, x_d[tb * 128:(tb + 1) * 128, :]), writes=[("xb", tb % 2)])
        norm_block(xb, ("xb", tb % 2), ssq_t[:, tb:tb + 1], rstd_t[:, tb:tb + 1], junkb[tb % 2], xnb[tb % 2], ("xn", tb % 2), tb)
        ba, bd = 4 + 2 * (tb % 2), 5 + 2 * (tb % 2)
        for k in range(8):
            bk_ = ba if k % 2 == 0 else bd
            P.pe(TR(bank_bf(bk_)[:, (k // 2) * 128:(k // 2 + 1) * 128], xnb[tb % 2][:, k * 128:(k + 1) * 128], identb),
                 reads=[("xn", tb % 2), "identb"], writes=[("bank", bk_)])
        for k in range(8):
            dst = hT[:, k, tb * 128:(tb + 1) * 128]
            bk_ = ba if k % 2 == 0 else bd
            src = bank_bf(bk_)[:, (k // 2) * 128:(k // 2 + 1) * 128]
            if k % 2 == 0:
                P.act(ACT(dst, src, AF.Identity, scale=s1[:, k:k + 1], bias=b1[:, k:k + 1]),
                      reads=[("bank", bk_), "s1", "b1"], writes=[("hT", tb, k)])
            else:
                P.dve(TS(dst, src, s1[:, k:k + 1], b1[:, k:k + 1], ALU.mult, ALU.add),
                      reads=[("bank", bk_), "s1", "b1"], writes=[("hT", tb, k)])
    if dbg:
        for k in range(8):
            P.dma("sp", DMA(dbg_o["hT"][:, k, 0:int(_os.environ.get('S1_N', NT)) * 128], hT[:, k, 0:int(_os.environ.get('S1_N', NT)) * 128]), reads=[("hT", tb, k) for tb in range(NT)], writes=[("dbg_hT", k)])
    if stop_after <= 1:
        P.emit()
        return nc, P

    P.barrier()
    A.reset("phase")
    onesb = A.alloc([128, 128], BF16)
    zerosb = A.alloc([128, 512], BF16)
    SLb = A.alloc([128, 128], BF16)
    mask_fox = [A.alloc([128, 512], BF16) for _ in range(4)]
    mask_sb = [A.alloc([128, 512], BF16) for _ in range(4)]
    negb = A.alloc([8, 1], F32)
    Qp = [A.alloc([128, S], BF16) for _ in range(2)]
    Kp = [A.alloc([128, S], BF16) for _ in range(2)]
    Vp = [A.alloc([128, NT, 128], BF16) for _ in range(2)]
    Pt = [A.alloc([128, 512], BF16) for _ in range(4)]
    e_sb = [A.alloc([128, 512], F32) for _ in range(2)]
    Lp = [A.alloc([128, 512], BF16) for _ in range(3)]
    accb = [A.alloc([128, 512], BF16) for _ in range(3)]
    acc32 = A.alloc([128, 512], F32)
    negonesb = A.alloc([128, 128], BF16)
    rcs = [A.alloc([128, 512], F32) for _ in range(2)]
    rc2 = [A.alloc([128, 512], F32) for _ in range(2)]
    osb = [A.alloc([128, 512], BF16) for _ in range(2)]
    f_e = [A.alloc([8, 512], F32)] * 2
    f_sp = [A.alloc([8, 512], F32)] * 2
    f_nF = [A.alloc([8, 512], F32) for _ in range(2)]
    f_r = [A.alloc([8, 512], F32)] * 2
    f_hb = [A.alloc([8, 12, 512], BF16)] * 2
    f_ones = A.alloc([8, 1], F32)

    P.dve(MEMSET(onesb, 1.0), writes=["onesb"])
    P.dve(MEMSET(negonesb, -1.0), writes=["negonesb"])
    P.dve(MEMSET(zerosb, 0.0), writes=["zerosb"])
    P.pool(lambda e: e.affine_select(out=SLb, in_=onesb, pattern=[[1, 128]], compare_op=ALU.is_ge,
                                     fill=0.0, base=-1, channel_multiplier=-1), reads=["onesb"], writes=["SLb"])
    for m in range(4):
        P.pool(lambda e, m=m: e.affine_select(out=mask_fox[m], in_=zerosb, pattern=[[1, 512]], compare_op=ALU.is_ge,
                                              fill=NEG, base=-128 * m, channel_multiplier=-1),
               reads=["zerosb"], writes=[("mfox", m)])
        P.pool(lambda e, m=m: e.affine_select(out=mask_sb[m], in_=zerosb, pattern=[[1, 512]], compare_op=ALU.is_ge,
                                              fill=NEG, base=-128 * m - 1, channel_multiplier=-1),
               reads=["zerosb"], writes=[("msb", m)])
    for i in range(2):
        P.dve(MEMSET(Qp[i][64:128, :], 0.0), writes=[("Qp", i, "augd")])
        P.dve(MEMSET(Kp[i][64:128, :], 0.0), writes=[("Kp", i, "augd")])
        P.dve(MEMSET(Vp[i][:, :, 64:128], 1.0), writes=[("Vp", i, "ones")])
    P.dve(MEMSET(f_ones, 1.0), writes=["f_ones"])
    P.dve(MEMSET(f_hb[0], 1.0), writes=[("f_hb", r) for r in (0, 9, 10, 11, "ones")])

    h2s_z = h2s_d.rearrange("r (a c) -> (r a) c", c=512)
    for i in range(NS * 8):
        P.dma("sp", DMA(h2s_z[i * 128:(i + 1) * 128, :], zerosb), reads=["zerosb"], writes=[("h2s_z", i)])
    P.dma("sp", DMA(cws_d.rearrange("(p a) e -> p (a e)", p=128), zerosb[:, 0:NS * 32].bitcast(F32)), reads=["zerosb"], writes=["cws_z"])
    P.dma("sp", DMA(negb, bfg_d), writes=["negb"])
    P.dve(TS(negb, negb, -1.0, None, ALU.mult), reads=["negb"], writes=["negb"])

    for tc in range(NC_):
        i2 = tc % 2
        fb = 7
        for k in range(8):
            P.pe(MM(banks[fb][0:8, :], wfa[:, k, :], hT[:, k, tc * 512:(tc + 1) * 512], k == 0, k == 7),
                 reads=["wfa"] + hkeys(tc, k), writes=[("bank", fb)])
        P.act(ACT(f_e[i2], banks[fb][0:8, :], AF.Exp, scale=-1.0, bias=negb[:, 0:1]), reads=[("bank", fb), "negb"], writes=["f_e"])
        P.act(ACT(f_sp[i2], f_e[i2], AF.Ln, bias=1.0), reads=["f_e"], writes=["f_sp"])
        init = 0.0 if tc == 0 else f_nF[1 - i2][:, 511:512]
        P.dve(lambda e, i2=i2, init=init: e.tensor_tensor_scan(out=f_nF[i2], data0=f_ones[:, 0:1].to_broadcast([8, 512]),
                                                                data1=f_sp[i2], initial=init, op0=ALU.mult, op1=ALU.add),
              reads=["f_sp", "f_ones", ("f_nF", 1 - i2)], writes=[("f_nF", i2)])
        hb = f_hb[i2]
        P.dve(CP(hb[:, 9, :], f_nF[i2]), reads=[("f_nF", i2)], writes=[("f_hb", 9)])
        P.dve(TT(f_r[i2], f_nF[i2], hb[:, 9, :], ALU.subtract), reads=[("f_nF", i2), ("f_hb", 9)], writes=["f_r"])
        P.dve(CP(hb[:, 10, :], f_r[i2]), reads=["f_r"], writes=[("f_hb", 10)])
        P.dve(TT(f_r[i2], f_r[i2], hb[:, 10, :], ALU.subtract), reads=["f_r", ("f_hb", 10)], writes=["f_r"])
        P.dve(CP(hb[:, 11, :], f_r[i2]), reads=["f_r"], writes=[("f_hb", 11)])
        P.pool(TS(hb[:, 0:3, :], hb[:, 9:12, :], -1.0, None, ALU.mult), reads=[("f_hb", 9), ("f_hb", 10), ("f_hb", 11)],
               writes=[("f_hb", 0)])
        P.dma("sp", DMA(fsc[:, :, tc * 512:(tc + 1) * 512], hb),
              reads=[("f_hb", r) for r in (0, 9, 10, 11, "ones")], writes=[("fsc", tc)])

    def proj_part(br, h, part, buf, pbanks=(7, 7, 7)):
        tc = part
        pb = pbanks[0]
        for k in range(8):
            P.pe(MM(banks[pb][:, :], wq[:, k, h * 64:h * 64 + 128], hT[:, k, tc * 512:(tc + 1) * 512], k == 0, k == 7),
                 reads=["wq"] + hkeys(tc, k), writes=[("bank", pb)])
        P.dve(TS(Qp[buf][0:64, tc * 512:(tc + 1) * 512], banks[pb][0:64, :], 0.125, None, ALU.mult),
              reads=[("bank", pb)], writes=[("Qp", buf, tc)])
        pb2 = pbanks[1]
        for k in range(8):
            P.pe(MM(banks[pb2][:, :], wk[:, k, h * 64:h * 64 + 128], hT[:, k, tc * 512:(tc + 1) * 512], k == 0, k == 7),
                 reads=["wk"] + hkeys(tc, k), writes=[("bank", pb2)])
        P.dve(CP(Kp[buf][0:64, tc * 512:(tc + 1) * 512], banks[pb2][0:64, :]),
              reads=[("bank", pb2)], writes=[("Kp", buf, tc)])
        pb = pbanks[2]
        vps = banks[pb][:, 0:256].rearrange("p (a b) -> p a b", b=64)
        for q in range(4):
            tb = part * 4 + q
            for k in range(8):
                P.pe(MM(vps[:, q, :], hT[:, k, tb * 128:(tb + 1) * 128], wv[:, k, h * 64:(h + 1) * 64], k == 0, k == 7),
                     reads=["wv", ("hT", tb, k)], writes=[("bank", pb)])
        P.dve(CP(Vp[buf][:, part * 4:(part + 1) * 4, 0:64], vps), reads=[("bank", pb)], writes=[("Vp", buf, part)])
        if br == 0 and part == 7:
            P.dma("sp", DMA(Qp[buf][64:70, :], fsc[h, 0:6, :]), reads=[("fsc", t) for t in range(NC_)], writes=[("Qp", buf, "augd")])
            P.dma("sp", DMA(Kp[buf][64:70, :], fsc[h, 6:12, :]), reads=[("fsc", t) for t in range(NC_)], writes=[("Kp", buf, "augd")])
        if br == 1 and h < 2 and part == 0:
            P.dve(MEMSET(Qp[buf][64:96, :], 0.0), writes=[("Qp", buf, "augd")])
            P.dve(MEMSET(Kp[buf][64:96, :], 0.0), writes=[("Kp", buf, "augd")])

    def qk_reads(buf, c, j, fox):
        return [("Qp", buf, c), ("Kp", buf, j // 4), ("Qp", buf, "augd"), ("Kp", buf, "augd")]

    def store_o(br, h, c, src_key, src):
        gh = br * 8 + h
        P.dma("sp", DMA(ot_scr[gh // 2, (gh % 2) * 64:(gh % 2) * 64 + 64, c * 512:(c + 1) * 512], src),
              reads=[src_key], writes=[("ot", gh, c)])

    def fox_head(h, buf, next_proj):
        tiles = []
        for c in range(NC_):
            if c == 0:
                tl = [(c, m, 0, m) for m in range(4)]
            else:
                tl = [(c, 4 * c + m, m * 128, m) for m in range(4)] + [(c, j, 0, None) for j in range(4 * c)]
            for n_, t in enumerate(tl):
                tiles.append(t + (n_ == 0, n_ == len(tl) - 1))
        nt = len(tiles)

        def pv(i):
            c, j, cs, m, first, last = tiles[i]
            ob = 4 + (c % 2)
            P.pe(MM(banks[ob][:, cs:512], Vp[buf][:, j, :], Pt[i % 4][:, cs:512], first, last),
                 reads=[("Vp", buf, j // 4), ("Vp", buf, "ones"), ("Pt", i % 4)], writes=[("bank", ob)])
            if last:
                c2 = c % 2
                P.dve(RECIP(rcs[c2][64:128, :], banks[ob][64:128, :]), reads=[("bank", ob)], writes=[("rcs", c2)])
                P.dma("sp", DMA(rc2[c2][0:64, :], rcs[c2][64:128, :]), reads=[("rcs", c2)], writes=[("rc2", c2)])
                P.dve(TT(osb[c2][0:64, :], banks[ob][0:64, :], rc2[c2][0:64, :], ALU.mult),
                      reads=[("bank", ob), ("rc2", c2)], writes=[("osb", c2)])
                store_o(0, h, c, ("osb", c2), osb[c2][0:64, :])
                if next_proj is not None:
                    next_proj(c)

        for i, (c, j, cs, m, first, last) in enumerate(tiles):
            ab = i % 3
            diag = m is not None
            P.pe(MM(banks[ab][:, cs:512], Kp[buf][:, j * 128:(j + 1) * 128], Qp[buf][:, c * 512 + cs:(c + 1) * 512], True, not diag),
                 reads=qk_reads(buf, c, j, True), writes=[("bank", ab)])
            if diag:
                mw = 512 if c == 0 else cs + 128
                P.pe(MM(banks[ab][:, cs:mw], identb, mask_fox[m][:, cs:mw], False, True),
                     reads=["identb", ("mfox", m)], writes=[("bank", ab)])
            P.act(ACT(Pt[i % 4][:, cs:512], banks[ab][:, cs:512], AF.Exp), reads=[("bank", ab)], writes=[("Pt", i % 4)])
            if i >= 2:
                pv(i - 2)
        pv(nt - 2)
        pv(nt - 1)

    A2B = (2, 3, 6)

    def sb_head(h, buf, next_proj):
        tiles = []
        for c in range(NC_):
            for j in range(4 * c + 3, -1, -1):
                m = j - 4 * c if j >= 4 * c else None
                tiles.append((c, j, 0 if (m is None or m == 3) else m * 128, m))
        nt = len(tiles)

        def zmm(i, bank, stop_last):
            c, j, cs, m = tiles[i]
            diag = m is not None
            P.pe(MM(banks[bank][:, cs:512], Kp[buf][:, j * 128:(j + 1) * 128], Qp[buf][:, c * 512 + cs:(c + 1) * 512], True, stop_last and not diag),
                 reads=qk_reads(buf, c, j, False), writes=[("bank", bank)])
            if diag:
                mw = 512 if m == 3 else cs + 128
                P.pe(MM(banks[bank][:, cs:mw], identb, mask_sb[m][:, cs:mw], False, stop_last),
                     reads=["identb", ("msb", m)], writes=[("bank", bank)])

        def st_z(i):
            zmm(i, i % 2, True)

        def st_exp1(i):
            cs = tiles[i][2]
            P.act(ACT(e_sb[i % 2][:, cs:512], banks[i % 2][:, cs:512], AF.Exp), reads=[("bank", i % 2)], writes=[("e_sb", i % 2)])

        def st_ln(i):
            cs = tiles[i][2]
            P.act(ACT(Lp[i % 3][:, cs:512], e_sb[i % 2][:, cs:512], AF.Ln, bias=1.0), reads=[("e_sb", i % 2)], writes=[("Lp", i % 3)])

        def st_acc(i):
            c, j, cs, m = tiles[i]
            L_ = Lp[i % 3]
            if m == 3:
                P.dve(CP(acc32, L_), reads=[("Lp", i % 3)], writes=["acc32"])
            else:
                P.dve(TT(acc32[:, cs:512], acc32[:, cs:512], L_[:, cs:512], ALU.add), reads=[("Lp", i % 3), "acc32"], writes=["acc32"])
            P.dve(CP(accb[i % 3][:, cs:512], acc32[:, cs:512]), reads=["acc32"], writes=[("accb", i % 3)])

        def st_g2(i):
            cs = tiles[i][2]
            bk = A2B[i % 3]
            zmm(i, bk, False)
            P.pe(MM(banks[bk][:, cs:512], SLb, Lp[i % 3][:, cs:512], False, False), reads=["SLb", ("Lp", i % 3)], writes=[("bank", bk)])
            P.pe(MM(banks[bk][:, cs:512], negonesb, accb[i % 3][:, cs:512], False, True), reads=["negonesb", ("accb", i % 3)], writes=[("bank", bk)])

        def st_exp2(i):
            cs = tiles[i][2]
            bk = A2B[i % 3]
            P.act(ACT(Pt[i % 3][:, cs:512], banks[bk][:, cs:512], AF.Exp), reads=[("bank", bk)], writes=[("Pt", i % 3)])

        def st_pv(i):
            c, j, cs, m = tiles[i]
            ob = 4 + (c % 2)
            rd = [("Vp", buf, j // 4), ("Vp", buf, "ones"), ("Pt", i % 3)]
            P.pe(MM(banks[ob][:, cs:512], Vp[buf][:, j, :], Pt[i % 3][:, cs:512], m == 3, j == 0), reads=rd, writes=[("bank", ob)])
            if j == 0:
                c2 = c % 2
                P.dve(CP(osb[c2][0:64, :], banks[ob][0:64, :]), reads=[("bank", ob)], writes=[("osb", c2)])
                store_o(1, h, c, ("osb", c2), osb[c2][0:64, :])
                if next_proj is not None:
                    next_proj(c)

        for i in range(nt + 4):
            if i < nt:
                st_z(i)
                st_exp1(i)
            if 0 <= i - 3 < nt:
                st_exp2(i - 3)
            if i < nt:
                st_ln(i)
            if 0 <= i - 1 < nt:
                st_acc(i - 1)
            if 0 <= i - 2 < nt:
                st_g2(i - 2)
            if 0 <= i - 4 < nt:
                st_pv(i - 4)

    heads = [(0, h) for h in range(8)] + [(1, h) for h in range(8)]
    n_heads_run = len(heads) if stop_after > 2 else (stop_after - 1) * 0 + 16
    if stop_after == 2 and dbg:
        n_heads_run = 16
    for part in range(8):
        proj_part(0, 0, part, 0, (7, 6, 3))
    for idx, (br, h) in enumerate(heads):
        buf = idx % 2
        if idx + 1 < len(heads):
            nbr, nh = heads[idx + 1]

            def next_proj(c, nbr=nbr, nh=nh, nbuf=1 - buf, idx=idx, br=br):
                if nbr == 1 and nh == 0 and c == 0:
                    load_branch_weights(1)
                proj_part(nbr, nh, c, nbuf, (7, 6, 3) if br == 0 else (7, 7, 7))
        else:
            next_proj = None
        ex_ = idx
        P.dma("pool", DMA(wg2b_d[ex_ * 256:(ex_ + 1) * 256, :], wg2_d[ex_ * 256:(ex_ + 1) * 256, :]), writes=[("wconv", ex_, 0)])
        P.dma("pool", DMA(wu2b_d[ex_ * 256:(ex_ + 1) * 256, :], wu2_d[ex_ * 256:(ex_ + 1) * 256, :]), writes=[("wconv", ex_, 1)])
        P.dma("pool", DMA(wd2b_d[ex_ * 512:(ex_ + 1) * 512, :], wd2_d[ex_ * 512:(ex_ + 1) * 512, :]), writes=[("wconv", ex_, 2)])
        if br == 0:
            fox_head(h, buf, next_proj)
        else:
            sb_head(h, buf, next_proj)
    if stop_after <= 2:
        P.emit()
        return nc, P

    P.barrier()
    A.reset("phase")
    A.top = A.nbytes
    wga = A.alloc([128, 8, D], BF16)
    wgb = A.alloc([128, 8, D], BF16)
    wbf = A.alloc([128, 4, D], BF16)
    wbs = A.alloc([128, 4, D], BF16)
    wout = A.alloc([128, 8, D], BF16)
    wr = A.alloc([128, 8, 20], F32)
    wrs = A.alloc([128, 8, 20], F32)
    rbias = A.alloc([128, 20], F32)
    br_b = A.alloc([128, 20], F32)
    otc = [A.alloc([128, 8, 512], BF16) for _ in range(2)]
    sg_sb = [A.alloc([128, 512], F32) for _ in range(2)]
    m_sb = [A.alloc([128, 512], F32) for _ in range(2)]
    mg = A.alloc([128, 8, 512], BF16)
    b2rep = mg[:, 0:4, :].rearrange("p a b -> p (a b)").bitcast(F32).rearrange("p (a b) -> p a b", b=128)
    xb3 = [A.alloc([128, D], F32) for _ in range(2)]
    x1b = [A.alloc([128, D], F32) for _ in range(2)]
    junk3 = [A.alloc([128, D], BF16)] * 2
    h2t = [A.alloc([128, D], BF16) for _ in range(2)]
    s2_b = A.alloc([128, D], F32)
    b2_b = A.alloc([128, D], F32)
    xT_sb = A.alloc([128, 8, 128], F32)
    ssq3 = A.alloc([128, NT], F32)
    rstd3 = A.alloc([128, NT], F32)
    gate1_b = A.alloc([128, D], F32)
    P.dma("sp", DMA(gate1_b[:, 0:512], gsc[0][:, 0:512]), writes=[("gate", 0, 0)])
    P.dma("sp", DMA(gate1_b[:, 512:1024], gsc[0][:, 512:1024]), writes=[("gate", 0, 1)])

    P.dma("pool", DMA(wga, win_v[:, :, 3080:4104]), writes=["wga"])
    P.dma("pool", DMA(wbf, wbf_d.rearrange("(q p) n -> p q n", p=128)), writes=["wbf"])
    P.dma("pool", DMA(wgb, win_v[:, :, 4104:5128]), writes=["wgb"])
    P.dma("pool", DMA(wbs, wbs_d.rearrange("(q p) n -> p q n", p=128)), writes=["wbs"])
    P.dma("pool", DMA(wout, wout_d.rearrange("(k p) n -> p k n", p=128)), writes=["wout"])
    P.dma("sp", DMA(wr, wr_d.rearrange("(k p) n -> p k n", p=128)), writes=["wr"])
    P.dma("sp", DMA(br_b, br_row_d[0:1, :].partition_broadcast(128)), writes=["br_b"])
    P.dve(TT(wrs, wr, s2[:, :].unsqueeze(2).to_broadcast([128, 8, 20]), ALU.mult), reads=["wr", "s2"], writes=["wrs"])
    P.dve(CP(b2rep, b2[:, :].unsqueeze(2).to_broadcast([128, 8, 128])), reads=["b2"], writes=[("mg", fc) for fc in range(8)])
    for k in range(8):
        P.pe(MM(banks[4][:, 0:20], b2rep[:, k, :], wr[:, k, :], k == 0, k == 7), reads=[("mg", fc) for fc in range(8)] + ["wr"], writes=[("bank", 4)])
    P.dma("sp", DMA(b2_b, gsc[2]), writes=["b2_b"])
    P.dma("sp", DMA(s2_b, gsc[3]), writes=["s2_b"])
    P.dve(TT(rbias, banks[4][:, 0:20], br_b, ALU.add), reads=[("bank", 4), "br_b"], writes=["rbias"])

    lg_all = A.alloc([128, NT, 20], F32)
    rbig = mg.rearrange("p a b -> p (a b)").bitcast(F32).rearrange("p (a b) -> p a b", b=64)

    def router_all():
        B_ = NT
        r = rbig
        X_ = mybir.AxisListType.X
        gl = lg_all[:, :, 0:4]
        el = lg_all[:, :, 4:20].rearrange("p b (g e) -> p b g e", e=4)
        gmax, m1, m2, d12, w1, w2, ssum, gp = (r[:, :, i] for i in range(8))
        dif, sg_, ex = r[:, :, 8:12], r[:, :, 12:16], r[:, :, 16:20]
        esel, oh1, es2, oh2, tmpw = r[:, :, 20:24], r[:, :, 24:28], r[:, :, 28:32], r[:, :, 32:36], r[:, :, 36:40]
        prod = r[:, :, 44:60].rearrange("p b (g e) -> p b g e", e=4)
        lk = [("lg", tb) for tb in range(NT)]

        def bc(v):
            return v.unsqueeze(2).to_broadcast([128, B_, 4])
        K = ["rbig"] + [("mg", fc) for fc in range(8)]
        P.dve(lambda e: e.reduce_max(out=gmax, in_=gl, axis=X_), reads=lk, writes=K)
        P.dve(TT(ohg_all, gl, bc(gmax), ALU.is_equal), reads=lk + K, writes=[("ohg", tb) for tb in range(NT)])
        P.dve(TT(dif, gl, bc(gmax), ALU.subtract), reads=lk + K, writes=K)
        P.act(ACT(sg_, dif, AF.Sigmoid), reads=K, writes=K)
        P.dve(TS(ex, sg_, -1.0, 1.0, ALU.mult, ALU.add), reads=K, writes=K)
        P.dve(RECIP(ex, ex), reads=K, writes=K)
        P.dve(TT(ex, ex, sg_, ALU.mult), reads=K, writes=K)
        P.dve(lambda e: e.reduce_sum(out=ssum, in_=ex, axis=X_), reads=K, writes=K)
        P.dve(RECIP(gp, ssum), reads=K, writes=K)
        P.dve(TT(prod, el, ohg_all.unsqueeze(3).to_broadcast([128, B_, 4, 4]), ALU.mult),
              reads=lk + [("ohg", tb) for tb in range(NT)], writes=K)
        P.dve(lambda e: e.reduce_sum(out=esel, in_=prod.rearrange("p b g e -> p b e g"), axis=X_), reads=K, writes=K)
        P.dve(lambda e: e.reduce_max(out=m1, in_=esel, axis=X_), reads=K, writes=K)
        P.dve(TT(oh1, esel, bc(m1), ALU.is_equal), reads=K, writes=K)
        P.dve(STT(es2, oh1, -1.0e30, esel, ALU.mult, ALU.add), reads=K, writes=K)
        P.dve(lambda e: e.reduce_max(out=m2, in_=es2, axis=X_), reads=K, writes=K)
        P.dve(TT(oh2, es2, bc(m2), ALU.is_equal), reads=K, writes=K)
        P.dve(TT(d12, m1, m2, ALU.subtract), reads=K, writes=K)
        P.act(ACT(w1, d12, AF.Sigmoid), reads=K, writes=K)
        P.dve(TS(w2, w1, -1.0, 1.0, ALU.mult, ALU.add), reads=K, writes=K)
        P.dve(TT(w1, w1, gp, ALU.mult), reads=K, writes=K)
        P.dve(TT(w2, w2, gp, ALU.mult), reads=K, writes=K)
        P.dve(TT(tmpw, oh1, bc(w1), ALU.mult), reads=K, writes=K)
        P.dve(TT(oh2, oh2, bc(w2), ALU.mult), reads=K, writes=K)
        P.dve(TT(cw_all, tmpw, oh2, ALU.add), reads=K, writes=[("cw", tb) for tb in range(NT)])

    ot_v = ot_scr.rearrange("q p t -> p q t")

    def blk_p1(tc, q4):
        tb = tc * 4 + q4
        t2 = tb % 2
        P.dma("sp", DMA(xb3[t2], x_d[tb * 128:(tb + 1) * 128, :]), writes=[("xb3", t2)])
        for hf in range(2):
            yb_ = 4 + hf
            for fc in range(8):
                P.pe(MM(banks[yb_][:, :], mg[:, fc, q4 * 128:(q4 + 1) * 128], wout[:, fc, hf * 512:(hf + 1) * 512], fc == 0, fc == 7),
                     reads=[("mg", fc), "wout"], writes=[("bank", yb_)])
            P.dve(TT(x1b[t2][:, hf * 512:(hf + 1) * 512], banks[yb_][:, :], gate1_b[:, hf * 512:(hf + 1) * 512], ALU.mult),
                  reads=[("bank", yb_), ("gate", 0, hf)], writes=[("x1b", t2, hf)])
        xk = [("x1b", t2, 0), ("x1b", t2, 1)]
        P.pool(TT(x1b[t2], x1b[t2], xb3[t2], ALU.add), reads=xk + [("xb3", t2)], writes=xk)
        P.dma("sp", DMA(out_d[tb * 128:(tb + 1) * 128, :], x1b[t2]), reads=xk, writes=[("out", tb)])
        P.act(ACT(junk3[t2], x1b[t2], AF.Square, accum_out=ssq3[:, tb:tb + 1]), reads=xk, writes=[("ssq", 100 + tb), "junk3"])

    def blk_p2(tb):
        t2 = tb % 2
        tg = 100 + tb
        xk = [("x1b", t2, 0), ("x1b", t2, 1)]
        P.dve(TS(rstd3[:, tb:tb + 1], ssq3[:, tb:tb + 1], 1.0 / D, EPS, ALU.mult, ALU.add), reads=[("ssq", tg)], writes=[("rs", tg)])
        P.act(ACT(rstd3[:, tb:tb + 1], rstd3[:, tb:tb + 1], AF.Sqrt), reads=[("rs", tg)], writes=[("rs", tg)])
        P.dve(RECIP(rstd3[:, tb:tb + 1], rstd3[:, tb:tb + 1]), reads=[("rs", tg)], writes=[("rs", tg)])
        for k in range(8):
            bk_ = 6 + k // 4
            tv = banks[bk_][:, :].rearrange("p (a b) -> p a b", b=128)
            P.pe(TR(tv[:, k % 4, :], x1b[t2][:, k * 128:(k + 1) * 128], identf), reads=xk + ["identf"], writes=[("bank", bk_)])
        for hh in range(2):
            P.dve(CP(xT_sb[:, hh * 4:(hh + 1) * 4, :], banks[6 + hh][:, :].rearrange("p (a b) -> p a b", b=128)),
                  reads=[("bank", 6 + hh)], writes=[("xT", hh)])
        for k in range(8):
            P.pe(MM(banks[6][:, 0:20], xT_sb[:, k, :], wrs[:, k, :], k == 0, k == 7), reads=[("xT", 0), ("xT", 1), "wrs"], writes=[("bank", 6)])
        P.dve(STT(lg_all[:, tb, :], banks[6][:, 0:20], rstd3[:, tb:tb + 1], rbias, ALU.mult, ALU.add),
              reads=[("bank", 6), ("rs", tg), "rbias"], writes=[("lg", tb)])
        P.dve(STT(xb3[t2], x1b[t2], rstd3[:, tb:tb + 1], s2_b, ALU.mult, ALU.mult), reads=xk + [("rs", tg), "s2_b"], writes=[("xb3", t2)])
        P.pool(TT(h2t[t2], xb3[t2], b2_b, ALU.add), reads=[("xb3", t2), "b2_b"], writes=[("h2t", t2)])
        P.dma("sp", DMA(h2tok_d[tb * 128:(tb + 1) * 128, :], h2t[t2]), reads=[("h2t", t2)], writes=[("h2tok", tb)])

    for tc in range(NC_):
        o2 = tc % 2
        P.dma("sp", DMA(otc[o2], ot_v[:, :, tc * 512:(tc + 1) * 512]),
              reads=[("ot", gh, tc) for gh in range(16)], writes=[("otc", o2)])
        for fc in range(8):
            for side in range(2):
                wgx = wga if side == 0 else wgb
                wbx = wbf if side == 0 else wbs
                wgk = "wga" if side == 0 else "wgb"
                wbk = "wbf" if side == 0 else "wbs"
                gbk, bbk = side, 2 + side
                for k in range(8):
                    P.pe(MM(banks[gbk][:, :], wgx[:, k, fc * 128:(fc + 1) * 128], hT[:, k, tc * 512:(tc + 1) * 512], k == 0, k == 7),
                         reads=[wgk] + hkeys(tc, k), writes=[("bank", gbk)])
                P.act(ACT(sg_sb[side], banks[gbk][:, :], AF.Sigmoid), reads=[("bank", gbk)], writes=[("sg", side)])
                for q in range(4):
                    P.pe(MM(banks[bbk][:, :], wbx[:, q, fc * 128:(fc + 1) * 128], otc[o2][:, side * 4 + q, :], q == 0, q == 3),
                         reads=[wbk, ("otc", o2)], writes=[("bank", bbk)])
                P.dve(TT(m_sb[side], banks[bbk][:, :], sg_sb[side], ALU.mult), reads=[("bank", bbk), ("sg", side)], writes=[("m", side)])
            P.pool(TT(mg[:, fc, :], m_sb[0], m_sb[1], ALU.add), reads=[("m", 0), ("m", 1)], writes=[("mg", fc)])
        for q4 in range(4):
            blk_p1(tc, q4)
            if tc * 4 + q4 >= 1:
                blk_p2(tc * 4 + q4 - 1)
    blk_p2(NT - 1)
    router_all()
    if dbg:
        P.dma("sp", DMA(dbg_o["comb"], cw_all), reads=[("cw", tb) for tb in range(NT)], writes=["dbg_comb"])
    if stop_after <= 3:
        P.emit()
        return nc, P

    P.barrier()
    A.reset("pre_hT")
    U32 = mybir.dt.uint32
    rconst = A.alloc([128, 40], F32)
    SUTf = A.alloc([128, 128], F32)
    rank_sb = A.alloc([128, NT, 4], F32)
    tot_sb = A.alloc([128, NT, 4], F32)
    incl = A.alloc([128, NT, 4], F32)
    Tt = A.alloc([128, NT, 4], F32)
    posf = A.alloc([128, NT], F32)
    idx = A.alloc([128, NT], U32)
    Ng = A.alloc([128, 4], F32)
    cnt = A.alloc([128, 4], F32)
    Pg = A.alloc([128, 4], F32)
    off = A.alloc([128, 4], F32)
    endg = A.alloc([128, 4], F32)
    gsl = A.alloc([128, NS], F32)
    gk = A.alloc([128, NS], F32)
    idx1f = A.alloc([128, NS, 8], F32)
    idx1 = A.alloc([128, NS, 8], U32)
    idx2f = A.alloc([128, NS, 16], F32)
    idx2 = A.alloc([128, NS, 16], U32)
    gate2_b = A.alloc([128, D], F32)
    fg_b = A.alloc([128, D], F32)
    xrow4 = [A.alloc([128, 4, D], BF16) for _ in range(2)]
    xs = [A.alloc([128, 4, D], BF16) for _ in range(2)]
    cwS = [A.alloc([128, 4, 4], F32) for _ in range(2)]
    h2Ts = [A.alloc([128, 8, 512], BF16) for _ in range(2)]
    wgs = [A.alloc([128, 8, 512], BF16) for _ in range(2)]
    wus = [A.alloc([128, 8, 512], BF16) for _ in range(2)]
    wds = [A.alloc([128, 4, D], BF16) for _ in range(2)]
    actT = [A.alloc([128, 4, 512], BF16) for _ in range(2)]
    sgm = [A.alloc([128, 512], F32) for _ in range(2)]
    yacc = [A.alloc([128, 4, D], F32) for _ in range(2)]
    yb = [A.alloc([128, D], F32) for _ in range(2)]
    x1f = [A.alloc([128, D], F32) for _ in range(2)]
    ssq4 = A.alloc([128, NT], F32)
    rstd4 = A.alloc([128, NT], F32)
    junk4 = A.alloc([128, D], BF16)

    P.dma("sp", DMA(rconst, rc_d), writes=["rconst"])
    P.dma("sp", DMA(gate2_b, gsc[1]), writes=[("gate", 1, 0), ("gate", 1, 1)])
    P.dma("sp", DMA(fg_b, fg_row_d[0:1, :].partition_broadcast(128)), writes=["fg_b"])
    P.pool(lambda e: e.affine_select(out=SUTf, in_=onesf, pattern=[[1, 128]], compare_op=ALU.is_ge,
                                     fill=0.0, base=-1, channel_multiplier=-1), reads=["onesf"], writes=["SUTf"])
    okeys = [("ohg", tb) for tb in range(NT)]
    ohg2d = ohg_all.rearrange("p a b -> p (a b)")
    P.pe(MM(banks[0][:, 0:128], SUTf, ohg2d, True, True), reads=["SUTf"] + okeys, writes=[("bank", 0)])
    P.pe(MM(banks[1][:, 0:128], onesf, ohg2d, True, True), reads=["onesf"] + okeys, writes=[("bank", 1)])
    P.dve(CP(rank_sb.rearrange("p a b -> p (a b)"), banks[0][:, 0:128]), reads=[("bank", 0)], writes=["rank_sb"])
    P.dve(CP(tot_sb.rearrange("p a b -> p (a b)"), banks[1][:, 0:128]), reads=[("bank", 1)], writes=["tot_sb"])
    for g in range(4):
        P.dve(lambda e, g=g: e.tensor_tensor_scan(out=incl[:, :, g], data0=onesf[:, 0:NT], data1=tot_sb[:, :, g], initial=0.0,
                                                  op0=ALU.mult, op1=ALU.add), reads=["tot_sb", "onesf"], writes=[("incl", g)])
    ik = [("incl", g) for g in range(4)]
    P.dve(TT(Tt, incl, tot_sb, ALU.subtract), reads=ik + ["tot_sb"], writes=["Tt"])
    P.dve(TT(Tt, Tt, rank_sb, ALU.add), reads=["Tt", "rank_sb"], writes=["Tt"])
    P.dve(CP(Ng, incl[:, NT - 1, :]), reads=ik, writes=["Ng"])
    P.dve(MEMSET(cnt, 0.0), writes=["cnt"])
    for k in range(8):
        P.dve(STT(cnt, Ng, 512.0 * k, cnt, ALU.is_gt, ALU.add), reads=["Ng", "cnt"], writes=["cnt"])
    P.dve(TS(Pg, cnt, 512.0, None, ALU.mult), reads=["cnt"], writes=["Pg"])
    P.dve(MEMSET(off, 0.0), writes=["off"])
    for g in range(1, 4):
        P.dve(TT(off[:, g:g + 1], off[:, g - 1:g], Pg[:, g - 1:g], ALU.add), reads=["off", "Pg"], writes=["off"])
    P.dve(TT(endg, off, Pg, ALU.add), reads=["off", "Pg"], writes=["endg"])
    for g in range(4):
        P.dve(TS(Tt[:, :, g], Tt[:, :, g], off[:, g:g + 1], None, ALU.add), reads=["Tt", "off"], writes=["Tt"])
    P.dve(TT(Tt, Tt, ohg_all, ALU.mult), reads=["Tt"] + okeys, writes=["Tt"])
    P.dve(lambda e: e.reduce_sum(out=posf, in_=Tt, axis=mybir.AxisListType.X), reads=["Tt"], writes=["posf"])
    P.dve(CP(idx, posf), reads=["posf"], writes=["idx"])
    P.dve(MEMSET(gsl, 0.0), writes=["gsl"])
    for g in range(4):
        P.dve(STT(gsl, rconst[:, 24:36], endg[:, g:g + 1], gsl, ALU.is_ge, ALU.add), reads=["rconst", "endg", "gsl"], writes=["gsl"])
    P.dve(TS(gsl, gsl, 3.0, None, ALU.min), reads=["gsl"], writes=["gsl"])
    P.dve(TS(gk, gsl, 1024.0, None, ALU.mult), reads=["gsl"], writes=["gk"])
    P.dve(TT(idx1f, rconst[:, 0:8].unsqueeze(1).to_broadcast([128, NS, 8]), gk[:, :].unsqueeze(2).to_broadcast([128, NS, 8]), ALU.add),
          reads=["rconst", "gk"], writes=["idx1f"])
    P.dve(CP(idx1, idx1f), reads=["idx1f"], writes=["idx1"])
    P.dve(TS(gk, gsl, 2048.0, None, ALU.mult), reads=["gsl", "idx1f"], writes=["gk"])
    P.dve(TT(idx2f, rconst[:, 8:24].unsqueeze(1).to_broadcast([128, NS, 16]), gk[:, :].unsqueeze(2).to_broadcast([128, NS, 16]), ALU.add),
          reads=["rconst", "gk"], writes=["idx2f"])
    P.dve(CP(idx2, idx2f), reads=["idx2f"], writes=["idx2"])

    IOA = bass.IndirectOffsetOnAxis
    for g4 in range(NT // 4):
        r2 = g4 % 2
        P.dma("sp", DMA(xrow4[r2], h2tok_d[g4 * 512:(g4 + 1) * 512, :].rearrange("(a p) n -> p a n", p=128)), writes=[("xrow", r2)])
        for a_ in range(4):
            tb = g4 * 4 + a_
            P.dma("pool", lambda e, tb=tb, r2=r2, a_=a_: e.indirect_dma_start(out=h2s_d[:, :], out_offset=IOA(ap=idx[:, tb:tb + 1], axis=0),
                                                                              in_=xrow4[r2][:, a_, :], in_offset=None),
                  reads=[("xrow", r2), "idx"], writes=[("h2s_sc", tb)])
            P.dma("pool", lambda e, tb=tb: e.indirect_dma_start(out=cws_d[:, :], out_offset=IOA(ap=idx[:, tb:tb + 1], axis=0),
                                                                in_=cw_all[:, tb, :], in_offset=None),
                  reads=[("cw", tb), "idx"], writes=[("cws_sc", tb)])
    sc_all = [("h2s_sc", tb) for tb in range(NT)]
    cw_sc_all = [("cws_sc", tb) for tb in range(NT)]

    def slot_prep(sl):
        s2_ = sl % 2
        P.dma("sp", DMA(xs[s2_], h2s_d[sl * 512:(sl + 1) * 512, :].rearrange("(a p) n -> p a n", p=128)),
              reads=sc_all, writes=[("xs", s2_)])
        P.dma("sp", DMA(cwS[s2_], cws_d[sl * 512:(sl + 1) * 512, :].rearrange("(a p) e -> p a e", p=128)),
              reads=cw_sc_all, writes=[("cwS", s2_)])
        for a_ in range(4):
            tbk = 6 + (a_ % 2)
            pT = bank_bf(tbk)
            for k in range(8):
                P.pe(TR(pT[:, k * 128:(k + 1) * 128], xs[s2_][:, a_, k:D:8], identb), reads=[("xs", s2_), "identb"], writes=[("bank", tbk)])
            P.act(ACT(h2Ts[s2_][:, :, a_ * 128:(a_ + 1) * 128], pT.rearrange("p (k t) -> p k t", t=128), AF.Identity),
                  reads=[("bank", tbk)], writes=[("h2Ts", s2_, a_)])

    slot_prep(0)
    for sl in range(NS):
        s2_ = sl % 2
        hk = [("h2Ts", s2_, a_) for a_ in range(4)]
        for el in range(4):
            it = sl * 4 + el
            wb = it % 2
            for hf in range(2):
                P.dma("pool", lambda e, wb=wb, hf=hf, sl=sl, el=el: e.indirect_dma_start(
                    out=wgs[wb].rearrange("p k n -> p (k n)")[:, hf * 2048:(hf + 1) * 2048], out_offset=None, in_=wg2b_d[:, :],
                    in_offset=IOA(ap=idx1[:, sl, el * 2 + hf:el * 2 + hf + 1], axis=0)), reads=["idx1"], writes=[("wgs", wb, hf)])
                P.dma("pool", lambda e, wb=wb, hf=hf, sl=sl, el=el: e.indirect_dma_start(
                    out=wus[wb].rearrange("p k n -> p (k n)")[:, hf * 2048:(hf + 1) * 2048], out_offset=None, in_=wu2b_d[:, :],
                    in_offset=IOA(ap=idx1[:, sl, el * 2 + hf:el * 2 + hf + 1], axis=0)), reads=["idx1"], writes=[("wus", wb, hf)])
            for q in range(4):
                P.dma("pool", lambda e, wb=wb, q=q, sl=sl, el=el: e.indirect_dma_start(
                    out=wds[wb][:, q, :], out_offset=None, in_=wd2b_d[:, :],
                    in_offset=IOA(ap=idx2[:, sl, el * 4 + q:el * 4 + q + 1], axis=0)), reads=["idx2"], writes=[("wds", wb, q)])
            wdk = [("wds", wb, q) for q in range(4)]
            if el == 2 and sl + 1 < NS:
                slot_prep(sl + 1)
            a2 = it % 2
            for ffc in range(4):
                gb_, ub_ = ffc % 2, 2 + (ffc % 2)
                for k in range(8):
                    P.pe(MM(banks[gb_][:, :], wgs[wb][:, k, ffc * 128:(ffc + 1) * 128], h2Ts[s2_][:, k, :], k == 0, k == 7),
                         reads=[("wgs", wb, 0), ("wgs", wb, 1)] + hk, writes=[("bank", gb_)])
                for k in range(8):
                    P.pe(MM(banks[ub_][:, :], wus[wb][:, k, ffc * 128:(ffc + 1) * 128], h2Ts[s2_][:, k, :], k == 0, k == 7),
                         reads=[("wus", wb, 0), ("wus", wb, 1)] + hk, writes=[("bank", ub_)])
                P.act(ACT(sgm[ffc % 2], banks[gb_][:, :], AF.Silu), reads=[("bank", gb_)], writes=[("sgm", ffc % 2)])
                P.dve(TT(actT[a2][:, ffc, :], banks[ub_][:, :], sgm[ffc % 2], ALU.mult),
                      reads=[("bank", ub_), ("sgm", ffc % 2)], writes=[("actT", a2, ffc)])
            for a_ in range(4):
                for hf in range(2):
                    ybk = 4 + ((a_ * 2 + hf) % 2)
                    for ffc in range(4):
                        P.pe(MM(banks[ybk][:, :], actT[a2][:, ffc, a_ * 128:(a_ + 1) * 128], wds[wb][:, ffc, hf * 512:(hf + 1) * 512], ffc == 0, ffc == 3),
                             reads=[("actT", a2, f) for f in range(4)] + wdk, writes=[("bank", ybk)])
                    yv = yacc[s2_][:, a_, hf * 512:(hf + 1) * 512]
                    cwv = cwS[s2_][:, a_, el:el + 1]
                    if el == 0:
                        P.dve(TS(yv, banks[ybk][:, :], cwv, None, ALU.mult), reads=[("bank", ybk), ("cwS", s2_)], writes=[("yacc", s2_, a_, hf)])
                    else:
                        P.dve(STT(yv, banks[ybk][:, :], cwv, yv, ALU.mult, ALU.add),
                              reads=[("bank", ybk), ("cwS", s2_), ("yacc", s2_, a_, hf)], writes=[("yacc", s2_, a_, hf)])
        P.dma("sp", DMA(ys_d[sl * 512:(sl + 1) * 512, :].rearrange("(a p) n -> p a n", p=128), yacc[s2_]),
              reads=[("yacc", s2_, a_, hf) for a_ in range(4) for hf in range(2)], writes=[("ys", sl)])
    ys_all = [("ys", sl) for sl in range(NS)]
    for tb in range(NT):
        t2 = tb % 2
        P.dma("pool", lambda e, tb=tb, t2=t2: e.indirect_dma_start(out=yb[t2], out_offset=None, in_=ys_d[:, :],
                                                                   in_offset=IOA(ap=idx[:, tb:tb + 1], axis=0)),
              reads=["idx"] + ys_all, writes=[("yb", t2)])
        P.dma("sp", DMA(x1f[t2], out_d[tb * 128:(tb + 1) * 128, :]), writes=[("x1f", t2)])
        P.dve(TT(yb[t2], yb[t2], gate2_b, ALU.mult), reads=[("yb", t2), ("gate", 1, 0), ("gate", 1, 1)], writes=[("yb", t2)])
        P.dve(TT(x1f[t2], x1f[t2], yb[t2], ALU.add), reads=[("x1f", t2), ("yb", t2)], writes=[("x1f", t2)])
        P.act(ACT(junk4, x1f[t2], AF.Square, accum_out=ssq4[:, tb:tb + 1]), reads=[("x1f", t2)], writes=[("ssq4", tb), "junk4"])
        P.dve(TS(rstd4[:, tb:tb + 1], ssq4[:, tb:tb + 1], 1.0 / D, EPS, ALU.mult, ALU.add), reads=[("ssq4", tb)], writes=[("rs4", tb)])
        P.act(ACT(rstd4[:, tb:tb + 1], rstd4[:, tb:tb + 1], AF.Sqrt), reads=[("rs4", tb)], writes=[("rs4", tb)])
        P.dve(RECIP(rstd4[:, tb:tb + 1], rstd4[:, tb:tb + 1]), reads=[("rs4", tb)], writes=[("rs4", tb)])
        P.dve(STT(x1f[t2], x1f[t2], rstd4[:, tb:tb + 1], fg_b, ALU.mult, ALU.mult), reads=[("x1f", t2), ("rs4", tb), "fg_b"], writes=[("x1f", t2)])
        P.dma("sp", DMA(out_d[tb * 128:(tb + 1) * 128, :], x1f[t2]), reads=[("x1f", t2)], writes=[("out", tb)])
    P.emit()
    return nc, P


def _rconst():
    c = np.zeros((128, 40), np.float32)
    p = np.arange(128, dtype=np.float32)
    for el in range(4):
        for hf in range(2):
            c[:, el * 2 + hf] = 2 * p + 256 * el + hf
        for q in range(4):
            c[:, 8 + el * 4 + q] = p + 512 * el + 128 * q
    for sl in range(12):
        c[:, 24 + sl] = 512.0 * sl
    return c


def make_in_maps(inp):
    f = lambda a: np.ascontiguousarray(np.asarray(a, dtype=np.float32))
    B = inp["x"].shape[0]
    ada_b = f(inp["ada_b"][0])
    shared = {
        "ada_w": f(inp["ada_w"][0]),
        "ada_bT": f(ada_b.reshape(48, 128).T),
        "ada_b_row": f(ada_b.reshape(1, -1)),
        "n1g": f(inp["norm1_g"][0].reshape(8, 128).T),
        "n2g": f(inp["norm2_g"][0].reshape(8, 128).T),
        "fg_row": f(inp["final_g"].reshape(1, -1)),
        "w_in": f(inp["w_in"][0]),
        "b_forget": f(inp["b_forget"][0].reshape(8, 1)),
        "w_bf": f(inp["w_branch_fox"][0]),
        "w_bs": f(inp["w_branch_sb"][0]),
        "w_out": f(inp["w_out"][0]),
        "w_r": f(np.concatenate([inp["router_group_w"][0], inp["router_expert_w"][0]], axis=1)),
        "b_r_row": f(np.concatenate([inp["router_group_b"][0], inp["router_expert_b"][0]], axis=0).reshape(1, 20)),
        "e_wg": f(inp["expert_w_gate"][0]).reshape(16 * 256, 2048),
        "e_wu": f(inp["expert_w_up"][0]).reshape(16 * 256, 2048),
        "e_wd": f(inp["expert_w_down"][0]).reshape(16 * 512, 1024),
        "n2g_row": f(inp["norm2_g"][0].reshape(1, -1)),
        "rconst": _rconst(),
    }
    maps = []
    for b in range(B):
        m = dict(shared)
        m["x"] = f(inp["x"][b])
        m["cT"] = f(np.asarray(inp["c"][b]).reshape(8, 128).T)
        maps.append(m)
    return maps


_CACHE = {}


def kernel(**inputs):
    if "nc" not in _CACHE:
        _CACHE["nc"] = build_program()[0]
    nc = _CACHE["nc"]
    in_maps = make_in_maps(inputs)
    res = run_bass_kernel_spmd(nc, in_maps, core_ids=list(range(len(in_maps))))
    return np.stack([np.asarray(r["out"], dtype=np.float32) for r in res.results], axis=0)
```

```python
import contextlib
import numpy as np
import concourse.bass as bass
import concourse.mybir as mybir
from concourse.bass_utils import run_bass_kernel_spmd

F32 = mybir.dt.float32
BF16 = mybir.dt.bfloat16
U8 = mybir.dt.uint8
AF = mybir.ActivationFunctionType
ALU = mybir.AluOpType

S = 4096
D = 1024
NT = S // 128
NC_ = S // 512
NIN = 5128
NEG = -30000.0
EPS = 1e-6


class Prog:
    ENGS = ("pe", "act", "dve", "pool", "sp")

    def __init__(self, nc, n_dma_sems=12):
        self.nc = nc
        self.ops = []
        self.n_dma_sems = n_dma_sems

    def add(self, eng, fn, reads=(), writes=(), dma=False, barrier=False):
        self.ops.append(dict(eng=eng, fn=fn, reads=tuple(reads), writes=tuple(writes), dma=dma, barrier=barrier))

    def pe(self, fn, reads=(), writes=()): self.add("pe", fn, reads, writes)
    def act(self, fn, reads=(), writes=()): self.add("act", fn, reads, writes)
    def dve(self, fn, reads=(), writes=()): self.add("dve", fn, reads, writes)
    def pool(self, fn, reads=(), writes=()): self.add("pool", fn, reads, writes)
    def dma(self, eng, fn, reads=(), writes=()): self.add(eng, fn, reads, writes, dma=True)
    def barrier(self): self.add(None, None, barrier=True)

    def build(self):
        ops = self.ops
        n = len(ops)
        last_w, readers = {}, {}
        deps = [set() for _ in range(n)]
        last_on_eng = {}
        dma_since = []
        bar_deps = None
        for i, o in enumerate(ops):
            if o["barrier"]:
                bar_deps = set(last_on_eng.values()) | set(dma_since)
                last_w, readers = {}, {}
                continue
            if bar_deps is not None:
                pass
            for k in o["reads"]:
                if k in last_w:
                    deps[i].add((last_w[k], False))
                if isinstance(k, tuple) and k[0] == "bank":
                    for r in readers.get(k, ()):
                        if ops[r]["eng"] != o["eng"]:
                            deps[i].add((r, False))
            for k in o["writes"]:
                if k in last_w:
                    deps[i].add((last_w[k], False))
                for r in readers.get(k, ()):
                    if r != i:
                        deps[i].add((r, True))
            for k in o["reads"]:
                readers.setdefault(k, []).append(i)
            for k in o["writes"]:
                last_w[k] = i
                readers[k] = []
            o["bar"] = bar_deps
            if o["dma"]:
                dma_since.append(i)
            else:
                last_on_eng[o["eng"]] = i
        fdeps = [set() for _ in range(n)]
        first_after_bar = {}
        for i, o in enumerate(ops):
            if o["barrier"]:
                continue
            for d, war in deps[i]:
                p = ops[d]
                if (not p["dma"]) and (not o["dma"]) and p["eng"] == o["eng"]:
                    if o["eng"] == "pe":
                        continue
                fdeps[i].add(d)
            bd = o.get("bar")
            if bd is not None and first_after_bar.get(o["eng"]) is not bd:
                first_after_bar[o["eng"]] = bd
                for d in bd:
                    if d != i and not (ops[d]["eng"] == o["eng"] and not ops[d]["dma"] and not o["dma"]):
                        fdeps[i].add(d)
        needs_inc = [False] * n
        for i in range(n):
            for d in fdeps[i]:
                needs_inc[d] = True
        eng_count = {e: 0 for e in self.ENGS}
        ticket = [None] * n
        dma_rr = {e: 0 for e in self.ENGS}
        dma_tot = {}
        pre_wait = [None] * n
        for i, o in enumerate(ops):
            if o["barrier"]:
                continue
            if o["dma"]:
                e = o["eng"]
                s = (e, dma_rr[e] % self.n_dma_sems)
                dma_rr[e] += 1
                prev = dma_tot.get(s, 0)
                if prev > 0:
                    pre_wait[i] = (s, prev)
                dma_tot[s] = prev + 16
                ticket[i] = (s, prev + 16)
            elif needs_inc[i]:
                eng_count[o["eng"]] += 1
                ticket[i] = (o["eng"], eng_count[o["eng"]])
        self.final_dma = dict(dma_tot)
        progs = {e: [] for e in self.ENGS}
        waited = {e: {} for e in self.ENGS}
        for i, o in enumerate(ops):
            if o["barrier"]:
                continue
            e = o["eng"]
            cand = [ticket[d] for d in fdeps[i]]
            if pre_wait[i] is not None:
                cand.append(pre_wait[i])
            best = {}
            for (s, v) in cand:
                if v > best.get(s, 0):
                    best[s] = v
            ws = []
            for s, v in best.items():
                if waited[e].get(s, 0) >= v:
                    continue
                waited[e][s] = v
                ws.append((s, v))
            progs[e].append((ws, o["fn"], ticket[i], o["dma"]))
        self.stats = {e: len(progs[e]) for e in self.ENGS}
        return progs

    def emit(self):
        nc = self.nc
        progs = self.build()
        semkeys = set()
        for e in self.ENGS:
            for ws, fn, t, isdma in progs[e]:
                for s, v in ws:
                    semkeys.add(s)
                if t is not None:
                    semkeys.add(t[0])
        semkeys = sorted(semkeys, key=str)
        with contextlib.ExitStack() as st:
            sems = {}
            for k in semkeys:
                nm = "s_" + (k if isinstance(k, str) else f"{k[0]}{k[1]}")
                sems[k] = st.enter_context(nc.semaphore(nm))
            block = st.enter_context(nc.Block())
            final = self.final_dma

            def make(e):
                def body(eng):
                    for ws, fn, t, isdma in progs[e]:
                        for s, v in ws:
                            eng.wait_ge(sems[s], v)
                        ins = fn(eng)
                        if t is not None:
                            ins.then_inc(sems[t[0]], 16 if isdma else 1)
                    if e == "sp":
                        for s, v in final.items():
                            eng.wait_ge(sems[s], v)
                return body
            block.tensor(make("pe"))
            block.scalar(make("act"))
            block.vector(make("dve"))
            block.gpsimd(make("pool"))
            block.sync(make("sp"))


class Arena:
    def __init__(self, nc, nbytes):
        self.t = nc.alloc_sbuf_tensor("arena", [128, nbytes], U8)
        self.nbytes = nbytes
        self.off = 0
        self.top = nbytes
        self.marks = {}

    def mark(self, name):
        self.marks[name] = self.off

    def reset(self, name):
        self.off = self.marks[name]

    def alloc_top(self, shape, dt):
        esz = 2 if dt == BF16 else 4
        n = int(np.prod(shape[1:]))
        nb = (n * esz + 31) // 32 * 32
        self.top -= nb
        v = self.t[0:shape[0], self.top:self.top + n * esz].bitcast(dt)
        if len(shape) == 3:
            v = v.rearrange("p (a b) -> p a b", b=shape[2])
        return v

    def alloc(self, shape, dt):
        esz = 2 if dt == BF16 else 4
        n = int(np.prod(shape[1:]))
        nb = (n * esz + 31) // 32 * 32
        assert self.off + nb <= self.top, (self.off, nb, self.top)
        v = self.t[0:shape[0], self.off:self.off + n * esz].bitcast(dt)
        self.off += nb
        if len(shape) == 3:
            v = v.rearrange("p (a b) -> p a b", b=shape[2])
        elif len(shape) == 4:
            v = v.rearrange("p (a b c) -> p a b c", b=shape[2], c=shape[3])
        return v


def MM(out, lhsT, rhs, start, stop):
    return lambda e: e.matmul(out, lhsT, rhs, start=start, stop=stop)


def TR(out, in_, ident):
    return lambda e: e.transpose(out, in_, ident)


def ACT(out, in_, func, **kw):
    return lambda e: e.activation(out=out, in_=in_, func=func, **kw)


def TT(out, in0, in1, op):
    return lambda e: e.tensor_tensor(out=out, in0=in0, in1=in1, op=op)


def TS(out, in0, s1, s2, op0, op1=None):
    if op1 is None:
        return lambda e: e.tensor_scalar(out=out, in0=in0, scalar1=s1, scalar2=None, op0=op0)
    return lambda e: e.tensor_scalar(out=out, in0=in0, scalar1=s1, scalar2=s2, op0=op0, op1=op1)


def STT(out, in0, scalar, in1, op0, op1):
    return lambda e: e.scalar_tensor_tensor(out=out, in0=in0, scalar=scalar, in1=in1, op0=op0, op1=op1)


def CP(out, in_):
    return lambda e: e.tensor_copy(out=out, in_=in_)


def DMA(out, in_):
    return lambda e: e.dma_start(out=out, in_=in_)


def MEMSET(ap, v):
    return lambda e: e.memset(ap, v)


def RECIP(out, in_):
    return lambda e: e.reciprocal(out=out, in_=in_)


def build_program(stop_after=99, dbg=False):
    nc = bass.Bass("TRN2", target_bir_lowering=False)

    def din(name, shape, dt=F32):
        return nc.dram_tensor(name, shape, dt, kind="ExternalInput").ap()

    x_d = din("x", [S, D])
    cT_d = din("cT", [128, 8])
    adaw_d = din("ada_w", [D, 6 * D])
    adabT_d = din("ada_bT", [128, 48])
    adab_row_d = din("ada_b_row", [1, 6 * D])
    n1g_d = din("n1g", [128, 8])
    n2g_d = din("n2g", [128, 8])
    fg_row_d = din("fg_row", [1, D])
    win_d = din("w_in", [D, NIN])
    bfg_d = din("b_forget", [8, 1])
    wbf_d = din("w_bf", [512, D])
    wbs_d = din("w_bs", [512, D])
    wout_d = din("w_out", [D, D])
    wr_d = din("w_r", [D, 20])
    br_row_d = din("b_r_row", [1, 20])
    wg2_d = din("e_wg", [16 * 256, 2048])
    wu2_d = din("e_wu", [16 * 256, 2048])
    wd2_d = din("e_wd", [16 * 512, D])
    n2g_row_d = din("n2g_row", [1, D])
    rc_d = din("rconst", [128, 40])
    out_d = nc.dram_tensor("out", [S, D], F32, kind="ExternalOutput").ap()
    ot_scr = nc.dram_tensor("ot_scr", [8, 128, S], BF16, kind="ExternalOutput" if dbg else "Internal").ap()
    fsc = nc.dram_tensor("fsc", [8, 12, S], BF16, kind="Internal").ap()
    gsc = nc.dram_tensor("gsc", [4, 128, D], F32, kind="Internal").ap()
    NS = 12
    h2tok_d = nc.dram_tensor("h2tok", [S, D], BF16, kind="Internal").ap()
    h2s_d = nc.dram_tensor("h2s", [NS * 512, D], BF16, kind="Internal").ap()
    cws_d = nc.dram_tensor("cws", [NS * 512, 4], F32, kind="Internal").ap()
    ys_d = nc.dram_tensor("ys", [NS * 512, D], F32, kind="Internal").ap()
    wg2b_d = nc.dram_tensor("wg2b", [16 * 256, 2048], BF16, kind="Internal").ap()
    wu2b_d = nc.dram_tensor("wu2b", [16 * 256, 2048], BF16, kind="Internal").ap()
    wd2b_d = nc.dram_tensor("wd2b", [16 * 512, D], BF16, kind="Internal").ap()
    dbg_o = {}
    if dbg:
        dbg_o["hT"] = nc.dram_tensor("dbg_hT", [128, 8, S], BF16, kind="ExternalOutput").ap()
        dbg_o["mod"] = nc.dram_tensor("dbg_mod", [128, 48], F32, kind="ExternalOutput").ap()
        dbg_o["g1"] = nc.dram_tensor("dbg_g1", [128, D], F32, kind="ExternalOutput").ap()
        dbg_o["comb"] = nc.dram_tensor("dbg_comb", [128, NT, 4], F32, kind="ExternalOutput").ap()
        dbg_o["h2T"] = nc.dram_tensor("dbg_h2T", [128, 8, S], BF16, kind="ExternalOutput").ap()

    win_v = win_d.rearrange("(k p) n -> p k n", p=128)
    adaw_v = adaw_d.rearrange("(k p) n -> p k n", p=128)

    P = Prog(nc)
    A = Arena(nc, 206 * 1024)
    banks = [nc.alloc_psum_tensor(f"bank{i}", [128, 512], F32) for i in range(8)]

    def bank_bf(i):
        return banks[i][:, :].bitcast(BF16)

    ohg_all = A.alloc([128, NT, 4], F32)
    cw_all = A.alloc([128, NT, 4], F32)
    s1 = A.alloc([128, 8], F32)
    b1 = A.alloc([128, 8], F32)
    s2 = A.alloc([128, 8], F32)
    b2 = A.alloc([128, 8], F32)
    n1g = A.alloc([128, 8], F32)
    n2g = A.alloc([128, 8], F32)
    modT = A.alloc([128, 48], F32)
    small = A.alloc([128, 64], F32)
    identb = A.alloc([128, 128], BF16)
    identf = A.alloc([128, 128], F32)
    onesf = A.alloc([128, 128], F32)
    A.mark("pre_hT")
    hT = A.alloc([128, 8, S], BF16)
    A.mark("phase")
    wq = A.alloc_top([128, 8, 576], BF16)
    wk = A.alloc_top([128, 8, 576], BF16)
    wv = A.alloc_top([128, 8, 512], BF16)
    wfa = A.alloc_top([128, 8, 8], BF16)

    P.pool(MEMSET(onesf, 1.0), writes=["onesf"])
    P.pool(lambda e: e.affine_select(out=identf, in_=onesf, pattern=[[-1, 128]], compare_op=ALU.is_equal,
                                     fill=0.0, base=0, channel_multiplier=1), reads=["onesf"], writes=["identf"])
    P.pool(CP(identb, identf), reads=["identf"], writes=["identb"])
    P.pool(MEMSET(modT, 0.0), writes=["modT_a", "modT_b"])
    P.dma("sp", DMA(n1g, n1g_d), writes=["n1g"])
    P.dma("sp", DMA(n2g, n2g_d), writes=["n2g"])

    gate1_b = A.alloc([128, D], F32)
    gate2_b = A.alloc([128, D], F32)
    cT = A.alloc([128, 8], F32)
    c_bf = A.alloc([128, 8], BF16)
    c_rep = A.alloc([128, 8, 128], BF16)
    adabT = A.alloc([128, 48], F32)
    adab_g = A.alloc([128, 4, D], F32)
    n2g_b = A.alloc([128, D], F32)
    sb2_b = [A.alloc([128, D], F32) for _ in range(2)]
    adaw_sb = [A.alloc([128, 8, 512], BF16) for _ in range(3)]
    P.dma("sp", DMA(cT, cT_d), writes=["cT"])
    P.dma("sp", DMA(adabT, adabT_d), writes=["adabT"])
    P.dma("sp", DMA(adab_g[:, 0, :], adab_row_d[0:1, 2 * D:3 * D].partition_broadcast(128)), writes=["adab_g0"])
    P.dma("sp", DMA(adab_g[:, 1, :], adab_row_d[0:1, 5 * D:6 * D].partition_broadcast(128)), writes=["adab_g1"])
    P.dma("sp", DMA(adab_g[:, 2, :], adab_row_d[0:1, 3 * D:4 * D].partition_broadcast(128)), writes=["adab_g2"])
    P.dma("sp", DMA(adab_g[:, 3, :], adab_row_d[0:1, 4 * D:5 * D].partition_broadcast(128)), writes=["adab_g3"])
    P.dma("sp", DMA(n2g_b, n2g_row_d[0:1, :].partition_broadcast(128)), writes=["n2g_b"])
    P.act(ACT(c_bf, cT, AF.Silu), reads=["cT"], writes=["c_bf"])
    P.dve(CP(c_rep, c_bf[:, :].unsqueeze(2).to_broadcast([128, 8, 128])), reads=["c_bf"], writes=["c_rep"])
    order = [0, 1, 2, 3, 6, 7, 8, 9, 4, 5, 10, 11]
    for n_, q in enumerate(order):
        buf = adaw_sb[n_ % 3]
        bk = ("adaw", n_ % 3)
        P.dma("pool", DMA(buf, adaw_v[:, :, q * 512:(q + 1) * 512]), writes=[bk])
        v, half = q // 2, q % 2
        if v in (2, 5):
            gi = 0 if v == 2 else 1
            bnk = banks[gi * 2 + half]
            for k in range(8):
                P.pe(MM(bnk[:, :], c_rep[:, k, :], buf[:, k, :], k == 0, k == 7), reads=[bk, "c_rep"], writes=[("bank", gi * 2 + half)])
            dst = (gate1_b if gi == 0 else gate2_b)[:, half * 512:(half + 1) * 512]
            P.dve(TT(dst, bnk[:, :], adab_g[:, gi, half * 512:(half + 1) * 512], ALU.add),
                  reads=[("bank", gi * 2 + half), f"adab_g{gi}"], writes=[("gate", gi, half)])
            if half == 1:
                P.dma("sp", DMA(gsc[gi], gate1_b if gi == 0 else gate2_b), reads=[("gate", gi, 0), ("gate", gi, 1)], writes=[("gsc", gi)])
        else:
            if v in (3, 4):
                gi = v - 1
                bi = (v - 3) * 2 + half
                for k in range(8):
                    P.pe(MM(banks[bi][:, :], c_rep[:, k, :], buf[:, k, :], k == 0, k == 7), reads=[bk, "c_rep"], writes=[("bank", bi)])
                dst = sb2_b[v - 3][:, half * 512:(half + 1) * 512]
                P.dve(TT(dst, banks[bi][:, :], adab_g[:, gi, half * 512:(half + 1) * 512], ALU.add),
                      reads=[("bank", bi), f"adab_g{gi}"], writes=[("sb2", v - 3, half)])
                if v == 4:
                    P.dve(STT(dst, dst, 1.0, n2g_b[:, half * 512:(half + 1) * 512], ALU.add, ALU.mult),
                          reads=[("sb2", 1, half), "n2g_b"], writes=[("sb2", 1, half)])
                if half == 1:
                    P.dma("sp", DMA(gsc[2 + v - 3], sb2_b[v - 3]), reads=[("sb2", v - 3, 0), ("sb2", v - 3, 1)], writes=[("gsc", 2 + v - 3)])
            for cc in range(4):
                j = v * 8 + half * 4 + cc
                for k in range(8):
                    mb = 4 if v < 2 else 5
                    P.pe(MM(banks[mb][:, j:j + 1], buf[:, k, cc * 128:(cc + 1) * 128], c_bf[:, k:k + 1], k == 0, k == 7),
                         reads=[bk, "c_bf"], writes=[("bank", mb)])
        if n_ == 3:
            P.dve(TT(modT[:, 0:16], banks[4][:, 0:16], adabT[:, 0:16], ALU.add),
                  reads=[("bank", 4), "adabT"], writes=["modT_a"])
            P.dve(STT(s1, modT[:, 8:16], 1.0, n1g, ALU.add, ALU.mult), reads=["modT_a", "n1g"], writes=["s1"])
            P.dve(CP(b1, modT[:, 0:8]), reads=["modT_a"], writes=["b1"])
        if n_ == 7:
            P.dve(TT(modT[:, 24:40], banks[5][:, 24:40], adabT[:, 24:40], ALU.add),
                  reads=[("bank", 5), "adabT"], writes=["modT_b"])
            P.dve(STT(s2, modT[:, 32:40], 1.0, n2g, ALU.add, ALU.mult), reads=["modT_b", "n2g"], writes=["s2"])
            P.dve(CP(b2, modT[:, 24:32]), reads=["modT_b"], writes=["b2"])
    if dbg:
        P.dma("sp", DMA(dbg_o["mod"], modT), reads=["modT_a", "modT_b"], writes=["dbg_mod"])
        P.dma("sp", DMA(dbg_o["g1"], gate1_b), reads=[("gate", 0, 0), ("gate", 0, 1)], writes=["dbg_g1"])

    if stop_after <= 0:
        P.emit()
        return nc, P
    def hkeys(tc, k=None):
        ks = range(8) if k is None else [k]
        return [("hT", tb, kk) for tb in range(tc * 4, tc * 4 + 4) for kk in ks]

    def norm_block(xb, xkey, ssq, rstd, junk, xn, xnkey, tb_tag, xkey2=None):
        xk = [xkey] if xkey2 is None else [xkey, xkey2]
        P.act(ACT(junk, xb, AF.Square, accum_out=ssq), reads=xk, writes=[("ssq", tb_tag), ("junk", tb_tag % 2)])
        P.dve(TS(rstd, ssq, 1.0 / D, EPS, ALU.mult, ALU.add), reads=[("ssq", tb_tag)], writes=[("rs", tb_tag)])
        P.act(ACT(rstd, rstd, AF.Sqrt), reads=[("rs", tb_tag)], writes=[("rs", tb_tag)])
        P.dve(RECIP(rstd, rstd), reads=[("rs", tb_tag)], writes=[("rs", tb_tag)])
        P.dve(TS(xn, xb, rstd, None, ALU.mult), reads=xk + [("rs", tb_tag)], writes=[xnkey])

    def load_branch_weights(br):
        base = 0 if br == 0 else 1544
        P.dma("pool", DMA(wq, win_v[:, :, base:base + 576]), writes=["wq"])
        P.dma("pool", DMA(wk, win_v[:, :, base + 512:base + 1088]), writes=["wk"])
        P.dma("pool", DMA(wv, win_v[:, :, base + 1024:base + 1536]), writes=["wv"])

    load_branch_weights(0)
    P.dma("pool", DMA(wfa, win_v[:, :, 1536:1544]), writes=["wfa"])
    xbuf = [A.alloc([128, D], F32) for _ in range(2)]
    junkb = [A.alloc([128, D], BF16) for _ in range(2)]
    xnb = [A.alloc([128, D], BF16) for _ in range(2)]
    ssq_t = A.alloc([128, NT], F32)
    rstd_t = A.alloc([128, NT], F32)
    import os as _os
    for tb in range(int(_os.environ.get('S1_N', NT))):
        xb = xbuf[tb % 2]
        P.dma("sp", DMA(xb, x_d[tb * 128:(tb + 1) * 128, :]), writes=[("xb", tb % 2)])
        norm_block(xb, ("xb", tb % 2), ssq_t[:, tb:tb + 1], rstd_t[:, tb:tb + 1], junkb[tb % 2], xnb[tb % 2], ("xn", tb % 2), tb)
        ba, bd = 4 + 2 * (tb % 2), 5 + 2 * (tb % 2)
        for k in range(8):
            bk_ = ba if k % 2 == 0 else bd
            P.pe(TR(bank_bf(bk_)[:, (k // 2) * 128:(k // 2 + 1) * 128], xnb[tb % 2][:, k * 128:(k + 1) * 128], identb),
                 reads=[("xn", tb % 2), "identb"], writes=[("bank", bk_)])
        for k in range(8):
            dst = hT[:, k, tb * 128:(tb + 1) * 128]
            bk_ = ba if k % 2 == 0 else bd
            src = bank_bf(bk_)[:, (k // 2) * 128:(k // 2 + 1) * 128]
            if k % 2 == 0:
                P.act(ACT(dst, src, AF.Identity, scale=s1[:, k:k + 1], bias=b1[:, k:k + 1]),
                      reads=[("bank", bk_), "s1", "b1"], writes=[("hT", tb, k)])
            else:
                P.dve(TS(dst, src, s1[:, k:k + 1], b1[:, k:k + 1], ALU.mult, ALU.add),
                      reads=[("bank", bk_), "s1", "b1"], writes=[("hT", tb, k)])
    if dbg:
        for k in range(8):
            P.dma("sp", DMA(dbg_o["hT"][:, k, 0:int(_os.environ.get('S1_N', NT)) * 128], hT[:, k, 0:int(_os.environ.get('S1_N', NT)) * 128]), reads=[("hT", tb, k) for tb in range(NT)], writes=[("dbg_hT", k)])
    if stop_after <= 1:
        P.emit()
        return nc, P

    P.barrier()
    A.reset("phase")
    onesb = A.alloc([128, 128], BF16)
    zerosb = A.alloc([128, 512], BF16)
    SLb = A.alloc([128, 128], BF16)
    mask_fox = [A.alloc([128, 512], BF16) for _ in range(4)]
    mask_sb = [A.alloc([128, 512], BF16) for _ in range(4)]
    negb = A.alloc([8, 1], F32)
    Qp = [A.alloc([128, S], BF16) for _ in range(2)]
    Kp = [A.alloc([128, S], BF16) for _ in range(2)]
    Vp = [A.alloc([128, NT, 128], BF16) for _ in range(2)]
    Pt = [A.alloc([128, 512], BF16) for _ in range(4)]
    e_sb = [A.alloc([128, 512], F32) for _ in range(2)]
    Lp = [A.alloc([128, 512], BF16) for _ in range(3)]
    accb = [A.alloc([128, 512], BF16) for _ in range(3)]
    acc32 = A.alloc([128, 512], F32)
    negonesb = A.alloc([128, 128], BF16)
    rcs = [A.alloc([128, 512], F32) for _ in range(2)]
    rc2 = [A.alloc([128, 512], F32) for _ in range(2)]
    osb = [A.alloc([128, 512], BF16) for _ in range(2)]
    f_e = [A.alloc([8, 512], F32)] * 2
    f_sp = [A.alloc([8, 512], F32)] * 2
    f_nF = [A.alloc([8, 512], F32) for _ in range(2)]
    f_r = [A.alloc([8, 512], F32)] * 2
    f_hb = [A.alloc([8, 12, 512], BF16)] * 2
    f_ones = A.alloc([8, 1], F32)

    P.dve(MEMSET(onesb, 1.0), writes=["onesb"])
    P.dve(MEMSET(negonesb, -1.0), writes=["negonesb"])
    P.dve(MEMSET(zerosb, 0.0), writes=["zerosb"])
    P.pool(lambda e: e.affine_select(out=SLb, in_=onesb, pattern=[[1, 128]], compare_op=ALU.is_ge,
                                     fill=0.0, base=-1, channel_multiplier=-1), reads=["onesb"], writes=["SLb"])
    for m in range(4):
        P.pool(lambda e, m=m: e.affine_select(out=mask_fox[m], in_=zerosb, pattern=[[1, 512]], compare_op=ALU.is_ge,
                                              fill=NEG, base=-128 * m, channel_multiplier=-1),
               reads=["zerosb"], writes=[("mfox", m)])
        P.pool(lambda e, m=m: e.affine_select(out=mask_sb[m], in_=zerosb, pattern=[[1, 512]], compare_op=ALU.is_ge,
                                              fill=NEG, base=-128 * m - 1, channel_multiplier=-1),
               reads=["zerosb"], writes=[("msb", m)])
    for i in range(2):
        P.dve(MEMSET(Qp[i][64:128, :], 0.0), writes=[("Qp", i, "augd")])
        P.dve(MEMSET(Kp[i][64:128, :], 0.0), writes=[("Kp", i, "augd")])
        P.dve(MEMSET(Vp[i][:, :, 64:128], 1.0), writes=[("Vp", i, "ones")])
    P.dve(MEMSET(f_ones, 1.0), writes=["f_ones"])
    P.dve(MEMSET(f_hb[0], 1.0), writes=[("f_hb", r) for r in (0, 9, 10, 11, "ones")])

    h2s_z = h2s_d.rearrange("r (a c) -> (r a) c", c=512)
    for i in range(NS * 8):
        P.dma("sp", DMA(h2s_z[i * 128:(i + 1) * 128, :], zerosb), reads=["zerosb"], writes=[("h2s_z", i)])
    P.dma("sp", DMA(cws_d.rearrange("(p a) e -> p (a e)", p=128), zerosb[:, 0:NS * 32].bitcast(F32)), reads=["zerosb"], writes=["cws_z"])
    P.dma("sp", DMA(negb, bfg_d), writes=["negb"])
    P.dve(TS(negb, negb, -1.0, None, ALU.mult), reads=["negb"], writes=["negb"])

    for tc in range(NC_):
        i2 = tc % 2
        fb = 7
        for k in range(8):
            P.pe(MM(banks[fb][0:8, :], wfa[:, k, :], hT[:, k, tc * 512:(tc + 1) * 512], k == 0, k == 7),
                 reads=["wfa"] + hkeys(tc, k), writes=[("bank", fb)])
        P.act(ACT(f_e[i2], banks[fb][0:8, :], AF.Exp, scale=-1.0, bias=negb[:, 0:1]), reads=[("bank", fb), "negb"], writes=["f_e"])
        P.act(ACT(f_sp[i2], f_e[i2], AF.Ln, bias=1.0), reads=["f_e"], writes=["f_sp"])
        init = 0.0 if tc == 0 else f_nF[1 - i2][:, 511:512]
        P.dve(lambda e, i2=i2, init=init: e.tensor_tensor_scan(out=f_nF[i2], data0=f_ones[:, 0:1].to_broadcast([8, 512]),
                                                                data1=f_sp[i2], initial=init, op0=ALU.mult, op1=ALU.add),
              reads=["f_sp", "f_ones", ("f_nF", 1 - i2)], writes=[("f_nF", i2)])
        hb = f_hb[i2]
        P.dve(CP(hb[:, 9, :], f_nF[i2]), reads=[("f_nF", i2)], writes=[("f_hb", 9)])
        P.dve(TT(f_r[i2], f_nF[i2], hb[:, 9, :], ALU.subtract), reads=[("f_nF", i2), ("f_hb", 9)], writes=["f_r"])
        P.dve(CP(hb[:, 10, :], f_r[i2]), reads=["f_r"], writes=[("f_hb", 10)])
        P.dve(TT(f_r[i2], f_r[i2], hb[:, 10, :], ALU.subtract), reads=["f_r", ("f_hb", 10)], writes=["f_r"])
        P.dve(CP(hb[:, 11, :], f_r[i2]), reads=["f_r"], writes=[("f_hb", 11)])
        P.dve(TS(hb[:, 0:3, :], hb[:, 9:12, :], -1.0, None, ALU.mult), reads=[("f_hb", 9), ("f_hb", 10), ("f_hb", 11)],
              writes=[("f_hb", 0)])
        P.dma("sp", DMA(fsc[:, :, tc * 512:(tc + 1) * 512], hb),
              reads=[("f_hb", r) for r in (0, 9, 10, 11, "ones")], writes=[("fsc", tc)])

    def proj_part(br, h, part, buf, pbanks=(7, 7, 7)):
        tc = part
        pb = pbanks[0]
        for k in range(8):
            P.pe(MM(banks[pb][:, :], wq[:, k, h * 64:h * 64 + 128], hT[:, k, tc * 512:(tc + 1) * 512], k == 0, k == 7),
                 reads=["wq"] + hkeys(tc, k), writes=[("bank", pb)])
        P.dve(TS(Qp[buf][0:64, tc * 512:(tc + 1) * 512], banks[pb][0:64, :], 0.125, None, ALU.mult),
              reads=[("bank", pb)], writes=[("Qp", buf, tc)])
        pb2 = pbanks[1]
        for k in range(8):
            P.pe(MM(banks[pb2][:, :], wk[:, k, h * 64:h * 64 + 128], hT[:, k, tc * 512:(tc + 1) * 512], k == 0, k == 7),
                 reads=["wk"] + hkeys(tc, k), writes=[("bank", pb2)])
        P.dve(CP(Kp[buf][0:64, tc * 512:(tc + 1) * 512], banks[pb2][0:64, :]),
              reads=[("bank", pb2)], writes=[("Kp", buf, tc)])
        pb = pbanks[2]
        vps = banks[pb][:, 0:256].rearrange("p (a b) -> p a b", b=64)
        for q in range(4):
            tb = part * 4 + q
            for k in range(8):
                P.pe(MM(vps[:, q, :], hT[:, k, tb * 128:(tb + 1) * 128], wv[:, k, h * 64:(h + 1) * 64], k == 0, k == 7),
                     reads=["wv", ("hT", tb, k)], writes=[("bank", pb)])
        P.dve(CP(Vp[buf][:, part * 4:(part + 1) * 4, 0:64], vps), reads=[("bank", pb)], writes=[("Vp", buf, part)])
        if br == 0 and part == 7:
            P.dma("sp", DMA(Qp[buf][64:70, :], fsc[h, 0:6, :]), reads=[("fsc", t) for t in range(NC_)], writes=[("Qp", buf, "augd")])
            P.dma("sp", DMA(Kp[buf][64:70, :], fsc[h, 6:12, :]), reads=[("fsc", t) for t in range(NC_)], writes=[("Kp", buf, "augd")])
        if br == 1 and h < 2 and part == 0:
            P.dve(MEMSET(Qp[buf][64:96, :], 0.0), writes=[("Qp", buf, "augd")])
            P.dve(MEMSET(Kp[buf][64:96, :], 0.0), writes=[("Kp", buf, "augd")])

    def qk_reads(buf, c, j, fox):
        return [("Qp", buf, c), ("Kp", buf, j // 4), ("Qp", buf, "augd"), ("Kp", buf, "augd")]

    def store_o(br, h, c, src_key, src):
        gh = br * 8 + h
        P.dma("sp", DMA(ot_scr[gh // 2, (gh % 2) * 64:(gh % 2) * 64 + 64, c * 512:(c + 1) * 512], src),
              reads=[src_key], writes=[("ot", gh, c)])

    def fox_head(h, buf, next_proj):
        tiles = []
        for c in range(NC_):
            if c == 0:
                tl = [(c, m, 0, m) for m in range(4)]
            else:
                tl = [(c, 4 * c + m, m * 128, m) for m in range(4)] + [(c, j, 0, None) for j in range(4 * c)]
            for n_, t in enumerate(tl):
                tiles.append(t + (n_ == 0, n_ == len(tl) - 1))
        nt = len(tiles)

        def pv(i):
            c, j, cs, m, first, last = tiles[i]
            ob = 4 + (c % 2)
            P.pe(MM(banks[ob][:, cs:512], Vp[buf][:, j, :], Pt[i % 4][:, cs:512], first, last),
                 reads=[("Vp", buf, j // 4), ("Vp", buf, "ones"), ("Pt", i % 4)], writes=[("bank", ob)])
            if last:
                c2 = c % 2
                P.dve(RECIP(rcs[c2][64:128, :], banks[ob][64:128, :]), reads=[("bank", ob)], writes=[("rcs", c2)])
                P.dma("sp", DMA(rc2[c2][0:64, :], rcs[c2][64:128, :]), reads=[("rcs", c2)], writes=[("rc2", c2)])
                P.dve(TT(osb[c2][0:64, :], banks[ob][0:64, :], rc2[c2][0:64, :], ALU.mult),
                      reads=[("bank", ob), ("rc2", c2)], writes=[("osb", c2)])
                store_o(0, h, c, ("osb", c2), osb[c2][0:64, :])
                if next_proj is not None:
                    next_proj(c)

        for i, (c, j, cs, m, first, last) in enumerate(tiles):
            ab = i % 3
            diag = m is not None
            P.pe(MM(banks[ab][:, cs:512], Kp[buf][:, j * 128:(j + 1) * 128], Qp[buf][:, c * 512 + cs:(c + 1) * 512], True, not diag),
                 reads=qk_reads(buf, c, j, True), writes=[("bank", ab)])
            if diag:
                mw = 512 if c == 0 else cs + 128
                P.pe(MM(banks[ab][:, cs:mw], identb, mask_fox[m][:, cs:mw], False, True),
                     reads=["identb", ("mfox", m)], writes=[("bank", ab)])
            P.act(ACT(Pt[i % 4][:, cs:512], banks[ab][:, cs:512], AF.Exp), reads=[("bank", ab)], writes=[("Pt", i % 4)])
            if i >= 2:
                pv(i - 2)
        pv(nt - 2)
        pv(nt - 1)

    A2B = (2, 3, 6)

    def sb_head(h, buf, next_proj):
        tiles = []
        for c in range(NC_):
            for j in range(4 * c + 3, -1, -1):
                m = j - 4 * c if j >= 4 * c else None
                tiles.append((c, j, 0 if (m is None or m == 3) else m * 128, m))
        nt = len(tiles)

        def zmm(i, bank, stop_last):
            c, j, cs, m = tiles[i]
            diag = m is not None
            P.pe(MM(banks[bank][:, cs:512], Kp[buf][:, j * 128:(j + 1) * 128], Qp[buf][:, c * 512 + cs:(c + 1) * 512], True, stop_last and not diag),
                 reads=qk_reads(buf, c, j, False), writes=[("bank", bank)])
            if diag:
                mw = 512 if m == 3 else cs + 128
                P.pe(MM(banks[bank][:, cs:mw], identb, mask_sb[m][:, cs:mw], False, stop_last),
                     reads=["identb", ("msb", m)], writes=[("bank", bank)])

        def st_z(i):
            zmm(i, i % 2, True)

        def st_exp1(i):
            cs = tiles[i][2]
            P.act(ACT(e_sb[i % 2][:, cs:512], banks[i % 2][:, cs:512], AF.Exp), reads=[("bank", i % 2)], writes=[("e_sb", i % 2)])

        def st_ln(i):
            cs = tiles[i][2]
            P.act(ACT(Lp[i % 3][:, cs:512], e_sb[i % 2][:, cs:512], AF.Ln, bias=1.0), reads=[("e_sb", i % 2)], writes=[("Lp", i % 3)])

        def st_acc(i):
            c, j, cs, m = tiles[i]
            L_ = Lp[i % 3]
            if m == 3:
                P.dve(CP(acc32, L_), reads=[("Lp", i % 3)], writes=["acc32"])
            else:
                P.dve(TT(acc32[:, cs:512], acc32[:, cs:512], L_[:, cs:512], ALU.add), reads=[("Lp", i % 3), "acc32"], writes=["acc32"])
            P.dve(CP(accb[i % 3][:, cs:512], acc32[:, cs:512]), reads=["acc32"], writes=[("accb", i % 3)])

        def st_g2(i):
            cs = tiles[i][2]
            bk = A2B[i % 3]
            zmm(i, bk, False)
            P.pe(MM(banks[bk][:, cs:512], SLb, Lp[i % 3][:, cs:512], False, False), reads=["SLb", ("Lp", i % 3)], writes=[("bank", bk)])
            P.pe(MM(banks[bk][:, cs:512], negonesb, accb[i % 3][:, cs:512], False, True), reads=["negonesb", ("accb", i % 3)], writes=[("bank", bk)])

        def st_exp2(i):
            cs = tiles[i][2]
            bk = A2B[i % 3]
            P.act(ACT(Pt[i % 3][:, cs:512], banks[bk][:, cs:512], AF.Exp), reads=[("bank", bk)], writes=[("Pt", i % 3)])

        def st_pv(i):
            c, j, cs, m = tiles[i]
            ob = 4 + (c % 2)
            rd = [("Vp", buf, j // 4), ("Vp", buf, "ones"), ("Pt", i % 3)]
            P.pe(MM(banks[ob][:, cs:512], Vp[buf][:, j, :], Pt[i % 3][:, cs:512], m == 3, j == 0), reads=rd, writes=[("bank", ob)])
            if j == 0:
                c2 = c % 2
                P.dve(CP(osb[c2][0:64, :], banks[ob][0:64, :]), reads=[("bank", ob)], writes=[("osb", c2)])
                store_o(1, h, c, ("osb", c2), osb[c2][0:64, :])
                if next_proj is not None:
                    next_proj(c)

        for i in range(nt + 4):
            if i < nt:
                st_z(i)
                st_exp1(i)
            if 0 <= i - 3 < nt:
                st_exp2(i - 3)
            if i < nt:
                st_ln(i)
            if 0 <= i - 1 < nt:
                st_acc(i - 1)
            if 0 <= i - 2 < nt:
                st_g2(i - 2)
            if 0 <= i - 4 < nt:
                st_pv(i - 4)

    heads = [(0, h) for h in range(8)] + [(1, h) for h in range(8)]
    n_heads_run = len(heads) if stop_after > 2 else (stop_after - 1) * 0 + 16
    if stop_after == 2 and dbg:
        n_heads_run = 16
    for part in range(8):
        proj_part(0, 0, part, 0, (7, 6, 3))
    for idx, (br, h) in enumerate(heads):
        buf = idx % 2
        if idx + 1 < len(heads):
            nbr, nh = heads[idx + 1]

            def next_proj(c, nbr=nbr, nh=nh, nbuf=1 - buf, idx=idx, br=br):
                if nbr == 1 and nh == 0 and c == 0:
                    load_branch_weights(1)
                proj_part(nbr, nh, c, nbuf, (7, 6, 3) if br == 0 else (7, 7, 7))
        else:
            next_proj = None
        ex_ = idx
        P.dma("pool", DMA(wg2b_d[ex_ * 256:(ex_ + 1) * 256, :], wg2_d[ex_ * 256:(ex_ + 1) * 256, :]), writes=[("wconv", ex_, 0)])
        P.dma("pool", DMA(wu2b_d[ex_ * 256:(ex_ + 1) * 256, :], wu2_d[ex_ * 256:(ex_ + 1) * 256, :]), writes=[("wconv", ex_, 1)])
        P.dma("pool", DMA(wd2b_d[ex_ * 512:(ex_ + 1) * 512, :], wd2_d[ex_ * 512:(ex_ + 1) * 512, :]), writes=[("wconv", ex_, 2)])
        if br == 0:
            fox_head(h, buf, next_proj)
        else:
            sb_head(h, buf, next_proj)
    if stop_after <= 2:
        P.emit()
        return nc, P

    P.barrier()
    A.reset("phase")
    A.top = A.nbytes
    wga = A.alloc([128, 8, D], BF16)
    wgb = A.alloc([128, 8, D], BF16)
    wbf = A.alloc([128, 4, D], BF16)
    wbs = A.alloc([128, 4, D], BF16)
    wout = A.alloc([128, 8, D], BF16)
    wr = A.alloc([128, 8, 20], F32)
    wrs = A.alloc([128, 8, 20], F32)
    rbias = A.alloc([128, 20], F32)
    br_b = A.alloc([128, 20], F32)
    otc = [A.alloc([128, 8, 512], BF16) for _ in range(2)]
    sg_sb = [A.alloc([128, 512], F32) for _ in range(2)]
    m_sb = [A.alloc([128, 512], F32) for _ in range(2)]
    mg = A.alloc([128, 8, 512], BF16)
    b2rep = mg[:, 0:4, :].rearrange("p a b -> p (a b)").bitcast(F32).rearrange("p (a b) -> p a b", b=128)
    xb3 = [A.alloc([128, D], F32) for _ in range(2)]
    x1b = [A.alloc([128, D], F32) for _ in range(2)]
    junk3 = [A.alloc([128, D], BF16)] * 2
    h2t = [A.alloc([128, D], BF16) for _ in range(2)]
    s2_b = A.alloc([128, D], F32)
    b2_b = A.alloc([128, D], F32)
    xT_sb = A.alloc([128, 8, 128], F32)
    ssq3 = A.alloc([128, NT], F32)
    rstd3 = A.alloc([128, NT], F32)
    gate1_b = A.alloc([128, D], F32)
    P.dma("sp", DMA(gate1_b[:, 0:512], gsc[0][:, 0:512]), writes=[("gate", 0, 0)])
    P.dma("sp", DMA(gate1_b[:, 512:1024], gsc[0][:, 512:1024]), writes=[("gate", 0, 1)])

    P.dma("pool", DMA(wga, win_v[:, :, 3080:4104]), writes=["wga"])
    P.dma("pool", DMA(wbf, wbf_d.rearrange("(q p) n -> p q n", p=128)), writes=["wbf"])
    P.dma("pool", DMA(wgb, win_v[:, :, 4104:5128]), writes=["wgb"])
    P.dma("pool", DMA(wbs, wbs_d.rearrange("(q p) n -> p q n", p=128)), writes=["wbs"])
    P.dma("pool", DMA(wout, wout_d.rearrange("(k p) n -> p k n", p=128)), writes=["wout"])
    P.dma("sp", DMA(wr, wr_d.rearrange("(k p) n -> p k n", p=128)), writes=["wr"])
    P.dma("sp", DMA(br_b, br_row_d[0:1, :].partition_broadcast(128)), writes=["br_b"])
    P.dve(TT(wrs, wr, s2[:, :].unsqueeze(2).to_broadcast([128, 8, 20]), ALU.mult), reads=["wr", "s2"], writes=["wrs"])
    P.dve(CP(b2rep, b2[:, :].unsqueeze(2).to_broadcast([128, 8, 128])), reads=["b2"], writes=[("mg", fc) for fc in range(8)])
    for k in range(8):
        P.pe(MM(banks[4][:, 0:20], b2rep[:, k, :], wr[:, k, :], k == 0, k == 7), reads=[("mg", fc) for fc in range(8)] + ["wr"], writes=[("bank", 4)])
    P.dma("sp", DMA(b2_b, gsc[2]), writes=["b2_b"])
    P.dma("sp", DMA(s2_b, gsc[3]), writes=["s2_b"])
    P.dve(TT(rbias, banks[4][:, 0:20], br_b, ALU.add), reads=[("bank", 4), "br_b"], writes=["rbias"])

    lg_all = A.alloc([128, NT, 20], F32)
    rbig = mg.rearrange("p a b -> p (a b)").bitcast(F32).rearrange("p (a b) -> p a b", b=64)

    def router_all():
        B_ = NT
        r = rbig
        X_ = mybir.AxisListType.X
        gl = lg_all[:, :, 0:4]
        el = lg_all[:, :, 4:20].rearrange("p b (g e) -> p b g e", e=4)
        gmax, m1, m2, d12, w1, w2, ssum, gp = (r[:, :, i] for i in range(8))
        dif, sg_, ex = r[:, :, 8:12], r[:, :, 12:16], r[:, :, 16:20]
        esel, oh1, es2, oh2, tmpw = r[:, :, 20:24], r[:, :, 24:28], r[:, :, 28:32], r[:, :, 32:36], r[:, :, 36:40]
        prod = r[:, :, 44:60].rearrange("p b (g e) -> p b g e", e=4)
        lk = [("lg", tb) for tb in range(NT)]

        def bc(v):
            return v.unsqueeze(2).to_broadcast([128, B_, 4])
        K = ["rbig"] + [("mg", fc) for fc in range(8)]
        P.dve(lambda e: e.reduce_max(out=gmax, in_=gl, axis=X_), reads=lk, writes=K)
        P.dve(TT(ohg_all, gl, bc(gmax), ALU.is_equal), reads=lk + K, writes=[("ohg", tb) for tb in range(NT)])
        P.dve(TT(dif, gl, bc(gmax), ALU.subtract), reads=lk + K, writes=K)
        P.act(ACT(sg_, dif, AF.Sigmoid), reads=K, writes=K)
        P.dve(TS(ex, sg_, -1.0, 1.0, ALU.mult, ALU.add), reads=K, writes=K)
        P.dve(RECIP(ex, ex), reads=K, writes=K)
        P.dve(TT(ex, ex, sg_, ALU.mult), reads=K, writes=K)
        P.dve(lambda e: e.reduce_sum(out=ssum, in_=ex, axis=X_), reads=K, writes=K)
        P.dve(RECIP(gp, ssum), reads=K, writes=K)
        P.dve(TT(prod, el, ohg_all.unsqueeze(3).to_broadcast([128, B_, 4, 4]), ALU.mult),
              reads=lk + [("ohg", tb) for tb in range(NT)], writes=K)
        P.dve(lambda e: e.reduce_sum(out=esel, in_=prod.rearrange("p b g e -> p b e g"), axis=X_), reads=K, writes=K)
        P.dve(lambda e: e.reduce_max(out=m1, in_=esel, axis=X_), reads=K, writes=K)
        P.dve(TT(oh1, esel, bc(m1), ALU.is_equal), reads=K, writes=K)
        P.dve(STT(es2, oh1, -1.0e30, esel, ALU.mult, ALU.add), reads=K, writes=K)
        P.dve(lambda e: e.reduce_max(out=m2, in_=es2, axis=X_), reads=K, writes=K)
        P.dve(TT(oh2, es2, bc(m2), ALU.is_equal), reads=K, writes=K)
        P.dve(TT(d12, m1, m2, ALU.subtract), reads=K, writes=K)
        P.act(ACT(w1, d12, AF.Sigmoid), reads=K, writes=K)
        P.dve(TS(w2, w1, -1.0, 1.0, ALU.mult, ALU.add), reads=K, writes=K)
        P.dve(TT(w1, w1, gp, ALU.mult), reads=K, writes=K)
        P.dve(TT(w2, w2, gp, ALU.mult), reads=K, writes=K)
        P.dve(TT(tmpw, oh1, bc(w1), ALU.mult), reads=K, writes=K)
        P.dve(TT(oh2, oh2, bc(w2), ALU.mult), reads=K, writes=K)
        P.dve(TT(cw_all, tmpw, oh2, ALU.add), reads=K, writes=[("cw", tb) for tb in range(NT)])

    ot_v = ot_scr.rearrange("q p t -> p q t")

    def blk_p1(tc, q4):
        tb = tc * 4 + q4
        t2 = tb % 2
        P.dma("sp", DMA(xb3[t2], x_d[tb * 128:(tb + 1) * 128, :]), writes=[("xb3", t2)])
        for hf in range(2):
            yb_ = 4 + hf
            for fc in range(8):
                P.pe(MM(banks[yb_][:, :], mg[:, fc, q4 * 128:(q4 + 1) * 128], wout[:, fc, hf * 512:(hf + 1) * 512], fc == 0, fc == 7),
                     reads=[("mg", fc), "wout"], writes=[("bank", yb_)])
            P.dve(TT(x1b[t2][:, hf * 512:(hf + 1) * 512], banks[yb_][:, :], gate1_b[:, hf * 512:(hf + 1) * 512], ALU.mult),
                  reads=[("bank", yb_), ("gate", 0, hf)], writes=[("x1b", t2, hf)])
        xk = [("x1b", t2, 0), ("x1b", t2, 1)]
        P.pool(TT(x1b[t2], x1b[t2], xb3[t2], ALU.add), reads=xk + [("xb3", t2)], writes=xk)
        P.dma("sp", DMA(out_d[tb * 128:(tb + 1) * 128, :], x1b[t2]), reads=xk, writes=[("out", tb)])
        P.act(ACT(junk3[t2], x1b[t2], AF.Square, accum_out=ssq3[:, tb:tb + 1]), reads=xk, writes=[("ssq", 100 + tb), "junk3"])

    def blk_p2(tb):
        t2 = tb % 2
        tg = 100 + tb
        xk = [("x1b", t2, 0), ("x1b", t2, 1)]
        P.dve(TS(rstd3[:, tb:tb + 1], ssq3[:, tb:tb + 1], 1.0 / D, EPS, ALU.mult, ALU.add), reads=[("ssq", tg)], writes=[("rs", tg)])
        P.act(ACT(rstd3[:, tb:tb + 1], rstd3[:, tb:tb + 1], AF.Sqrt), reads=[("rs", tg)], writes=[("rs", tg)])
        P.dve(RECIP(rstd3[:, tb:tb + 1], rstd3[:, tb:tb + 1]), reads=[("rs", tg)], writes=[("rs", tg)])
        for k in range(8):
            bk_ = 6 + k // 4
            tv = banks[bk_][:, :].rearrange("p (a b) -> p a b", b=128)
            P.pe(TR(tv[:, k % 4, :], x1b[t2][:, k * 128:(k + 1) * 128], identf), reads=xk + ["identf"], writes=[("bank", bk_)])
        for hh in range(2):
            P.dve(CP(xT_sb[:, hh * 4:(hh + 1) * 4, :], banks[6 + hh][:, :].rearrange("p (a b) -> p a b", b=128)),
                  reads=[("bank", 6 + hh)], writes=[("xT", hh)])
        for k in range(8):
            P.pe(MM(banks[6][:, 0:20], xT_sb[:, k, :], wrs[:, k, :], k == 0, k == 7), reads=[("xT", 0), ("xT", 1), "wrs"], writes=[("bank", 6)])
        P.dve(STT(lg_all[:, tb, :], banks[6][:, 0:20], rstd3[:, tb:tb + 1], rbias, ALU.mult, ALU.add),
              reads=[("bank", 6), ("rs", tg), "rbias"], writes=[("lg", tb)])
        P.dve(STT(xb3[t2], x1b[t2], rstd3[:, tb:tb + 1], s2_b, ALU.mult, ALU.mult), reads=xk + [("rs", tg), "s2_b"], writes=[("xb3", t2)])
        P.pool(TT(h2t[t2], xb3[t2], b2_b, ALU.add), reads=[("xb3", t2), "b2_b"], writes=[("h2t", t2)])
        P.dma("sp", DMA(h2tok_d[tb * 128:(tb + 1) * 128, :], h2t[t2]), reads=[("h2t", t2)], writes=[("h2tok", tb)])

    for tc in range(NC_):
        o2 = tc % 2
        P.dma("sp", DMA(otc[o2], ot_v[:, :, tc * 512:(tc + 1) * 512]),
              reads=[("ot", gh, tc) for gh in range(16)], writes=[("otc", o2)])
        for fc in range(8):
            for side in range(2):
                wgx = wga if side == 0 else wgb
                wbx = wbf if side == 0 else wbs
                wgk = "wga" if side == 0 else "wgb"
                wbk = "wbf" if side == 0 else "wbs"
                gbk, bbk = side, 2 + side
                for k in range(8):
                    P.pe(MM(banks[gbk][:, :], wgx[:, k, fc * 128:(fc + 1) * 128], hT[:, k, tc * 512:(tc + 1) * 512], k == 0, k == 7),
                         reads=[wgk] + hkeys(tc, k), writes=[("bank", gbk)])
                P.act(ACT(sg_sb[side], banks[gbk][:, :], AF.Sigmoid), reads=[("bank", gbk)], writes=[("sg", side)])
                for q in range(4):
                    P.pe(MM(banks[bbk][:, :], wbx[:, q, fc * 128:(fc + 1) * 128], otc[o2][:, side * 4 + q, :], q == 0, q == 3),
                         reads=[wbk, ("otc", o2)], writes=[("bank", bbk)])
                P.dve(TT(m_sb[side], banks[bbk][:, :], sg_sb[side], ALU.mult), reads=[("bank", bbk), ("sg", side)], writes=[("m", side)])
            P.pool(TT(mg[:, fc, :], m_sb[0], m_sb[1], ALU.add), reads=[("m", 0), ("m", 1)], writes=[("mg", fc)])
        for q4 in range(4):
            blk_p1(tc, q4)
            if tc * 4 + q4 >= 1:
                blk_p2(tc * 4 + q4 - 1)
    blk_p2(NT - 1)
    router_all()
    if dbg:
        P.dma("sp", DMA(dbg_o["comb"], cw_all), reads=[("cw", tb) for tb in range(NT)], writes=["dbg_comb"])
    if stop_after <= 3:
        P.emit()
        return nc, P

    P.barrier()
    A.reset("pre_hT")
    U32 = mybir.dt.uint32
    rconst = A.alloc([128, 40], F32)
    SUTf = A.alloc([128, 128], F32)
    rank_sb = A.alloc([128, NT, 4], F32)
    tot_sb = A.alloc([128, NT, 4], F32)
    incl = A.alloc([128, NT, 4], F32)
    Tt = A.alloc([128, NT, 4], F32)
    posf = A.alloc([128, NT], F32)
    idx = A.alloc([128, NT], U32)
    Ng = A.alloc([128, 4], F32)
    cnt = A.alloc([128, 4], F32)
    Pg = A.alloc([128, 4], F32)
    off = A.alloc([128, 4], F32)
    endg = A.alloc([128, 4], F32)
    gsl = A.alloc([128, NS], F32)
    gk = A.alloc([128, NS], F32)
    idx1f = A.alloc([128, NS, 8], F32)
    idx1 = A.alloc([128, NS, 8], U32)
    idx2f = A.alloc([128, NS, 16], F32)
    idx2 = A.alloc([128, NS, 16], U32)
    gate2_b = A.alloc([128, D], F32)
    fg_b = A.alloc([128, D], F32)
    xrow4 = [A.alloc([128, 4, D], BF16) for _ in range(2)]
    xs = [A.alloc([128, 4, D], BF16) for _ in range(2)]
    cwS = [A.alloc([128, 4, 4], F32) for _ in range(2)]
    h2Ts = [A.alloc([128, 8, 512], BF16) for _ in range(2)]
    wgs = [A.alloc([128, 8, 512], BF16) for _ in range(2)]
    wus = [A.alloc([128, 8, 512], BF16) for _ in range(2)]
    wds = [A.alloc([128, 4, D], BF16) for _ in range(2)]
    actT = [A.alloc([128, 4, 512], BF16) for _ in range(2)]
    sgm = [A.alloc([128, 512], F32) for _ in range(2)]
    yacc = [A.alloc([128, 4, D], F32) for _ in range(2)]
    yb = [A.alloc([128, D], F32) for _ in range(2)]
    x1f = [A.alloc([128, D], F32) for _ in range(2)]
    ssq4 = A.alloc([128, NT], F32)
    rstd4 = A.alloc([128, NT], F32)
    junk4 = A.alloc([128, D], BF16)

    P.dma("sp", DMA(rconst, rc_d), writes=["rconst"])
    P.dma("sp", DMA(gate2_b, gsc[1]), writes=[("gate", 1, 0), ("gate", 1, 1)])
    P.dma("sp", DMA(fg_b, fg_row_d[0:1, :].partition_broadcast(128)), writes=["fg_b"])
    P.pool(lambda e: e.affine_select(out=SUTf, in_=onesf, pattern=[[1, 128]], compare_op=ALU.is_ge,
                                     fill=0.0, base=-1, channel_multiplier=-1), reads=["onesf"], writes=["SUTf"])
    okeys = [("ohg", tb) for tb in range(NT)]
    ohg2d = ohg_all.rearrange("p a b -> p (a b)")
    P.pe(MM(banks[0][:, 0:128], SUTf, ohg2d, True, True), reads=["SUTf"] + okeys, writes=[("bank", 0)])
    P.pe(MM(banks[1][:, 0:128], onesf, ohg2d, True, True), reads=["onesf"] + okeys, writes=[("bank", 1)])
    P.dve(CP(rank_sb.rearrange("p a b -> p (a b)"), banks[0][:, 0:128]), reads=[("bank", 0)], writes=["rank_sb"])
    P.dve(CP(tot_sb.rearrange("p a b -> p (a b)"), banks[1][:, 0:128]), reads=[("bank", 1)], writes=["tot_sb"])
    for g in range(4):
        P.dve(lambda e, g=g: e.tensor_tensor_scan(out=incl[:, :, g], data0=onesf[:, 0:NT], data1=tot_sb[:, :, g], initial=0.0,
                                                  op0=ALU.mult, op1=ALU.add), reads=["tot_sb", "onesf"], writes=[("incl", g)])
    ik = [("incl", g) for g in range(4)]
    P.dve(TT(Tt, incl, tot_sb, ALU.subtract), reads=ik + ["tot_sb"], writes=["Tt"])
    P.dve(TT(Tt, Tt, rank_sb, ALU.add), reads=["Tt", "rank_sb"], writes=["Tt"])
    P.dve(CP(Ng, incl[:, NT - 1, :]), reads=ik, writes=["Ng"])
    P.dve(MEMSET(cnt, 0.0), writes=["cnt"])
    for k in range(8):
        P.dve(STT(cnt, Ng, 512.0 * k, cnt, ALU.is_gt, ALU.add), reads=["Ng", "cnt"], writes=["cnt"])
    P.dve(TS(Pg, cnt, 512.0, None, ALU.mult), reads=["cnt"], writes=["Pg"])
    P.dve(MEMSET(off, 0.0), writes=["off"])
    for g in range(1, 4):
        P.dve(TT(off[:, g:g + 1], off[:, g - 1:g], Pg[:, g - 1:g], ALU.add), reads=["off", "Pg"], writes=["off"])
    P.dve(TT(endg, off, Pg, ALU.add), reads=["off", "Pg"], writes=["endg"])
    for g in range(4):
        P.dve(TS(Tt[:, :, g], Tt[:, :, g], off[:, g:g + 1], None, ALU.add), reads=["Tt", "off"], writes=["Tt"])
    P.dve(TT(Tt, Tt, ohg_all, ALU.mult), reads=["Tt"] + okeys, writes=["Tt"])
    P.dve(lambda e: e.reduce_sum(out=posf, in_=Tt, axis=mybir.AxisListType.X), reads=["Tt"], writes=["posf"])
    P.dve(CP(idx, posf), reads=["posf"], writes=["idx"])
    P.dve(MEMSET(gsl, 0.0), writes=["gsl"])
    for g in range(4):
        P.dve(STT(gsl, rconst[:, 24:36], endg[:, g:g + 1], gsl, ALU.is_ge, ALU.add), reads=["rconst", "endg", "gsl"], writes=["gsl"])
    P.dve(TS(gsl, gsl, 3.0, None, ALU.min), reads=["gsl"], writes=["gsl"])
    P.dve(TS(gk, gsl, 1024.0, None, ALU.mult), reads=["gsl"], writes=["gk"])
    P.dve(TT(idx1f, rconst[:, 0:8].unsqueeze(1).to_broadcast([128, NS, 8]), gk[:, :].unsqueeze(2).to_broadcast([128, NS, 8]), ALU.add),
          reads=["rconst", "gk"], writes=["idx1f"])
    P.dve(CP(idx1, idx1f), reads=["idx1f"], writes=["idx1"])
    P.dve(TS(gk, gsl, 2048.0, None, ALU.mult), reads=["gsl", "idx1f"], writes=["gk"])
    P.dve(TT(idx2f, rconst[:, 8:24].unsqueeze(1).to_broadcast([128, NS, 16]), gk[:, :].unsqueeze(2).to_broadcast([128, NS, 16]), ALU.add),
          reads=["rconst", "gk"], writes=["idx2f"])
    P.dve(CP(idx2, idx2f), reads=["idx2f"], writes=["idx2"])

    IOA = bass.IndirectOffsetOnAxis
    for g4 in range(NT // 4):
        r2 = g4 % 2
        P.dma("sp", DMA(xrow4[r2], h2tok_d[g4 * 512:(g4 + 1) * 512, :].rearrange("(a p) n -> p a n", p=128)), writes=[("xrow", r2)])
        for a_ in range(4):
            tb = g4 * 4 + a_
            P.dma("pool", lambda e, tb=tb, r2=r2, a_=a_: e.indirect_dma_start(out=h2s_d[:, :], out_offset=IOA(ap=idx[:, tb:tb + 1], axis=0),
                                                                              in_=xrow4[r2][:, a_, :], in_offset=None),
                  reads=[("xrow", r2), "idx"], writes=[("h2s_sc", tb)])
            P.dma("pool", lambda e, tb=tb: e.indirect_dma_start(out=cws_d[:, :], out_offset=IOA(ap=idx[:, tb:tb + 1], axis=0),
                                                                in_=cw_all[:, tb, :], in_offset=None),
                  reads=[("cw", tb), "idx"], writes=[("cws_sc", tb)])
    sc_all = [("h2s_sc", tb) for tb in range(NT)]
    cw_sc_all = [("cws_sc", tb) for tb in range(NT)]

    def slot_prep(sl):
        s2_ = sl % 2
        P.dma("sp", DMA(xs[s2_], h2s_d[sl * 512:(sl + 1) * 512, :].rearrange("(a p) n -> p a n", p=128)),
              reads=sc_all, writes=[("xs", s2_)])
        P.dma("sp", DMA(cwS[s2_], cws_d[sl * 512:(sl + 1) * 512, :].rearrange("(a p) e -> p a e", p=128)),
              reads=cw_sc_all, writes=[("cwS", s2_)])
        for a_ in range(4):
            tbk = 6 + (a_ % 2)
            pT = bank_bf(tbk)
            for k in range(8):
                P.pe(TR(pT[:, k * 128:(k + 1) * 128], xs[s2_][:, a_, k:D:8], identb), reads=[("xs", s2_), "identb"], writes=[("bank", tbk)])
            P.act(ACT(h2Ts[s2_][:, :, a_ * 128:(a_ + 1) * 128], pT.rearrange("p (k t) -> p k t", t=128), AF.Identity),
                  reads=[("bank", tbk)], writes=[("h2Ts", s2_, a_)])

    slot_prep(0)
    for sl in range(NS):
        s2_ = sl % 2
        hk = [("h2Ts", s2_, a_) for a_ in range(4)]
        for el in range(4):
            it = sl * 4 + el
            wb = it % 2
            for hf in range(2):
                P.dma("pool", lambda e, wb=wb, hf=hf, sl=sl, el=el: e.indirect_dma_start(
                    out=wgs[wb].rearrange("p k n -> p (k n)")[:, hf * 2048:(hf + 1) * 2048], out_offset=None, in_=wg2b_d[:, :],
                    in_offset=IOA(ap=idx1[:, sl, el * 2 + hf:el * 2 + hf + 1], axis=0)), reads=["idx1"], writes=[("wgs", wb, hf)])
                P.dma("pool", lambda e, wb=wb, hf=hf, sl=sl, el=el: e.indirect_dma_start(
                    out=wus[wb].rearrange("p k n -> p (k n)")[:, hf * 2048:(hf + 1) * 2048], out_offset=None, in_=wu2b_d[:, :],
                    in_offset=IOA(ap=idx1[:, sl, el * 2 + hf:el * 2 + hf + 1], axis=0)), reads=["idx1"], writes=[("wus", wb, hf)])
            for q in range(4):
                P.dma("pool", lambda e, wb=wb, q=q, sl=sl, el=el: e.indirect_dma_start(
                    out=wds[wb][:, q, :], out_offset=None, in_=wd2b_d[:, :],
                    in_offset=IOA(ap=idx2[:, sl, el * 4 + q:el * 4 + q + 1], axis=0)), reads=["idx2"], writes=[("wds", wb, q)])
            wdk = [("wds", wb, q) for q in range(4)]
            if el == 2 and sl + 1 < NS:
                slot_prep(sl + 1)
            a2 = it % 2
            for ffc in range(4):
                gb_, ub_ = ffc % 2, 2 + (ffc % 2)
                for k in range(8):
                    P.pe(MM(banks[gb_][:, :], wgs[wb][:, k, ffc * 128:(ffc + 1) * 128], h2Ts[s2_][:, k, :], k == 0, k == 7),
                         reads=[("wgs", wb, 0), ("wgs", wb, 1)] + hk, writes=[("bank", gb_)])
                for k in range(8):
                    P.pe(MM(banks[ub_][:, :], wus[wb][:, k, ffc * 128:(ffc + 1) * 128], h2Ts[s2_][:, k, :], k == 0, k == 7),
                         reads=[("wus", wb, 0), ("wus", wb, 1)] + hk, writes=[("bank", ub_)])
                P.act(ACT(sgm[ffc % 2], banks[gb_][:, :], AF.Silu), reads=[("bank", gb_)], writes=[("sgm", ffc % 2)])
                P.dve(TT(actT[a2][:, ffc, :], banks[ub_][:, :], sgm[ffc % 2], ALU.mult),
                      reads=[("bank", ub_), ("sgm", ffc % 2)], writes=[("actT", a2, ffc)])
            for a_ in range(4):
                for hf in range(2):
                    ybk = 4 + ((a_ * 2 + hf) % 2)
                    for ffc in range(4):
                        P.pe(MM(banks[ybk][:, :], actT[a2][:, ffc, a_ * 128:(a_ + 1) * 128], wds[wb][:, ffc, hf * 512:(hf + 1) * 512], ffc == 0, ffc == 3),
                             reads=[("actT", a2, f) for f in range(4)] + wdk, writes=[("bank", ybk)])
                    yv = yacc[s2_][:, a_, hf * 512:(hf + 1) * 512]
                    cwv = cwS[s2_][:, a_, el:el + 1]
                    if el == 0:
                        P.dve(TS(yv, banks[ybk][:, :], cwv, None, ALU.mult), reads=[("bank", ybk), ("cwS", s2_)], writes=[("yacc", s2_, a_, hf)])
                    else:
                        P.dve(STT(yv, banks[ybk][:, :], cwv, yv, ALU.mult, ALU.add),
                              reads=[("bank", ybk), ("cwS", s2_), ("yacc", s2_, a_, hf)], writes=[("yacc", s2_, a_, hf)])
        P.dma("sp", DMA(ys_d[sl * 512:(sl + 1) * 512, :].rearrange("(a p) n -> p a n", p=128), yacc[s2_]),
              reads=[("yacc", s2_, a_, hf) for a_ in range(4) for hf in range(2)], writes=[("ys", sl)])
    ys_all = [("ys", sl) for sl in range(NS)]
    for tb in range(NT):
        t2 = tb % 2
        P.dma("pool", lambda e, tb=tb, t2=t2: e.indirect_dma_start(out=yb[t2], out_offset=None, in_=ys_d[:, :],
                                                                   in_offset=IOA(ap=idx[:, tb:tb + 1], axis=0)),
              reads=["idx"] + ys_all, writes=[("yb", t2)])
        P.dma("sp", DMA(x1f[t2], out_d[tb * 128:(tb + 1) * 128, :]), writes=[("x1f", t2)])
        P.dve(TT(yb[t2], yb[t2], gate2_b, ALU.mult), reads=[("yb", t2), ("gate", 1, 0), ("gate", 1, 1)], writes=[("yb", t2)])
        P.dve(TT(x1f[t2], x1f[t2], yb[t2], ALU.add), reads=[("x1f", t2), ("yb", t2)], writes=[("x1f", t2)])
        P.act(ACT(junk4, x1f[t2], AF.Square, accum_out=ssq4[:, tb:tb + 1]), reads=[("x1f", t2)], writes=[("ssq4", tb), "junk4"])
        P.dve(TS(rstd4[:, tb:tb + 1], ssq4[:, tb:tb + 1], 1.0 / D, EPS, ALU.mult, ALU.add), reads=[("ssq4", tb)], writes=[("rs4", tb)])
        P.act(ACT(rstd4[:, tb:tb + 1], rstd4[:, tb:tb + 1], AF.Sqrt), reads=[("rs4", tb)], writes=[("rs4", tb)])
        P.dve(RECIP(rstd4[:, tb:tb + 1], rstd4[:, tb:tb + 1]), reads=[("rs4", tb)], writes=[("rs4", tb)])
        P.dve(STT(x1f[t2], x1f[t2], rstd4[:, tb:tb + 1], fg_b, ALU.mult, ALU.mult), reads=[("x1f", t2), ("rs4", tb), "fg_b"], writes=[("x1f", t2)])
        P.dma("sp", DMA(out_d[tb * 128:(tb + 1) * 128, :], x1f[t2]), reads=[("x1f", t2)], writes=[("out", tb)])
    P.emit()
    return nc, P


def _rconst():
    c = np.zeros((128, 40), np.float32)
    p = np.arange(128, dtype=np.float32)
    for el in range(4):
        for hf in range(2):
            c[:, el * 2 + hf] = 2 * p + 256 * el + hf
        for q in range(4):
            c[:, 8 + el * 4 + q] = p + 512 * el + 128 * q
    for sl in range(12):
        c[:, 24 + sl] = 512.0 * sl
    return c


def make_in_maps(inp):
    f = lambda a: np.ascontiguousarray(np.asarray(a, dtype=np.float32))
    B = inp["x"].shape[0]
    ada_b = f(inp["ada_b"][0])
    shared = {
        "ada_w": f(inp["ada_w"][0]),
        "ada_bT": f(ada_b.reshape(48, 128).T),
        "ada_b_row": f(ada_b.reshape(1, -1)),
        "n1g": f(inp["norm1_g"][0].reshape(8, 128).T),
        "n2g": f(inp["norm2_g"][0].reshape(8, 128).T),
        "fg_row": f(inp["final_g"].reshape(1, -1)),
        "w_in": f(inp["w_in"][0]),
        "b_forget": f(inp["b_forget"][0].reshape(8, 1)),
        "w_bf": f(inp["w_branch_fox"][0]),
        "w_bs": f(inp["w_branch_sb"][0]),
        "w_out": f(inp["w_out"][0]),
        "w_r": f(np.concatenate([inp["router_group_w"][0], inp["router_expert_w"][0]], axis=1)),
        "b_r_row": f(np.concatenate([inp["router_group_b"][0], inp["router_expert_b"][0]], axis=0).reshape(1, 20)),
        "e_wg": f(inp["expert_w_gate"][0]).reshape(16 * 256, 2048),
        "e_wu": f(inp["expert_w_up"][0]).reshape(16 * 256, 2048),
        "e_wd": f(inp["expert_w_down"][0]).reshape(16 * 512, 1024),
        "n2g_row": f(inp["norm2_g"][0].reshape(1, -1)),
        "rconst": _rconst(),
    }
    maps = []
    for b in range(B):
        m = dict(shared)
        m["x"] = f(inp["x"][b])
        m["cT"] = f(np.asarray(inp["c"][b]).reshape(8, 128).T)
        maps.append(m)
    return maps


_CACHE = {}


def kernel(**inputs):
    if "nc" not in _CACHE:
        _CACHE["nc"] = build_program()[0]
    nc = _CACHE["nc"]
    in_maps = make_in_maps(inputs)
    res = run_bass_kernel_spmd(nc, in_maps, core_ids=list(range(len(in_maps))))
    return np.stack([np.asarray(r["out"], dtype=np.float32) for r in res.results], axis=0)
```

```python
import contextlib
import numpy as np
import concourse.bass as bass
import concourse.mybir as mybir
from concourse.bass_utils import run_bass_kernel_spmd

F32 = mybir.dt.float32
BF16 = mybir.dt.bfloat16
U8 = mybir.dt.uint8
AF = mybir.ActivationFunctionType
ALU = mybir.AluOpType

S = 4096
D = 1024
NT = S // 128
NC_ = S // 512
NIN = 5128
NEG = -30000.0
EPS = 1e-6


class Prog:
    ENGS = ("pe", "act", "dve", "pool", "sp")

    def __init__(self, nc, n_dma_sems=12):
        self.nc = nc
        self.ops = []
        self.n_dma_sems = n_dma_sems

    def add(self, eng, fn, reads=(), writes=(), dma=False, barrier=False):
        self.ops.append(dict(eng=eng, fn=fn, reads=tuple(reads), writes=tuple(writes), dma=dma, barrier=barrier))

    def pe(self, fn, reads=(), writes=()): self.add("pe", fn, reads, writes)
    def act(self, fn, reads=(), writes=()): self.add("act", fn, reads, writes)
    def dve(self, fn, reads=(), writes=()): self.add("dve", fn, reads, writes)
    def pool(self, fn, reads=(), writes=()): self.add("pool", fn, reads, writes)
    def dma(self, eng, fn, reads=(), writes=()): self.add(eng, fn, reads, writes, dma=True)
    def barrier(self): self.add(None, None, barrier=True)

    def build(self):
        ops = self.ops
        n = len(ops)
        last_w, readers = {}, {}
        deps = [set() for _ in range(n)]
        last_on_eng = {}
        dma_since = []
        bar_deps = None
        for i, o in enumerate(ops):
            if o["barrier"]:
                bar_deps = set(last_on_eng.values()) | set(dma_since)
                last_w, readers = {}, {}
                continue
            if bar_deps is not None:
                pass
            for k in o["reads"]:
                if k in last_w:
                    deps[i].add((last_w[k], False))
                if isinstance(k, tuple) and k[0] == "bank":
                    for r in readers.get(k, ()):
                        if ops[r]["eng"] != o["eng"]:
                            deps[i].add((r, False))
            for k in o["writes"]:
                if k in last_w:
                    deps[i].add((last_w[k], False))
                for r in readers.get(k, ()):
                    if r != i:
                        deps[i].add((r, True))
            for k in o["reads"]:
                readers.setdefault(k, []).append(i)
            for k in o["writes"]:
                last_w[k] = i
                readers[k] = []
            o["bar"] = bar_deps
            if o["dma"]:
                dma_since.append(i)
            else:
                last_on_eng[o["eng"]] = i
        fdeps = [set() for _ in range(n)]
        first_after_bar = {}
        for i, o in enumerate(ops):
            if o["barrier"]:
                continue
            for d, war in deps[i]:
                p = ops[d]
                if (not p["dma"]) and (not o["dma"]) and p["eng"] == o["eng"]:
                    if o["eng"] == "pe":
                        continue
                fdeps[i].add(d)
            bd = o.get("bar")
            if bd is not None and first_after_bar.get(o["eng"]) is not bd:
                first_after_bar[o["eng"]] = bd
                for d in bd:
                    if d != i and not (ops[d]["eng"] == o["eng"] and not ops[d]["dma"] and not o["dma"]):
                        fdeps[i].add(d)
        needs_inc = [False] * n
        for i in range(n):
            for d in fdeps[i]:
                needs_inc[d] = True
        eng_count = {e: 0 for e in self.ENGS}
        ticket = [None] * n
        dma_rr = {e: 0 for e in self.ENGS}
        dma_tot = {}
        pre_wait = [None] * n
        for i, o in enumerate(ops):
            if o["barrier"]:
                continue
            if o["dma"]:
                e = o["eng"]
                s = (e, dma_rr[e] % self.n_dma_sems)
                dma_rr[e] += 1
                prev = dma_tot.get(s, 0)
                if prev > 0:
                    pre_wait[i] = (s, prev)
                dma_tot[s] = prev + 16
                ticket[i] = (s, prev + 16)
            elif needs_inc[i]:
                eng_count[o["eng"]] += 1
                ticket[i] = (o["eng"], eng_count[o["eng"]])
        self.final_dma = dict(dma_tot)
        progs = {e: [] for e in self.ENGS}
        waited = {e: {} for e in self.ENGS}
        for i, o in enumerate(ops):
            if o["barrier"]:
                continue
            e = o["eng"]
            cand = [ticket[d] for d in fdeps[i]]
            if pre_wait[i] is not None:
                cand.append(pre_wait[i])
            best = {}
            for (s, v) in cand:
                if v > best.get(s, 0):
                    best[s] = v
            ws = []
            for s, v in best.items():
                if waited[e].get(s, 0) >= v:
                    continue
                waited[e][s] = v
                ws.append((s, v))
            progs[e].append((ws, o["fn"], ticket[i], o["dma"]))
        self.stats = {e: len(progs[e]) for e in self.ENGS}
        return progs

    def emit(self):
        nc = self.nc
        progs = self.build()
        semkeys = set()
        for e in self.ENGS:
            for ws, fn, t, isdma in progs[e]:
                for s, v in ws:
                    semkeys.add(s)
                if t is not None:
                    semkeys.add(t[0])
        semkeys = sorted(semkeys, key=str)
        with contextlib.ExitStack() as st:
            sems = {}
            for k in semkeys:
                nm = "s_" + (k if isinstance(k, str) else f"{k[0]}{k[1]}")
                sems[k] = st.enter_context(nc.semaphore(nm))
            block = st.enter_context(nc.Block())
            final = self.final_dma

            def make(e):
                def body(eng):
                    for ws, fn, t, isdma in progs[e]:
                        for s, v in ws:
                            eng.wait_ge(sems[s], v)
                        ins = fn(eng)
                        if t is not None:
                            ins.then_inc(sems[t[0]], 16 if isdma else 1)
                    if e == "sp":
                        for s, v in final.items():
                            eng.wait_ge(sems[s], v)
                return body
            block.tensor(make("pe"))
            block.scalar(make("act"))
            block.vector(make("dve"))
            block.gpsimd(make("pool"))
            block.sync(make("sp"))


class Arena:
    def __init__(self, nc, nbytes):
        self.t = nc.alloc_sbuf_tensor("arena", [128, nbytes], U8)
        self.nbytes = nbytes
        self.off = 0
        self.top = nbytes
        self.marks = {}

    def mark(self, name):
        self.marks[name] = self.off

    def reset(self, name):
        self.off = self.marks[name]

    def alloc_top(self, shape, dt):
        esz = 2 if dt == BF16 else 4
        n = int(np.prod(shape[1:]))
        nb = (n * esz + 31) // 32 * 32
        self.top -= nb
        v = self.t[0:shape[0], self.top:self.top + n * esz].bitcast(dt)
        if len(shape) == 3:
            v = v.rearrange("p (a b) -> p a b", b=shape[2])
        return v

    def alloc(self, shape, dt):
        esz = 2 if dt == BF16 else 4
        n = int(np.prod(shape[1:]))
        nb = (n * esz + 31) // 32 * 32
        assert self.off + nb <= self.top, (self.off, nb, self.top)
        v = self.t[0:shape[0], self.off:self.off + n * esz].bitcast(dt)
        self.off += nb
        if len(shape) == 3:
            v = v.rearrange("p (a b) -> p a b", b=shape[2])
        elif len(shape) == 4:
            v = v.rearrange("p (a b c) -> p a b c", b=shape[2], c=shape[3])
        return v


def MM(out, lhsT, rhs, start, stop):
    return lambda e: e.matmul(out, lhsT, rhs, start=start, stop=stop)


def TR(out, in_, ident):
    return lambda e: e.transpose(out, in_, ident)


def ACT(out, in_, func, **kw):
    return lambda e: e.activation(out=out, in_=in_, func=func, **kw)


def TT(out, in0, in1, op):
    return lambda e: e.tensor_tensor(out=out, in0=in0, in1=in1, op=op)


def TS(out, in0, s1, s2, op0, op1=None):
    if op1 is None:
        return lambda e: e.tensor_scalar(out=out, in0=in0, scalar1=s1, scalar2=None, op0=op0)
    return lambda e: e.tensor_scalar(out=out, in0=in0, scalar1=s1, scalar2=s2, op0=op0, op1=op1)


def STT(out, in0, scalar, in1, op0, op1):
    return lambda e: e.scalar_tensor_tensor(out=out, in0=in0, scalar=scalar, in1=in1, op0=op0, op1=op1)


def CP(out, in_):
    return lambda e: e.tensor_copy(out=out, in_=in_)


def DMA(out, in_):
    return lambda e: e.dma_start(out=out, in_=in_)


def MEMSET(ap, v):
    return lambda e: e.memset(ap, v)


def RECIP(out, in_):
    return lambda e: e.reciprocal(out=out, in_=in_)


def build_program(stop_after=99, dbg=False):
    nc = bass.Bass("TRN2", target_bir_lowering=False)

    def din(name, shape, dt=F32):
        return nc.dram_tensor(name, shape, dt, kind="ExternalInput").ap()

    x_d = din("x", [S, D])
    cT_d = din("cT", [128, 8])
    adaw_d = din("ada_w", [D, 6 * D])
    adabT_d = din("ada_bT", [128, 48])
    adab_row_d = din("ada_b_row", [1, 6 * D])
    n1g_d = din("n1g", [128, 8])
    n2g_d = din("n2g", [128, 8])
    fg_row_d = din("fg_row", [1, D])
    win_d = din("w_in", [D, NIN])
    bfg_d = din("b_forget", [8, 1])
    wbf_d = din("w_bf", [512, D])
    wbs_d = din("w_bs", [512, D])
    wout_d = din("w_out", [D, D])
    wr_d = din("w_r", [D, 20])
    br_row_d = din("b_r_row", [1, 20])
    wg2_d = din("e_wg", [16 * 256, 2048])
    wu2_d = din("e_wu", [16 * 256, 2048])
    wd2_d = din("e_wd", [16 * 512, D])
    n2g_row_d = din("n2g_row", [1, D])
    rc_d = din("rconst", [128, 40])
    out_d = nc.dram_tensor("out", [S, D], F32, kind="ExternalOutput").ap()
    ot_scr = nc.dram_tensor("ot_scr", [8, 128, S], BF16, kind="ExternalOutput" if dbg else "Internal").ap()
    fsc = nc.dram_tensor("fsc", [8, 12, S], BF16, kind="Internal").ap()
    gsc = nc.dram_tensor("gsc", [4, 128, D], F32, kind="Internal").ap()
    NS = 12
    h2tok_d = nc.dram_tensor("h2tok", [S, D], BF16, kind="Internal").ap()
    h2s_d = nc.dram_tensor("h2s", [NS * 512, D], BF16, kind="Internal").ap()
    cws_d = nc.dram_tensor("cws", [NS * 512, 4], F32, kind="Internal").ap()
    ys_d = nc.dram_tensor("ys", [NS * 512, D], F32, kind="Internal").ap()
    wg2b_d = nc.dram_tensor("wg2b", [16 * 256, 2048], BF16, kind="Internal").ap()
    wu2b_d = nc.dram_tensor("wu2b", [16 * 256, 2048], BF16, kind="Internal").ap()
    wd2b_d = nc.dram_tensor("wd2b", [16 * 512, D], BF16, kind="Internal").ap()
    dbg_o = {}
    if dbg:
        dbg_o["hT"] = nc.dram_tensor("dbg_hT", [128, 8, S], BF16, kind="ExternalOutput").ap()
        dbg_o["mod"] = nc.dram_tensor("dbg_mod", [128, 48], F32, kind="ExternalOutput").ap()
        dbg_o["g1"] = nc.dram_tensor("dbg_g1", [128, D], F32, kind="ExternalOutput").ap()
        dbg_o["comb"] = nc.dram_tensor("dbg_comb", [128, NT, 4], F32, kind="ExternalOutput").ap()
        dbg_o["h2T"] = nc.dram_tensor("dbg_h2T", [128, 8, S], BF16, kind="ExternalOutput").ap()

    win_v = win_d.rearrange("(k p) n -> p k n", p=128)
    adaw_v = adaw_d.rearrange("(k p) n -> p k n", p=128)

    P = Prog(nc)
    A = Arena(nc, 206 * 1024)
    banks = [nc.alloc_psum_tensor(f"bank{i}", [128, 512], F32) for i in range(8)]

    def bank_bf(i):
        return banks[i][:, :].bitcast(BF16)

    ohg_all = A.alloc([128, NT, 4], F32)
    cw_all = A.alloc([128, NT, 4], F32)
    s1 = A.alloc([128, 8], F32)
    b1 = A.alloc([128, 8], F32)
    s2 = A.alloc([128, 8], F32)
    b2 = A.alloc([128, 8], F32)
    n1g = A.alloc([128, 8], F32)
    n2g = A.alloc([128, 8], F32)
    modT = A.alloc([128, 48], F32)
    small = A.alloc([128, 64], F32)
    identb = A.alloc([128, 128], BF16)
    identf = A.alloc([128, 128], F32)
    onesf = A.alloc([128, 128], F32)
    A.mark("pre_hT")
    hT = A.alloc([128, 8, S], BF16)
    A.mark("phase")
    wq = A.alloc_top([128, 8, 576], BF16)
    wk = A.alloc_top([128, 8, 576], BF16)
    wv = A.alloc_top([128, 8, 512], BF16)
    wfa = A.alloc_top([128, 8, 8], BF16)

    P.pool(MEMSET(onesf, 1.0), writes=["onesf"])
    P.pool(lambda e: e.affine_select(out=identf, in_=onesf, pattern=[[-1, 128]], compare_op=ALU.is_equal,
                                     fill=0.0, base=0, channel_multiplier=1), reads=["onesf"], writes=["identf"])
    P.pool(CP(identb, identf), reads=["identf"], writes=["identb"])
    P.pool(MEMSET(modT, 0.0), writes=["modT_a", "modT_b"])
    P.dma("sp", DMA(n1g, n1g_d), writes=["n1g"])
    P.dma("sp", DMA(n2g, n2g_d), writes=["n2g"])

    gate1_b = A.alloc([128, D], F32)
    gate2_b = A.alloc([128, D], F32)
    cT = A.alloc([128, 8], F32)
    c_bf = A.alloc([128, 8], BF16)
    c_rep = A.alloc([128, 8, 128], BF16)
    adabT = A.alloc([128, 48], F32)
    adab_g = A.alloc([128, 4, D], F32)
    n2g_b = A.alloc([128, D], F32)
    sb2_b = [A.alloc([128, D], F32) for _ in range(2)]
    adaw_sb = [A.alloc([128, 8, 512], BF16) for _ in range(3)]
    P.dma("sp", DMA(cT, cT_d), writes=["cT"])
    P.dma("sp", DMA(adabT, adabT_d), writes=["adabT"])
    P.dma("sp", DMA(adab_g[:, 0, :], adab_row_d[0:1, 2 * D:3 * D].partition_broadcast(128)), writes=["adab_g0"])
    P.dma("sp", DMA(adab_g[:, 1, :], adab_row_d[0:1, 5 * D:6 * D].partition_broadcast(128)), writes=["adab_g1"])
    P.dma("sp", DMA(adab_g[:, 2, :], adab_row_d[0:1, 3 * D:4 * D].partition_broadcast(128)), writes=["adab_g2"])
    P.dma("sp", DMA(adab_g[:, 3, :], adab_row_d[0:1, 4 * D:5 * D].partition_broadcast(128)), writes=["adab_g3"])
    P.dma("sp", DMA(n2g_b, n2g_row_d[0:1, :].partition_broadcast(128)), writes=["n2g_b"])
    P.act(ACT(c_bf, cT, AF.Silu), reads=["cT"], writes=["c_bf"])
    P.dve(CP(c_rep, c_bf[:, :].unsqueeze(2).to_broadcast([128, 8, 128])), reads=["c_bf"], writes=["c_rep"])
    order = [0, 1, 2, 3, 6, 7, 8, 9, 4, 5, 10, 11]
    for n_, q in enumerate(order):
        buf = adaw_sb[n_ % 3]
        bk = ("adaw", n_ % 3)
        P.dma("pool", DMA(buf, adaw_v[:, :, q * 512:(q + 1) * 512]), writes=[bk])
        v, half = q // 2, q % 2
        if v in (2, 5):
            gi = 0 if v == 2 else 1
            bnk = banks[gi * 2 + half]
            for k in range(8):
                P.pe(MM(bnk[:, :], c_rep[:, k, :], buf[:, k, :], k == 0, k == 7), reads=[bk, "c_rep"], writes=[("bank", gi * 2 + half)])
            dst = (gate1_b if gi == 0 else gate2_b)[:, half * 512:(half + 1) * 512]
            P.dve(TT(dst, bnk[:, :], adab_g[:, gi, half * 512:(half + 1) * 512], ALU.add),
                  reads=[("bank", gi * 2 + half), f"adab_g{gi}"], writes=[("gate", gi, half)])
            if half == 1:
                P.dma("sp", DMA(gsc[gi], gate1_b if gi == 0 else gate2_b), reads=[("gate", gi, 0), ("gate", gi, 1)], writes=[("gsc", gi)])
        else:
            if v in (3, 4):
                gi = v - 1
                bi = (v - 3) * 2 + half
                for k in range(8):
                    P.pe(MM(banks[bi][:, :], c_rep[:, k, :], buf[:, k, :], k == 0, k == 7), reads=[bk, "c_rep"], writes=[("bank", bi)])
                dst = sb2_b[v - 3][:, half * 512:(half + 1) * 512]
                P.dve(TT(dst, banks[bi][:, :], adab_g[:, gi, half * 512:(half + 1) * 512], ALU.add),
                      reads=[("bank", bi), f"adab_g{gi}"], writes=[("sb2", v - 3, half)])
                if v == 4:
                    P.dve(STT(dst, dst, 1.0, n2g_b[:, half * 512:(half + 1) * 512], ALU.add, ALU.mult),
                          reads=[("sb2", 1, half), "n2g_b"], writes=[("sb2", 1, half)])
                if half == 1:
                    P.dma("sp", DMA(gsc[2 + v - 3], sb2_b[v - 3]), reads=[("sb2", v - 3, 0), ("sb2", v - 3, 1)], writes=[("gsc", 2 + v - 3)])
            for cc in range(4):
                j = v * 8 + half * 4 + cc
                for k in range(8):
                    mb = 4 if v < 2 else 5
                    P.pe(MM(banks[mb][:, j:j + 1], buf[:, k, cc * 128:(cc + 1) * 128], c_bf[:, k:k + 1], k == 0, k == 7),
                         reads=[bk, "c_bf"], writes=[("bank", mb)])
        if n_ == 3:
            P.dve(TT(modT[:, 0:16], banks[4][:, 0:16], adabT[:, 0:16], ALU.add),
                  reads=[("bank", 4), "adabT"], writes=["modT_a"])
            P.dve(STT(s1, modT[:, 8:16], 1.0, n1g, ALU.add, ALU.mult), reads=["modT_a", "n1g"], writes=["s1"])
            P.dve(CP(b1, modT[:, 0:8]), reads=["modT_a"], writes=["b1"])
        if n_ == 7:
            P.dve(TT(modT[:, 24:40], banks[5][:, 24:40], adabT[:, 24:40], ALU.add),
                  reads=[("bank", 5), "adabT"], writes=["modT_b"])
            P.dve(STT(s2, modT[:, 32:40], 1.0, n2g, ALU.add, ALU.mult), reads=["modT_b", "n2g"], writes=["s2"])
            P.dve(CP(b2, modT[:, 24:32]), reads=["modT_b"], writes=["b2"])
    if dbg:
        P.dma("sp", DMA(dbg_o["mod"], modT), reads=["modT_a", "modT_b"], writes=["dbg_mod"])
        P.dma("sp", DMA(dbg_o["g1"], gate1_b), reads=[("gate", 0, 0), ("gate", 0, 1)], writes=["dbg_g1"])

    if stop_after <= 0:
        P.emit()
        return nc, P
    def hkeys(tc, k=None):
        ks = range(8) if k is None else [k]
        return [("hT", tb, kk) for tb in range(tc * 4, tc * 4 + 4) for kk in ks]

    def norm_block(xb, xkey, ssq, rstd, junk, xn, xnkey, tb_tag, xkey2=None):
        xk = [xkey] if xkey2 is None else [xkey, xkey2]
        P.act(ACT(junk, xb, AF.Square, accum_out=ssq), reads=xk, writes=[("ssq", tb_tag), ("junk", tb_tag % 2)])
        P.dve(TS(rstd, ssq, 1.0 / D, EPS, ALU.mult, ALU.add), reads=[("ssq", tb_tag)], writes=[("rs", tb_tag)])
        P.act(ACT(rstd, rstd, AF.Sqrt), reads=[("rs", tb_tag)], writes=[("rs", tb_tag)])
        P.dve(RECIP(rstd, rstd), reads=[("rs", tb_tag)], writes=[("rs", tb_tag)])
        P.dve(TS(xn, xb, rstd, None, ALU.mult), reads=xk + [("rs", tb_tag)], writes=[xnkey])

    def load_branch_weights(br):
        base = 0 if br == 0 else 1544
        P.dma("pool", DMA(wq, win_v[:, :, base:base + 576]), writes=["wq"])
        P.dma("pool", DMA(wk, win_v[:, :, base + 512:base + 1088]), writes=["wk"])
        P.dma("pool", DMA(wv, win_v[:, :, base + 1024:base + 1536]), writes=["wv"])

    load_branch_weights(0)
    P.dma("pool", DMA(wfa, win_v[:, :, 1536:1544]), writes=["wfa"])
    xbuf = [A.alloc([128, D], F32) for _ in range(2)]
    junkb = [A.alloc([128, D], BF16) for _ in range(2)]
    xnb = [A.alloc([128, D], BF16) for _ in range(2)]
    ssq_t = A.alloc([128, NT], F32)
    rstd_t = A.alloc([128, NT], F32)
    import os as _os
    for tb in range(int(_os.environ.get('S1_N', NT))):
        xb = xbuf[tb % 2]
        P.dma("sp", DMA(xb, x_d[tb * 128:(tb + 1) * 128, :]), writes=[("xb", tb % 2)])
        norm_block(xb, ("xb", tb % 2), ssq_t[:, tb:tb + 1], rstd_t[:, tb:tb + 1], junkb[tb % 2], xnb[tb % 2], ("xn", tb % 2), tb)
        ba, bd = 4 + 2 * (tb % 2), 5 + 2 * (tb % 2)
        for k in range(8):
            bk_ = ba if k % 2 == 0 else bd
            P.pe(TR(bank_bf(bk_)[:, (k // 2) * 128:(k // 2 + 1) * 128], xnb[tb % 2][:, k * 128:(k + 1) * 128], identb),
                 reads=[("xn", tb % 2), "identb"], writes=[("bank", bk_)])
        for k in range(8):
            dst = hT[:, k, tb * 128:(tb + 1) * 128]
            bk_ = ba if k % 2 == 0 else bd
            src = bank_bf(bk_)[:, (k // 2) * 128:(k // 2 + 1) * 128]
            if k % 2 == 0:
                P.act(ACT(dst, src, AF.Identity, scale=s1[:, k:k + 1], bias=b1[:, k:k + 1]),
                      reads=[("bank", bk_), "s1", "b1"], writes=[("hT", tb, k)])
            else:
                P.dve(TS(dst, src, s1[:, k:k + 1], b1[:, k:k + 1], ALU.mult, ALU.add),
                      reads=[("bank", bk_), "s1", "b1"], writes=[("hT", tb, k)])
    if dbg:
        for k in range(8):
            P.dma("sp", DMA(dbg_o["hT"][:, k, 0:int(_os.environ.get('S1_N', NT)) * 128], hT[:, k, 0:int(_os.environ.get('S1_N', NT)) * 128]), reads=[("hT", tb, k) for tb in range(NT)], writes=[("dbg_hT", k)])
    if stop_after <= 1:
        P.emit()
        return nc, P

    P.barrier()
    A.reset("phase")
    onesb = A.alloc([128, 128], BF16)
    zerosb = A.alloc([128, 512], BF16)
    SLb = A.alloc([128, 128], BF16)
    mask_fox = [A.alloc([128, 512], BF16) for _ in range(4)]
    mask_sb = [A.alloc([128, 512], BF16) for _ in range(4)]
    negb = A.alloc([8, 1], F32)
    Qp = [A.alloc([128, S], BF16) for _ in range(2)]
    Kp = [A.alloc([128, S], BF16) for _ in range(2)]
    Vp = [A.alloc([128, NT, 128], BF16) for _ in range(2)]
    Pt = [A.alloc([128, 512], BF16) for _ in range(4)]
    e_sb = [A.alloc([128, 512], F32) for _ in range(2)]
    Lp = [A.alloc([128, 512], BF16) for _ in range(3)]
    accb = [A.alloc([128, 512], BF16) for _ in range(3)]
    acc32 = A.alloc([128, 512], F32)
    negonesb = A.alloc([128, 128], BF16)
    rcs = [A.alloc([128, 512], F32) for _ in range(2)]
    rc2 = [A.alloc([128, 512], F32) for _ in range(2)]
    osb = [A.alloc([128, 512], BF16) for _ in range(2)]
    f_e = [A.alloc([8, 512], F32)] * 2
    f_sp = [A.alloc([8, 512], F32)] * 2
    f_nF = [A.alloc([8, 512], F32) for _ in range(2)]
    f_r = [A.alloc([8, 512], F32)] * 2
    f_hb = [A.alloc([8, 12, 512], BF16)] * 2
    f_ones = A.alloc([8, 1], F32)

    P.dve(MEMSET(onesb, 1.0), writes=["onesb"])
    P.dve(MEMSET(negonesb, -1.0), writes=["negonesb"])
    P.dve(MEMSET(zerosb, 0.0), writes=["zerosb"])
    P.pool(lambda e: e.affine_select(out=SLb, in_=onesb, pattern=[[1, 128]], compare_op=ALU.is_ge,
                                     fill=0.0, base=-1, channel_multiplier=-1), reads=["onesb"], writes=["SLb"])
    for m in range(4):
        P.pool(lambda e, m=m: e.affine_select(out=mask_fox[m], in_=zerosb, pattern=[[1, 512]], compare_op=ALU.is_ge,
                                              fill=NEG, base=-128 * m, channel_multiplier=-1),
               reads=["zerosb"], writes=[("mfox", m)])
        P.pool(lambda e, m=m: e.affine_select(out=mask_sb[m], in_=zerosb, pattern=[[1, 512]], compare_op=ALU.is_ge,
                                              fill=NEG, base=-128 * m - 1, channel_multiplier=-1),
               reads=["zerosb"], writes=[("msb", m)])
    for i in range(2):
        P.dve(MEMSET(Qp[i][64:128, :], 0.0), writes=[("Qp", i, "augd")])
        P.dve(MEMSET(Kp[i][64:128, :], 0.0), writes=[("Kp", i, "augd")])
        P.dve(MEMSET(Vp[i][:, :, 64:128], 1.0), writes=[("Vp", i, "ones")])
    P.dve(MEMSET(f_ones, 1.0), writes=["f_ones"])
    P.dve(MEMSET(f_hb[0], 1.0), writes=[("f_hb", r) for r in (0, 9, 10, 11, "ones")])

    h2s_z = h2s_d.rearrange("r (a c) -> (r a) c", c=512)
    P.dma("sp", DMA(negb, bfg_d), writes=["negb"])
    P.dve(TS(negb, negb, -1.0, None, ALU.mult), reads=["negb"], writes=["negb"])

    for tc in range(NC_):
        i2 = tc % 2
        fb = 7
        for k in range(8):
            P.pe(MM(banks[fb][0:8, :], wfa[:, k, :], hT[:, k, tc * 512:(tc + 1) * 512], k == 0, k == 7),
                 reads=["wfa"] + hkeys(tc, k), writes=[("bank", fb)])
        P.act(ACT(f_e[i2], banks[fb][0:8, :], AF.Exp, scale=-1.0, bias=negb[:, 0:1]), reads=[("bank", fb), "negb"], writes=["f_e"])
        P.act(ACT(f_sp[i2], f_e[i2], AF.Ln, bias=1.0), reads=["f_e"], writes=["f_sp"])
        init = 0.0 if tc == 0 else f_nF[1 - i2][:, 511:512]
        P.dve(lambda e, i2=i2, init=init: e.tensor_tensor_scan(out=f_nF[i2], data0=f_ones[:, 0:1].to_broadcast([8, 512]),
                                                                data1=f_sp[i2], initial=init, op0=ALU.mult, op1=ALU.add),
              reads=["f_sp", "f_ones", ("f_nF", 1 - i2)], writes=[("f_nF", i2)])
        hb = f_hb[i2]
        P.dve(CP(hb[:, 9, :], f_nF[i2]), reads=[("f_nF", i2)], writes=[("f_hb", 9)])
        P.dve(TT(f_r[i2], f_nF[i2], hb[:, 9, :], ALU.subtract), reads=[("f_nF", i2), ("f_hb", 9)], writes=["f_r"])
        P.dve(CP(hb[:, 10, :], f_r[i2]), reads=["f_r"], writes=[("f_hb", 10)])
        P.dve(TT(f_r[i2], f_r[i2], hb[:, 10, :], ALU.subtract), reads=["f_r", ("f_hb", 10)], writes=["f_r"])
        P.dve(CP(hb[:, 11, :], f_r[i2]), reads=["f_r"], writes=[("f_hb", 11)])
        P.dve(TS(hb[:, 0:3, :], hb[:, 9:12, :], -1.0, None, ALU.mult), reads=[("f_hb", 9), ("f_hb", 10), ("f_hb", 11)],
              writes=[("f_hb", 0)])
        P.dma("sp", DMA(fsc[:, :, tc * 512:(tc + 1) * 512], hb),
              reads=[("f_hb", r) for r in (0, 9, 10, 11, "ones")], writes=[("fsc", tc)])

    def proj_part(br, h, part, buf, pbanks=(7, 7, 7)):
        tc = part
        pb = pbanks[0]
        for k in range(8):
            P.pe(MM(banks[pb][:, :], wq[:, k, h * 64:h * 64 + 128], hT[:, k, tc * 512:(tc + 1) * 512], k == 0, k == 7),
                 reads=["wq"] + hkeys(tc, k), writes=[("bank", pb)])
        P.dve(TS(Qp[buf][0:64, tc * 512:(tc + 1) * 512], banks[pb][0:64, :], 0.125, None, ALU.mult),
              reads=[("bank", pb)], writes=[("Qp", buf, tc)])
        pb2 = pbanks[1]
        for k in range(8):
            P.pe(MM(banks[pb2][:, :], wk[:, k, h * 64:h * 64 + 128], hT[:, k, tc * 512:(tc + 1) * 512], k == 0, k == 7),
                 reads=["wk"] + hkeys(tc, k), writes=[("bank", pb2)])
        P.dve(CP(Kp[buf][0:64, tc * 512:(tc + 1) * 512], banks[pb2][0:64, :]),
              reads=[("bank", pb2)], writes=[("Kp", buf, tc)])
        pb = pbanks[2]
        vps = banks[pb][:, 0:256].rearrange("p (a b) -> p a b", b=64)
        for q in range(4):
            tb = part * 4 + q
            for k in range(8):
                P.pe(MM(vps[:, q, :], hT[:, k, tb * 128:(tb + 1) * 128], wv[:, k, h * 64:(h + 1) * 64], k == 0, k == 7),
                     reads=["wv", ("hT", tb, k)], writes=[("bank", pb)])
        P.dve(CP(Vp[buf][:, part * 4:(part + 1) * 4, 0:64], vps), reads=[("bank", pb)], writes=[("Vp", buf, part)])
        if br == 0 and part == 7:
            P.dma("sp", DMA(Qp[buf][64:70, :], fsc[h, 0:6, :]), reads=[("fsc", t) for t in range(NC_)], writes=[("Qp", buf, "augd")])
            P.dma("sp", DMA(Kp[buf][64:70, :], fsc[h, 6:12, :]), reads=[("fsc", t) for t in range(NC_)], writes=[("Kp", buf, "augd")])
        if br == 1 and h < 2 and part == 0:
            P.dve(MEMSET(Qp[buf][64:96, :], 0.0), writes=[("Qp", buf, "augd")])
            P.dve(MEMSET(Kp[buf][64:96, :], 0.0), writes=[("Kp", buf, "augd")])

    def qk_reads(buf, c, j, fox):
        return [("Qp", buf, c), ("Kp", buf, j // 4), ("Qp", buf, "augd"), ("Kp", buf, "augd")]

    def store_o(br, h, c, src_key, src):
        gh = br * 8 + h
        P.dma("sp", DMA(ot_scr[gh // 2, (gh % 2) * 64:(gh % 2) * 64 + 64, c * 512:(c + 1) * 512], src),
              reads=[src_key], writes=[("ot", gh, c)])

    def fox_head(h, buf, next_proj):
        tiles = []
        for c in range(NC_):
            if c == 0:
                tl = [(c, m, 0, m) for m in range(4)]
            else:
                tl = [(c, 4 * c + m, m * 128, m) for m in range(4)] + [(c, j, 0, None) for j in range(4 * c)]
            for n_, t in enumerate(tl):
                tiles.append(t + (n_ == 0, n_ == len(tl) - 1))
        nt = len(tiles)

        def pv(i):
            c, j, cs, m, first, last = tiles[i]
            ob = 4 + (c % 2)
            P.pe(MM(banks[ob][:, cs:512], Vp[buf][:, j, :], Pt[i % 4][:, cs:512], first, last),
                 reads=[("Vp", buf, j // 4), ("Vp", buf, "ones"), ("Pt", i % 4)], writes=[("bank", ob)])
            if last:
                c2 = c % 2
                P.dve(RECIP(rcs[c2][64:128, :], banks[ob][64:128, :]), reads=[("bank", ob)], writes=[("rcs", c2)])
                P.dma("sp", DMA(rc2[c2][0:64, :], rcs[c2][64:128, :]), reads=[("rcs", c2)], writes=[("rc2", c2)])
                P.dve(TT(osb[c2][0:64, :], banks[ob][0:64, :], rc2[c2][0:64, :], ALU.mult),
                      reads=[("bank", ob), ("rc2", c2)], writes=[("osb", c2)])
                store_o(0, h, c, ("osb", c2), osb[c2][0:64, :])
                if next_proj is not None:
                    next_proj(c)

        for i, (c, j, cs, m, first, last) in enumerate(tiles):
            ab = i % 3
            diag = m is not None
            P.pe(MM(banks[ab][:, cs:512], Kp[buf][:, j * 128:(j + 1) * 128], Qp[buf][:, c * 512 + cs:(c + 1) * 512], True, not diag),
                 reads=qk_reads(buf, c, j, True), writes=[("bank", ab)])
            if diag:
                mw = 512 if c == 0 else cs + 128
                P.pe(MM(banks[ab][:, cs:mw], identb, mask_fox[m][:, cs:mw], False, True),
                     reads=["identb", ("mfox", m)], writes=[("bank", ab)])
            P.act(ACT(Pt[i % 4][:, cs:512], banks[ab][:, cs:512], AF.Exp), reads=[("bank", ab)], writes=[("Pt", i % 4)])
            if i >= 2:
                pv(i - 2)
        pv(nt - 2)
        pv(nt - 1)

    A2B = (2, 3, 6)

    def sb_head(h, buf, next_proj):
        tiles = []
        for c in range(NC_):
            for j in range(4 * c + 3, -1, -1):
                m = j - 4 * c if j >= 4 * c else None
                tiles.append((c, j, 0 if (m is None or m == 3) else m * 128, m))
        nt = len(tiles)

        def zmm(i, bank, stop_last):
            c, j, cs, m = tiles[i]
            diag = m is not None
            P.pe(MM(banks[bank][:, cs:512], Kp[buf][:, j * 128:(j + 1) * 128], Qp[buf][:, c * 512 + cs:(c + 1) * 512], True, stop_last and not diag),
                 reads=qk_reads(buf, c, j, False), writes=[("bank", bank)])
            if diag:
                mw = 512 if m == 3 else cs + 128
                P.pe(MM(banks[bank][:, cs:mw], identb, mask_sb[m][:, cs:mw], False, stop_last),
                     reads=["identb", ("msb", m)], writes=[("bank", bank)])

        def st_z(i):
            zmm(i, i % 2, True)

        def st_exp1(i):
            cs = tiles[i][2]
            P.act(ACT(e_sb[i % 2][:, cs:512], banks[i % 2][:, cs:512], AF.Exp), reads=[("bank", i % 2)], writes=[("e_sb", i % 2)])

        def st_ln(i):
            cs = tiles[i][2]
            P.act(ACT(Lp[i % 3][:, cs:512], e_sb[i % 2][:, cs:512], AF.Ln, bias=1.0), reads=[("e_sb", i % 2)], writes=[("Lp", i % 3)])

        def st_acc(i):
            c, j, cs, m = tiles[i]
            L_ = Lp[i % 3]
            if m == 3:
                P.dve(CP(acc32, L_), reads=[("Lp", i % 3)], writes=["acc32"])
            else:
                P.dve(TT(acc32[:, cs:512], acc32[:, cs:512], L_[:, cs:512], ALU.add), reads=[("Lp", i % 3), "acc32"], writes=["acc32"])
            P.dve(CP(accb[i % 3][:, cs:512], acc32[:, cs:512]), reads=["acc32"], writes=[("accb", i % 3)])

        def st_g2(i):
            cs = tiles[i][2]
            bk = A2B[i % 3]
            zmm(i, bk, False)
            P.pe(MM(banks[bk][:, cs:512], SLb, Lp[i % 3][:, cs:512], False, False), reads=["SLb", ("Lp", i % 3)], writes=[("bank", bk)])
            P.pe(MM(banks[bk][:, cs:512], negonesb, accb[i % 3][:, cs:512], False, True), reads=["negonesb", ("accb", i % 3)], writes=[("bank", bk)])

        def st_exp2(i):
            cs = tiles[i][2]
            bk = A2B[i % 3]
            P.act(ACT(Pt[i % 3][:, cs:512], banks[bk][:, cs:512], AF.Exp), reads=[("bank", bk)], writes=[("Pt", i % 3)])

        def st_pv(i):
            c, j, cs, m = tiles[i]
            ob = 4 + (c % 2)
            rd = [("Vp", buf, j // 4), ("Vp", buf, "ones"), ("Pt", i % 3)]
            P.pe(MM(banks[ob][:, cs:512], Vp[buf][:, j, :], Pt[i % 3][:, cs:512], m == 3, j == 0), reads=rd, writes=[("bank", ob)])
            if j == 0:
                c2 = c % 2
                P.dve(CP(osb[c2][0:64, :], banks[ob][0:64, :]), reads=[("bank", ob)], writes=[("osb", c2)])
                store_o(1, h, c, ("osb", c2), osb[c2][0:64, :])
                if next_proj is not None:
                    next_proj(c)

        for i in range(nt + 4):
            if i < nt:
                st_z(i)
                st_exp1(i)
            if 0 <= i - 3 < nt:
                st_exp2(i - 3)
            if i < nt:
                st_ln(i)
            if 0 <= i - 1 < nt:
                st_acc(i - 1)
            if 0 <= i - 2 < nt:
                st_g2(i - 2)
            if 0 <= i - 4 < nt:
                st_pv(i - 4)

    heads = [(0, h) for h in range(8)] + [(1, h) for h in range(8)]
    n_heads_run = len(heads) if stop_after > 2 else (stop_after - 1) * 0 + 16
    if stop_after == 2 and dbg:
        n_heads_run = 16
    for part in range(8):
        proj_part(0, 0, part, 0, (7, 6, 3))
    for idx, (br, h) in enumerate(heads):
        buf = idx % 2
        if idx + 1 < len(heads):
            nbr, nh = heads[idx + 1]

            def next_proj(c, nbr=nbr, nh=nh, nbuf=1 - buf, idx=idx, br=br):
                if nbr == 1 and nh == 0 and c == 0:
                    load_branch_weights(1)
                proj_part(nbr, nh, c, nbuf, (7, 6, 3) if br == 0 else (7, 7, 7))
        else:
            next_proj = None
        ex_ = idx
        for i in range(idx * 6, idx * 6 + 6):
            P.dma("sp", DMA(h2s_z[i * 128:(i + 1) * 128, :], zerosb), reads=["zerosb"], writes=[("h2s_z", i)])
        if idx == 0:
            P.dma("sp", DMA(cws_d.rearrange("(p a) e -> p (a e)", p=128), zerosb[:, 0:NS * 32].bitcast(F32)), reads=["zerosb"], writes=["cws_z"])
        P.dma("pool", DMA(wg2b_d[ex_ * 256:(ex_ + 1) * 256, :], wg2_d[ex_ * 256:(ex_ + 1) * 256, :]), writes=[("wconv", ex_, 0)])
        P.dma("pool", DMA(wu2b_d[ex_ * 256:(ex_ + 1) * 256, :], wu2_d[ex_ * 256:(ex_ + 1) * 256, :]), writes=[("wconv", ex_, 1)])
        P.dma("pool", DMA(wd2b_d[ex_ * 512:(ex_ + 1) * 512, :], wd2_d[ex_ * 512:(ex_ + 1) * 512, :]), writes=[("wconv", ex_, 2)])
        if br == 0:
            fox_head(h, buf, next_proj)
        else:
            sb_head(h, buf, next_proj)
    if stop_after <= 2:
        P.emit()
        return nc, P

    P.barrier()
    A.reset("phase")
    A.top = A.nbytes
    wga = A.alloc([128, 8, D], BF16)
    wgb = A.alloc([128, 8, D], BF16)
    wbf = A.alloc([128, 4, D], BF16)
    wbs = A.alloc([128, 4, D], BF16)
    wout = A.alloc([128, 8, D], BF16)
    wr = A.alloc([128, 8, 20], F32)
    wrs = A.alloc([128, 8, 20], F32)
    rbias = A.alloc([128, 20], F32)
    br_b = A.alloc([128, 20], F32)
    otc = [A.alloc([128, 8, 512], BF16) for _ in range(2)]
    sg_sb = [A.alloc([128, 512], F32) for _ in range(2)]
    m_sb = [A.alloc([128, 512], F32) for _ in range(2)]
    mg = A.alloc([128, 8, 512], BF16)
    b2rep = mg[:, 0:4, :].rearrange("p a b -> p (a b)").bitcast(F32).rearrange("p (a b) -> p a b", b=128)
    xb3 = [A.alloc([128, D], F32) for _ in range(2)]
    x1b = [A.alloc([128, D], F32) for _ in range(2)]
    junk3 = [A.alloc([128, D], BF16)] * 2
    h2t = [A.alloc([128, D], BF16) for _ in range(2)]
    s2_b = A.alloc([128, D], F32)
    b2_b = A.alloc([128, D], F32)
    xT_sb = A.alloc([128, 8, 128], F32)
    ssq3 = A.alloc([128, NT], F32)
    rstd3 = A.alloc([128, NT], F32)
    gate1_b = A.alloc([128, D], F32)
    P.dma("sp", DMA(gate1_b[:, 0:512], gsc[0][:, 0:512]), writes=[("gate", 0, 0)])
    P.dma("sp", DMA(gate1_b[:, 512:1024], gsc[0][:, 512:1024]), writes=[("gate", 0, 1)])

    P.dma("pool", DMA(wga, win_v[:, :, 3080:4104]), writes=["wga"])
    P.dma("pool", DMA(wbf, wbf_d.rearrange("(q p) n -> p q n", p=128)), writes=["wbf"])
    P.dma("pool", DMA(wgb, win_v[:, :, 4104:5128]), writes=["wgb"])
    P.dma("pool", DMA(wbs, wbs_d.rearrange("(q p) n -> p q n", p=128)), writes=["wbs"])
    P.dma("pool", DMA(wout, wout_d.rearrange("(k p) n -> p k n", p=128)), writes=["wout"])
    P.dma("sp", DMA(wr, wr_d.rearrange("(k p) n -> p k n", p=128)), writes=["wr"])
    P.dma("sp", DMA(br_b, br_row_d[0:1, :].partition_broadcast(128)), writes=["br_b"])
    P.dve(TT(wrs, wr, s2[:, :].unsqueeze(2).to_broadcast([128, 8, 20]), ALU.mult), reads=["wr", "s2"], writes=["wrs"])
    P.dve(CP(b2rep, b2[:, :].unsqueeze(2).to_broadcast([128, 8, 128])), reads=["b2"], writes=[("mg", fc) for fc in range(8)])
    for k in range(8):
        P.pe(MM(banks[4][:, 0:20], b2rep[:, k, :], wr[:, k, :], k == 0, k == 7), reads=[("mg", fc) for fc in range(8)] + ["wr"], writes=[("bank", 4)])
    P.dma("sp", DMA(b2_b, gsc[2]), writes=["b2_b"])
    P.dma("sp", DMA(s2_b, gsc[3]), writes=["s2_b"])
    P.dve(TT(rbias, banks[4][:, 0:20], br_b, ALU.add), reads=[("bank", 4), "br_b"], writes=["rbias"])

    lg_all = A.alloc([128, NT, 20], F32)
    rbig = mg.rearrange("p a b -> p (a b)").bitcast(F32).rearrange("p (a b) -> p a b", b=64)

    def router_all():
        B_ = NT
        r = rbig
        X_ = mybir.AxisListType.X
        gl = lg_all[:, :, 0:4]
        el = lg_all[:, :, 4:20].rearrange("p b (g e) -> p b g e", e=4)
        gmax, m1, m2, d12, w1, w2, ssum, gp = (r[:, :, i] for i in range(8))
        dif, sg_, ex = r[:, :, 8:12], r[:, :, 12:16], r[:, :, 16:20]
        esel, oh1, es2, oh2, tmpw = r[:, :, 20:24], r[:, :, 24:28], r[:, :, 28:32], r[:, :, 32:36], r[:, :, 36:40]
        prod = r[:, :, 44:60].rearrange("p b (g e) -> p b g e", e=4)
        lk = [("lg", tb) for tb in range(NT)]

        def bc(v):
            return v.unsqueeze(2).to_broadcast([128, B_, 4])
        K = ["rbig"] + [("mg", fc) for fc in range(8)]
        P.dve(lambda e: e.reduce_max(out=gmax, in_=gl, axis=X_), reads=lk, writes=K)
        P.dve(TT(ohg_all, gl, bc(gmax), ALU.is_equal), reads=lk + K, writes=[("ohg", tb) for tb in range(NT)])
        P.dve(TT(dif, gl, bc(gmax), ALU.subtract), reads=lk + K, writes=K)
        P.act(ACT(sg_, dif, AF.Sigmoid), reads=K, writes=K)
        P.dve(TS(ex, sg_, -1.0, 1.0, ALU.mult, ALU.add), reads=K, writes=K)
        P.dve(RECIP(ex, ex), reads=K, writes=K)
        P.dve(TT(ex, ex, sg_, ALU.mult), reads=K, writes=K)
        P.dve(lambda e: e.reduce_sum(out=ssum, in_=ex, axis=X_), reads=K, writes=K)
        P.dve(RECIP(gp, ssum), reads=K, writes=K)
        P.dve(TT(prod, el, ohg_all.unsqueeze(3).to_broadcast([128, B_, 4, 4]), ALU.mult),
              reads=lk + [("ohg", tb) for tb in range(NT)], writes=K)
        P.dve(lambda e: e.reduce_sum(out=esel, in_=prod.rearrange("p b g e -> p b e g"), axis=X_), reads=K, writes=K)
        P.dve(lambda e: e.reduce_max(out=m1, in_=esel, axis=X_), reads=K, writes=K)
        P.dve(TT(oh1, esel, bc(m1), ALU.is_equal), reads=K, writes=K)
        P.dve(STT(es2, oh1, -1.0e30, esel, ALU.mult, ALU.add), reads=K, writes=K)
        P.dve(lambda e: e.reduce_max(out=m2, in_=es2, axis=X_), reads=K, writes=K)
        P.dve(TT(oh2, es2, bc(m2), ALU.is_equal), reads=K, writes=K)
        P.dve(TT(d12, m1, m2, ALU.subtract), reads=K, writes=K)
        P.act(ACT(w1, d12, AF.Sigmoid), reads=K, writes=K)
        P.dve(TS(w2, w1, -1.0, 1.0, ALU.mult, ALU.add), reads=K, writes=K)
        P.dve(TT(w1, w1, gp, ALU.mult), reads=K, writes=K)
        P.dve(TT(w2, w2, gp, ALU.mult), reads=K, writes=K)
        P.dve(TT(tmpw, oh1, bc(w1), ALU.mult), reads=K, writes=K)
        P.dve(TT(oh2, oh2, bc(w2), ALU.mult), reads=K, writes=K)
        P.dve(TT(cw_all, tmpw, oh2, ALU.add), reads=K, writes=[("cw", tb) for tb in range(NT)])

    ot_v = ot_scr.rearrange("q p t -> p q t")

    def blk_p1(tc, q4):
        tb = tc * 4 + q4
        t2 = tb % 2
        P.dma("sp", DMA(xb3[t2], x_d[tb * 128:(tb + 1) * 128, :]), writes=[("xb3", t2)])
        for hf in range(2):
            yb_ = 4 + hf
            for fc in range(8):
                P.pe(MM(banks[yb_][:, :], mg[:, fc, q4 * 128:(q4 + 1) * 128], wout[:, fc, hf * 512:(hf + 1) * 512], fc == 0, fc == 7),
                     reads=[("mg", fc), "wout"], writes=[("bank", yb_)])
            P.dve(TT(x1b[t2][:, hf * 512:(hf + 1) * 512], banks[yb_][:, :], gate1_b[:, hf * 512:(hf + 1) * 512], ALU.mult),
                  reads=[("bank", yb_), ("gate", 0, hf)], writes=[("x1b", t2, hf)])
        xk = [("x1b", t2, 0), ("x1b", t2, 1)]
        P.pool(TT(x1b[t2], x1b[t2], xb3[t2], ALU.add), reads=xk + [("xb3", t2)], writes=xk)
        P.dma("sp", DMA(out_d[tb * 128:(tb + 1) * 128, :], x1b[t2]), reads=xk, writes=[("out", tb)])
        P.act(ACT(junk3[t2], x1b[t2], AF.Square, accum_out=ssq3[:, tb:tb + 1]), reads=xk, writes=[("ssq", 100 + tb), "junk3"])

    def blk_p2(tb):
        t2 = tb % 2
        tg = 100 + tb
        xk = [("x1b", t2, 0), ("x1b", t2, 1)]
        P.dve(TS(rstd3[:, tb:tb + 1], ssq3[:, tb:tb + 1], 1.0 / D, EPS, ALU.mult, ALU.add), reads=[("ssq", tg)], writes=[("rs", tg)])
        P.act(ACT(rstd3[:, tb:tb + 1], rstd3[:, tb:tb + 1], AF.Sqrt), reads=[("rs", tg)], writes=[("rs", tg)])
        P.dve(RECIP(rstd3[:, tb:tb + 1], rstd3[:, tb:tb + 1]), reads=[("rs", tg)], writes=[("rs", tg)])
        for k in range(8):
            bk_ = 6 + k // 4
            tv = banks[bk_][:, :].rearrange("p (a b) -> p a b", b=128)
            P.pe(TR(tv[:, k % 4, :], x1b[t2][:, k * 128:(k + 1) * 128], identf), reads=xk + ["identf"], writes=[("bank", bk_)])
        for hh in range(2):
            P.dve(CP(xT_sb[:, hh * 4:(hh + 1) * 4, :], banks[6 + hh][:, :].rearrange("p (a b) -> p a b", b=128)),
                  reads=[("bank", 6 + hh)], writes=[("xT", hh)])
        for k in range(8):
            P.pe(MM(banks[6][:, 0:20], xT_sb[:, k, :], wrs[:, k, :], k == 0, k == 7), reads=[("xT", 0), ("xT", 1), "wrs"], writes=[("bank", 6)])
        P.dve(STT(lg_all[:, tb, :], banks[6][:, 0:20], rstd3[:, tb:tb + 1], rbias, ALU.mult, ALU.add),
              reads=[("bank", 6), ("rs", tg), "rbias"], writes=[("lg", tb)])
        P.dve(STT(xb3[t2], x1b[t2], rstd3[:, tb:tb + 1], s2_b, ALU.mult, ALU.mult), reads=xk + [("rs", tg), "s2_b"], writes=[("xb3", t2)])
        P.pool(TT(h2t[t2], xb3[t2], b2_b, ALU.add), reads=[("xb3", t2), "b2_b"], writes=[("h2t", t2)])
        P.dma("sp", DMA(h2tok_d[tb * 128:(tb + 1) * 128, :], h2t[t2]), reads=[("h2t", t2)], writes=[("h2tok", tb)])

    for tc in range(NC_):
        o2 = tc % 2
        P.dma("sp", DMA(otc[o2], ot_v[:, :, tc * 512:(tc + 1) * 512]),
              reads=[("ot", gh, tc) for gh in range(16)], writes=[("otc", o2)])
        for fc in range(8):
            for side in range(2):
                wgx = wga if side == 0 else wgb
                wbx = wbf if side == 0 else wbs
                wgk = "wga" if side == 0 else "wgb"
                wbk = "wbf" if side == 0 else "wbs"
                gbk, bbk = side, 2 + side
                for k in range(8):
                    P.pe(MM(banks[gbk][:, :], wgx[:, k, fc * 128:(fc + 1) * 128], hT[:, k, tc * 512:(tc + 1) * 512], k == 0, k == 7),
                         reads=[wgk] + hkeys(tc, k), writes=[("bank", gbk)])
                P.act(ACT(sg_sb[side], banks[gbk][:, :], AF.Sigmoid), reads=[("bank", gbk)], writes=[("sg", side)])
                for q in range(4):
                    P.pe(MM(banks[bbk][:, :], wbx[:, q, fc * 128:(fc + 1) * 128], otc[o2][:, side * 4 + q, :], q == 0, q == 3),
                         reads=[wbk, ("otc", o2)], writes=[("bank", bbk)])
                P.dve(TT(m_sb[side], banks[bbk][:, :], sg_sb[side], ALU.mult), reads=[("bank", bbk), ("sg", side)], writes=[("m", side)])
            P.pool(TT(mg[:, fc, :], m_sb[0], m_sb[1], ALU.add), reads=[("m", 0), ("m", 1)], writes=[("mg", fc)])
        for q4 in range(4):
            blk_p1(tc, q4)
            if tc * 4 + q4 >= 1:
                blk_p2(tc * 4 + q4 - 1)
    blk_p2(NT - 1)
    router_all()
    if dbg:
        P.dma("sp", DMA(dbg_o["comb"], cw_all), reads=[("cw", tb) for tb in range(NT)], writes=["dbg_comb"])
    if stop_after <= 3:
        P.emit()
        return nc, P

    P.barrier()
    A.reset("pre_hT")
    U32 = mybir.dt.uint32
    rconst = A.alloc([128, 40], F32)
    SUTf = A.alloc([128, 128], F32)
    rank_sb = A.alloc([128, NT, 4], F32)
    tot_sb = A.alloc([128, NT, 4], F32)
    incl = A.alloc([128, NT, 4], F32)
    Tt = A.alloc([128, NT, 4], F32)
    posf = A.alloc([128, NT], F32)
    idx = A.alloc([128, NT], U32)
    Ng = A.alloc([128, 4], F32)
    cnt = A.alloc([128, 4], F32)
    Pg = A.alloc([128, 4], F32)
    off = A.alloc([128, 4], F32)
    endg = A.alloc([128, 4], F32)
    gsl = A.alloc([128, NS], F32)
    gk = A.alloc([128, NS], F32)
    idx1f = A.alloc([128, NS, 8], F32)
    idx1 = A.alloc([128, NS, 8], U32)
    idx2f = A.alloc([128, NS, 16], F32)
    idx2 = A.alloc([128, NS, 16], U32)
    gate2_b = A.alloc([128, D], F32)
    fg_b = A.alloc([128, D], F32)
    xrow4 = [A.alloc([128, 4, D], BF16) for _ in range(2)]
    xs = [A.alloc([128, 4, D], BF16) for _ in range(2)]
    cwS = [A.alloc([128, 4, 4], F32) for _ in range(2)]
    h2Ts = [A.alloc([128, 8, 512], BF16) for _ in range(2)]
    wgs = [A.alloc([128, 8, 512], BF16) for _ in range(2)]
    wus = [A.alloc([128, 8, 512], BF16) for _ in range(2)]
    wds = [A.alloc([128, 4, D], BF16) for _ in range(2)]
    actT = [A.alloc([128, 4, 512], BF16) for _ in range(2)]
    sgm = [A.alloc([128, 512], F32) for _ in range(2)]
    yacc = [A.alloc([128, 4, D], F32) for _ in range(2)]
    yb = [A.alloc([128, D], F32) for _ in range(2)]
    x1f = [A.alloc([128, D], F32) for _ in range(2)]
    ssq4 = A.alloc([128, NT], F32)
    rstd4 = A.alloc([128, NT], F32)
    junk4 = A.alloc([128, D], BF16)

    P.dma("sp", DMA(rconst, rc_d), writes=["rconst"])
    P.dma("sp", DMA(gate2_b, gsc[1]), writes=[("gate", 1, 0), ("gate", 1, 1)])
    P.dma("sp", DMA(fg_b, fg_row_d[0:1, :].partition_broadcast(128)), writes=["fg_b"])
    P.pool(lambda e: e.affine_select(out=SUTf, in_=onesf, pattern=[[1, 128]], compare_op=ALU.is_ge,
                                     fill=0.0, base=-1, channel_multiplier=-1), reads=["onesf"], writes=["SUTf"])
    okeys = [("ohg", tb) for tb in range(NT)]
    ohg2d = ohg_all.rearrange("p a b -> p (a b)")
    P.pe(MM(banks[0][:, 0:128], SUTf, ohg2d, True, True), reads=["SUTf"] + okeys, writes=[("bank", 0)])
    P.pe(MM(banks[1][:, 0:128], onesf, ohg2d, True, True), reads=["onesf"] + okeys, writes=[("bank", 1)])
    P.dve(CP(rank_sb.rearrange("p a b -> p (a b)"), banks[0][:, 0:128]), reads=[("bank", 0)], writes=["rank_sb"])
    P.dve(CP(tot_sb.rearrange("p a b -> p (a b)"), banks[1][:, 0:128]), reads=[("bank", 1)], writes=["tot_sb"])
    for g in range(4):
        P.dve(lambda e, g=g: e.tensor_tensor_scan(out=incl[:, :, g], data0=onesf[:, 0:NT], data1=tot_sb[:, :, g], initial=0.0,
                                                  op0=ALU.mult, op1=ALU.add), reads=["tot_sb", "onesf"], writes=[("incl", g)])
    ik = [("incl", g) for g in range(4)]
    P.dve(TT(Tt, incl, tot_sb, ALU.subtract), reads=ik + ["tot_sb"], writes=["Tt"])
    P.dve(TT(Tt, Tt, rank_sb, ALU.add), reads=["Tt", "rank_sb"], writes=["Tt"])
    P.dve(CP(Ng, incl[:, NT - 1, :]), reads=ik, writes=["Ng"])
    P.dve(MEMSET(cnt, 0.0), writes=["cnt"])
    for k in range(8):
        P.dve(STT(cnt, Ng, 512.0 * k, cnt, ALU.is_gt, ALU.add), reads=["Ng", "cnt"], writes=["cnt"])
    P.dve(TS(Pg, cnt, 512.0, None, ALU.mult), reads=["cnt"], writes=["Pg"])
    P.dve(MEMSET(off, 0.0), writes=["off"])
    for g in range(1, 4):
        P.dve(TT(off[:, g:g + 1], off[:, g - 1:g], Pg[:, g - 1:g], ALU.add), reads=["off", "Pg"], writes=["off"])
    P.dve(TT(endg, off, Pg, ALU.add), reads=["off", "Pg"], writes=["endg"])
    for g in range(4):
        P.dve(TS(Tt[:, :, g], Tt[:, :, g], off[:, g:g + 1], None, ALU.add), reads=["Tt", "off"], writes=["Tt"])
    P.dve(TT(Tt, Tt, ohg_all, ALU.mult), reads=["Tt"] + okeys, writes=["Tt"])
    P.dve(lambda e: e.reduce_sum(out=posf, in_=Tt, axis=mybir.AxisListType.X), reads=["Tt"], writes=["posf"])
    P.dve(CP(idx, posf), reads=["posf"], writes=["idx"])
    P.dve(MEMSET(gsl, 0.0), writes=["gsl"])
    for g in range(4):
        P.dve(STT(gsl, rconst[:, 24:36], endg[:, g:g + 1], gsl, ALU.is_ge, ALU.add), reads=["rconst", "endg", "gsl"], writes=["gsl"])
    P.dve(TS(gsl, gsl, 3.0, None, ALU.min), reads=["gsl"], writes=["gsl"])
    P.dve(TS(gk, gsl, 1024.0, None, ALU.mult), reads=["gsl"], writes=["gk"])
    P.dve(TT(idx1f, rconst[:, 0:8].unsqueeze(1).to_broadcast([128, NS, 8]), gk[:, :].unsqueeze(2).to_broadcast([128, NS, 8]), ALU.add),
          reads=["rconst", "gk"], writes=["idx1f"])
    P.dve(CP(idx1, idx1f), reads=["idx1f"], writes=["idx1"])
    P.dve(TS(gk, gsl, 2048.0, None, ALU.mult), reads=["gsl", "idx1f"], writes=["gk"])
    P.dve(TT(idx2f, rconst[:, 8:24].unsqueeze(1).to_broadcast([128, NS, 16]), gk[:, :].unsqueeze(2).to_broadcast([128, NS, 16]), ALU.add),
          reads=["rconst", "gk"], writes=["idx2f"])
    P.dve(CP(idx2, idx2f), reads=["idx2f"], writes=["idx2"])

    IOA = bass.IndirectOffsetOnAxis
    for g4 in range(NT // 4):
        r2 = g4 % 2
        P.dma("sp", DMA(xrow4[r2], h2tok_d[g4 * 512:(g4 + 1) * 512, :].rearrange("(a p) n -> p a n", p=128)), writes=[("xrow", r2)])
        for a_ in range(4):
            tb = g4 * 4 + a_
            P.dma("pool", lambda e, tb=tb, r2=r2, a_=a_: e.indirect_dma_start(out=h2s_d[:, :], out_offset=IOA(ap=idx[:, tb:tb + 1], axis=0),
                                                                              in_=xrow4[r2][:, a_, :], in_offset=None),
                  reads=[("xrow", r2), "idx"], writes=[("h2s_sc", tb)])
            P.dma("pool", lambda e, tb=tb: e.indirect_dma_start(out=cws_d[:, :], out_offset=IOA(ap=idx[:, tb:tb + 1], axis=0),
                                                                in_=cw_all[:, tb, :], in_offset=None),
                  reads=[("cw", tb), "idx"], writes=[("cws_sc", tb)])
    sc_all = [("h2s_sc", tb) for tb in range(NT)]
    cw_sc_all = [("cws_sc", tb) for tb in range(NT)]

    def slot_prep(sl):
        s2_ = sl % 2
        P.dma("sp", DMA(xs[s2_], h2s_d[sl * 512:(sl + 1) * 512, :].rearrange("(a p) n -> p a n", p=128)),
              reads=sc_all, writes=[("xs", s2_)])
        P.dma("sp", DMA(cwS[s2_], cws_d[sl * 512:(sl + 1) * 512, :].rearrange("(a p) e -> p a e", p=128)),
              reads=cw_sc_all, writes=[("cwS", s2_)])
        for a_ in range(4):
            tbk = 6 + (a_ % 2)
            pT = bank_bf(tbk)
            for k in range(8):
                P.pe(TR(pT[:, k * 128:(k + 1) * 128], xs[s2_][:, a_, k:D:8], identb), reads=[("xs", s2_), "identb"], writes=[("bank", tbk)])
            P.act(ACT(h2Ts[s2_][:, :, a_ * 128:(a_ + 1) * 128], pT.rearrange("p (k t) -> p k t", t=128), AF.Identity),
                  reads=[("bank", tbk)], writes=[("h2Ts", s2_, a_)])

    slot_prep(0)
    for sl in range(NS):
        s2_ = sl % 2
        hk = [("h2Ts", s2_, a_) for a_ in range(4)]
        for el in range(4):
            it = sl * 4 + el
            wb = it % 2
            for hf in range(2):
                P.dma("pool", lambda e, wb=wb, hf=hf, sl=sl, el=el: e.indirect_dma_start(
                    out=wgs[wb].rearrange("p k n -> p (k n)")[:, hf * 2048:(hf + 1) * 2048], out_offset=None, in_=wg2b_d[:, :],
                    in_offset=IOA(ap=idx1[:, sl, el * 2 + hf:el * 2 + hf + 1], axis=0)), reads=["idx1"], writes=[("wgs", wb, hf)])
                P.dma("pool", lambda e, wb=wb, hf=hf, sl=sl, el=el: e.indirect_dma_start(
                    out=wus[wb].rearrange("p k n -> p (k n)")[:, hf * 2048:(hf + 1) * 2048], out_offset=None, in_=wu2b_d[:, :],
                    in_offset=IOA(ap=idx1[:, sl, el * 2 + hf:el * 2 + hf + 1], axis=0)), reads=["idx1"], writes=[("wus", wb, hf)])
            for q in range(4):
                P.dma("pool", lambda e, wb=wb, q=q, sl=sl, el=el: e.indirect_dma_start(
                    out=wds[wb][:, q, :], out_offset=None, in_=wd2b_d[:, :],
                    in_offset=IOA(ap=idx2[:, sl, el * 4 + q:el * 4 + q + 1], axis=0)), reads=["idx2"], writes=[("wds", wb, q)])
            wdk = [("wds", wb, q) for q in range(4)]
            if el == 2 and sl + 1 < NS:
                slot_prep(sl + 1)
            a2 = it % 2
            for ffc in range(4):
                gb_, ub_ = ffc % 2, 2 + (ffc % 2)
                for k in range(8):
                    P.pe(MM(banks[gb_][:, :], wgs[wb][:, k, ffc * 128:(ffc + 1) * 128], h2Ts[s2_][:, k, :], k == 0, k == 7),
                         reads=[("wgs", wb, 0), ("wgs", wb, 1)] + hk, writes=[("bank", gb_)])
                for k in range(8):
                    P.pe(MM(banks[ub_][:, :], wus[wb][:, k, ffc * 128:(ffc + 1) * 128], h2Ts[s2_][:, k, :], k == 0, k == 7),
                         reads=[("wus", wb, 0), ("wus", wb, 1)] + hk, writes=[("bank", ub_)])
                P.act(ACT(sgm[ffc % 2], banks[gb_][:, :], AF.Silu), reads=[("bank", gb_)], writes=[("sgm", ffc % 2)])
                P.dve(TT(actT[a2][:, ffc, :], banks[ub_][:, :], sgm[ffc % 2], ALU.mult),
                      reads=[("bank", ub_), ("sgm", ffc % 2)], writes=[("actT", a2, ffc)])
            for a_ in range(4):
                for hf in range(2):
                    ybk = 4 + ((a_ * 2 + hf) % 2)
                    for ffc in range(4):
                        P.pe(MM(banks[ybk][:, :], actT[a2][:, ffc, a_ * 128:(a_ + 1) * 128], wds[wb][:, ffc, hf * 512:(hf + 1) * 512], ffc == 0, ffc == 3),
                             reads=[("actT", a2, f) for f in range(4)] + wdk, writes=[("bank", ybk)])
                    yv = yacc[s2_][:, a_, hf * 512:(hf + 1) * 512]
                    cwv = cwS[s2_][:, a_, el:el + 1]
                    if el == 0:
                        P.dve(TS(yv, banks[ybk][:, :], cwv, None, ALU.mult), reads=[("bank", ybk), ("cwS", s2_)], writes=[("yacc", s2_, a_, hf)])
                    else:
                        P.dve(STT(yv, banks[ybk][:, :], cwv, yv, ALU.mult, ALU.add),
                              reads=[("bank", ybk), ("cwS", s2_), ("yacc", s2_, a_, hf)], writes=[("yacc", s2_, a_, hf)])
        P.dma("sp", DMA(ys_d[sl * 512:(sl + 1) * 512, :].rearrange("(a p) n -> p a n", p=128), yacc[s2_]),
              reads=[("yacc", s2_, a_, hf) for a_ in range(4) for hf in range(2)], writes=[("ys", sl)])
    ys_all = [("ys", sl) for sl in range(NS)]
    for tb in range(NT):
        t2 = tb % 2
        P.dma("pool", lambda e, tb=tb, t2=t2: e.indirect_dma_start(out=yb[t2], out_offset=None, in_=ys_d[:, :],
                                                                   in_offset=IOA(ap=idx[:, tb:tb + 1], axis=0)),
              reads=["idx"] + ys_all, writes=[("yb", t2)])
        P.dma("sp", DMA(x1f[t2], out_d[tb * 128:(tb + 1) * 128, :]), writes=[("x1f", t2)])
        P.dve(TT(yb[t2], yb[t2], gate2_b, ALU.mult), reads=[("yb", t2), ("gate", 1, 0), ("gate", 1, 1)], writes=[("yb", t2)])
        P.dve(TT(x1f[t2], x1f[t2], yb[t2], ALU.add), reads=[("x1f", t2), ("yb", t2)], writes=[("x1f", t2)])
        P.act(ACT(junk4, x1f[t2], AF.Square, accum_out=ssq4[:, tb:tb + 1]), reads=[("x1f", t2)], writes=[("ssq4", tb), "junk4"])
        P.dve(TS(rstd4[:, tb:tb + 1], ssq4[:, tb:tb + 1], 1.0 / D, EPS, ALU.mult, ALU.add), reads=[("ssq4", tb)], writes=[("rs4", tb)])
        P.act(ACT(rstd4[:, tb:tb + 1], rstd4[:, tb:tb + 1], AF.Sqrt), reads=[("rs4", tb)], writes=[("rs4", tb)])
        P.dve(RECIP(rstd4[:, tb:tb + 1], rstd4[:, tb:tb + 1]), reads=[("rs4", tb)], writes=[("rs4", tb)])
        P.dve(STT(x1f[t2], x1f[t2], rstd4[:, tb:tb + 1], fg_b, ALU.mult, ALU.mult), reads=[("x1f", t2), ("rs4", tb), "fg_b"], writes=[("x1f", t2)])
        P.dma("sp", DMA(out_d[tb * 128:(tb + 1) * 128, :], x1f[t2]), reads=[("x1f", t2)], writes=[("out", tb)])
    P.emit()
    return nc, P


def _rconst():
    c = np.zeros((128, 40), np.float32)
    p = np.arange(128, dtype=np.float32)
    for el in range(4):
        for hf in range(2):
            c[:, el * 2 + hf] = 2 * p + 256 * el + hf
        for q in range(4):
            c[:, 8 + el * 4 + q] = p + 512 * el + 128 * q
    for sl in range(12):
        c[:, 24 + sl] = 512.0 * sl
    return c


def make_in_maps(inp):
    f = lambda a: np.ascontiguousarray(np.asarray(a, dtype=np.float32))
    B = inp["x"].shape[0]
    ada_b = f(inp["ada_b"][0])
    shared = {
        "ada_w": f(inp["ada_w"][0]),
        "ada_bT": f(ada_b.reshape(48, 128).T),
        "ada_b_row": f(ada_b.reshape(1, -1)),
        "n1g": f(inp["norm1_g"][0].reshape(8, 128).T),
        "n2g": f(inp["norm2_g"][0].reshape(8, 128).T),
        "fg_row": f(inp["final_g"].reshape(1, -1)),
        "w_in": f(inp["w_in"][0]),
        "b_forget": f(inp["b_forget"][0].reshape(8, 1)),
        "w_bf": f(inp["w_branch_fox"][0]),
        "w_bs": f(inp["w_branch_sb"][0]),
        "w_out": f(inp["w_out"][0]),
        "w_r": f(np.concatenate([inp["router_group_w"][0], inp["router_expert_w"][0]], axis=1)),
        "b_r_row": f(np.concatenate([inp["router_group_b"][0], inp["router_expert_b"][0]], axis=0).reshape(1, 20)),
        "e_wg": f(inp["expert_w_gate"][0]).reshape(16 * 256, 2048),
        "e_wu": f(inp["expert_w_up"][0]).reshape(16 * 256, 2048),
        "e_wd": f(inp["expert_w_down"][0]).reshape(16 * 512, 1024),
        "n2g_row": f(inp["norm2_g"][0].reshape(1, -1)),
        "rconst": _rconst(),
    }
    maps = []
    for b in range(B):
        m = dict(shared)
        m["x"] = f(inp["x"][b])
        m["cT"] = f(np.asarray(inp["c"][b]).reshape(8, 128).T)
        maps.append(m)
    return maps


_CACHE = {}


def kernel(**inputs):
    if "nc" not in _CACHE:
        _CACHE["nc"] = build_program()[0]
    nc = _CACHE["nc"]
    in_maps = make_in_maps(inputs)
    res = run_bass_kernel_spmd(nc, in_maps, core_ids=list(range(len(in_maps))))
    return np.stack([np.asarray(r["out"], dtype=np.float32) for r in res.results], axis=0)
```

```python
import contextlib
import numpy as np
import concourse.bass as bass
import concourse.mybir as mybir
from concourse.bass_utils import run_bass_kernel_spmd

F32 = mybir.dt.float32
BF16 = mybir.dt.bfloat16
U8 = mybir.dt.uint8
AF = mybir.ActivationFunctionType
ALU = mybir.AluOpType

S = 4096
D = 1024
NT = S // 128
NC_ = S // 512
NIN = 5128
NEG = -30000.0
EPS = 1e-6


class Prog:
    ENGS = ("pe", "act", "dve", "pool", "sp")

    def __init__(self, nc, n_dma_sems=12):
        self.nc = nc
        self.ops = []
        self.n_dma_sems = n_dma_sems

    def add(self, eng, fn, reads=(), writes=(), dma=False, barrier=False):
        self.ops.append(dict(eng=eng, fn=fn, reads=tuple(reads), writes=tuple(writes), dma=dma, barrier=barrier))

    def pe(self, fn, reads=(), writes=()): self.add("pe", fn, reads, writes)
    def act(self, fn, reads=(), writes=()): self.add("act", fn, reads, writes)
    def dve(self, fn, reads=(), writes=()): self.add("dve", fn, reads, writes)
    def pool(self, fn, reads=(), writes=()): self.add("pool", fn, reads, writes)
    def dma(self, eng, fn, reads=(), writes=()): self.add(eng, fn, reads, writes, dma=True)
    def barrier(self): self.add(None, None, barrier=True)

    def build(self):
        ops = self.ops
        n = len(ops)
        last_w, readers = {}, {}
        deps = [set() for _ in range(n)]
        last_on_eng = {}
        dma_since = []
        bar_deps = None
        for i, o in enumerate(ops):
            if o["barrier"]:
                bar_deps = set(last_on_eng.values()) | set(dma_since)
                last_w, readers = {}, {}
                continue
            if bar_deps is not None:
                pass
            for k in o["reads"]:
                if k in last_w:
                    deps[i].add((last_w[k], False))
                if isinstance(k, tuple) and k[0] == "bank":
                    for r in readers.get(k, ()):
                        if ops[r]["eng"] != o["eng"]:
                            deps[i].add((r, False))
            for k in o["writes"]:
                if k in last_w:
                    deps[i].add((last_w[k], False))
                for r in readers.get(k, ()):
                    if r != i:
                        deps[i].add((r, True))
            for k in o["reads"]:
                readers.setdefault(k, []).append(i)
            for k in o["writes"]:
                last_w[k] = i
                readers[k] = []
            o["bar"] = bar_deps
            if o["dma"]:
                dma_since.append(i)
            else:
                last_on_eng[o["eng"]] = i
        fdeps = [set() for _ in range(n)]
        first_after_bar = {}
        for i, o in enumerate(ops):
            if o["barrier"]:
                continue
            for d, war in deps[i]:
                p = ops[d]
                if (not p["dma"]) and (not o["dma"]) and p["eng"] == o["eng"]:
                    if o["eng"] == "pe":
                        continue
                fdeps[i].add(d)
            bd = o.get("bar")
            if bd is not None and first_after_bar.get(o["eng"]) is not bd:
                first_after_bar[o["eng"]] = bd
                for d in bd:
                    if d != i and not (ops[d]["eng"] == o["eng"] and not ops[d]["dma"] and not o["dma"]):
                        fdeps[i].add(d)
        needs_inc = [False] * n
        for i in range(n):
            for d in fdeps[i]:
                needs_inc[d] = True
        eng_count = {e: 0 for e in self.ENGS}
        ticket = [None] * n
        dma_rr = {e: 0 for e in self.ENGS}
        dma_tot = {}
        pre_wait = [None] * n
        for i, o in enumerate(ops):
            if o["barrier"]:
                continue
            if o["dma"]:
                e = o["eng"]
                s = (e, dma_rr[e] % self.n_dma_sems)
                dma_rr[e] += 1
                prev = dma_tot.get(s, 0)
                if prev > 0:
                    pre_wait[i] = (s, prev)
                dma_tot[s] = prev + 16
                ticket[i] = (s, prev + 16)
            elif needs_inc[i]:
                eng_count[o["eng"]] += 1
                ticket[i] = (o["eng"], eng_count[o["eng"]])
        self.final_dma = dict(dma_tot)
        progs = {e: [] for e in self.ENGS}
        waited = {e: {} for e in self.ENGS}
        for i, o in enumerate(ops):
            if o["barrier"]:
                continue
            e = o["eng"]
            cand = [ticket[d] for d in fdeps[i]]
            if pre_wait[i] is not None:
                cand.append(pre_wait[i])
            best = {}
            for (s, v) in cand:
                if v > best.get(s, 0):
                    best[s] = v
            ws = []
            for s, v in best.items():
                if waited[e].get(s, 0) >= v:
                    continue
                waited[e][s] = v
                ws.append((s, v))
            progs[e].append((ws, o["fn"], ticket[i], o["dma"]))
        self.stats = {e: len(progs[e]) for e in self.ENGS}
        return progs

    def emit(self):
        nc = self.nc
        progs = self.build()
        semkeys = set()
        for e in self.ENGS:
            for ws, fn, t, isdma in progs[e]:
                for s, v in ws:
                    semkeys.add(s)
                if t is not None:
                    semkeys.add(t[0])
        semkeys = sorted(semkeys, key=str)
        with contextlib.ExitStack() as st:
            sems = {}
            for k in semkeys:
                nm = "s_" + (k if isinstance(k, str) else f"{k[0]}{k[1]}")
                sems[k] = st.enter_context(nc.semaphore(nm))
            block = st.enter_context(nc.Block())
            final = self.final_dma

            def make(e):
                def body(eng):
                    for ws, fn, t, isdma in progs[e]:
                        for s, v in ws:
                            eng.wait_ge(sems[s], v)
                        ins = fn(eng)
                        if t is not None:
                            ins.then_inc(sems[t[0]], 16 if isdma else 1)
                    if e == "sp":
                        for s, v in final.items():
                            eng.wait_ge(sems[s], v)
                return body
            block.tensor(make("pe"))
            block.scalar(make("act"))
            block.vector(make("dve"))
            block.gpsimd(make("pool"))
            block.sync(make("sp"))


class Arena:
    def __init__(self, nc, nbytes):
        self.t = nc.alloc_sbuf_tensor("arena", [128, nbytes], U8)
        self.nbytes = nbytes
        self.off = 0
        self.top = nbytes
        self.marks = {}

    def mark(self, name):
        self.marks[name] = self.off

    def reset(self, name):
        self.off = self.marks[name]

    def alloc_top(self, shape, dt):
        esz = 2 if dt == BF16 else 4
        n = int(np.prod(shape[1:]))
        nb = (n * esz + 31) // 32 * 32
        self.top -= nb
        v = self.t[0:shape[0], self.top:self.top + n * esz].bitcast(dt)
        if len(shape) == 3:
            v = v.rearrange("p (a b) -> p a b", b=shape[2])
        return v

    def alloc(self, shape, dt):
        esz = 2 if dt == BF16 else 4
        n = int(np.prod(shape[1:]))
        nb = (n * esz + 31) // 32 * 32
        assert self.off + nb <= self.top, (self.off, nb, self.top)
        v = self.t[0:shape[0], self.off:self.off + n * esz].bitcast(dt)
        self.off += nb
        if len(shape) == 3:
            v = v.rearrange("p (a b) -> p a b", b=shape[2])
        elif len(shape) == 4:
            v = v.rearrange("p (a b c) -> p a b c", b=shape[2], c=shape[3])
        return v


def MM(out, lhsT, rhs, start, stop):
    return lambda e: e.matmul(out, lhsT, rhs, start=start, stop=stop)


def TR(out, in_, ident):
    return lambda e: e.transpose(out, in_, ident)


def ACT(out, in_, func, **kw):
    return lambda e: e.activation(out=out, in_=in_, func=func, **kw)


def TT(out, in0, in1, op):
    return lambda e: e.tensor_tensor(out=out, in0=in0, in1=in1, op=op)


def TS(out, in0, s1, s2, op0, op1=None):
    if op1 is None:
        return lambda e: e.tensor_scalar(out=out, in0=in0, scalar1=s1, scalar2=None, op0=op0)
    return lambda e: e.tensor_scalar(out=out, in0=in0, scalar1=s1, scalar2=s2, op0=op0, op1=op1)


def STT(out, in0, scalar, in1, op0, op1):
    return lambda e: e.scalar_tensor_tensor(out=out, in0=in0, scalar=scalar, in1=in1, op0=op0, op1=op1)


def CP(out, in_):
    return lambda e: e.tensor_copy(out=out, in_=in_)


def DMA(out, in_):
    return lambda e: e.dma_start(out=out, in_=in_)


def MEMSET(ap, v):
    return lambda e: e.memset(ap, v)


def RECIP(out, in_):
    return lambda e: e.reciprocal(out=out, in_=in_)


def build_program(stop_after=99, dbg=False):
    nc = bass.Bass("TRN2", target_bir_lowering=False)

    def din(name, shape, dt=F32):
        return nc.dram_tensor(name, shape, dt, kind="ExternalInput").ap()

    x_d = din("x", [S, D])
    cT_d = din("cT", [128, 8])
    adaw_d = din("ada_w", [D, 6 * D])
    adabT_d = din("ada_bT", [128, 48])
    adab_row_d = din("ada_b_row", [1, 6 * D])
    n1g_d = din("n1g", [128, 8])
    n2g_d = din("n2g", [128, 8])
    fg_row_d = din("fg_row", [1, D])
    win_d = din("w_in", [D, NIN])
    bfg_d = din("b_forget", [8, 1])
    wbf_d = din("w_bf", [512, D])
    wbs_d = din("w_bs", [512, D])
    wout_d = din("w_out", [D, D])
    wr_d = din("w_r", [D, 20])
    br_row_d = din("b_r_row", [1, 20])
    wg2_d = din("e_wg", [16 * 256, 2048])
    wu2_d = din("e_wu", [16 * 256, 2048])
    wd2_d = din("e_wd", [16 * 512, D])
    n2g_row_d = din("n2g_row", [1, D])
    rc_d = din("rconst", [128, 40])
    out_d = nc.dram_tensor("out", [S, D], F32, kind="ExternalOutput").ap()
    ot_scr = nc.dram_tensor("ot_scr", [8, 128, S], BF16, kind="ExternalOutput" if dbg else "Internal").ap()
    fsc = nc.dram_tensor("fsc", [8, 12, S], BF16, kind="Internal").ap()
    gsc = nc.dram_tensor("gsc", [4, 128, D], F32, kind="Internal").ap()
    NS = 12
    h2tok_d = nc.dram_tensor("h2tok", [S, D], BF16, kind="Internal").ap()
    h2s_d = nc.dram_tensor("h2s", [NS * 512, D], BF16, kind="Internal").ap()
    cws_d = nc.dram_tensor("cws", [NS * 512, 4], F32, kind="Internal").ap()
    ys_d = nc.dram_tensor("ys", [NS * 512, D], F32, kind="Internal").ap()
    wg2b_d = nc.dram_tensor("wg2b", [16 * 256, 2048], BF16, kind="Internal").ap()
    wu2b_d = nc.dram_tensor("wu2b", [16 * 256, 2048], BF16, kind="Internal").ap()
    wd2b_d = nc.dram_tensor("wd2b", [16 * 512, D], BF16, kind="Internal").ap()
    dbg_o = {}
    if dbg:
        dbg_o["hT"] = nc.dram_tensor("dbg_hT", [128, 8, S], BF16, kind="ExternalOutput").ap()
        dbg_o["mod"] = nc.dram_tensor("dbg_mod", [128, 48], F32, kind="ExternalOutput").ap()
        dbg_o["g1"] = nc.dram_tensor("dbg_g1", [128, D], F32, kind="ExternalOutput").ap()
        dbg_o["comb"] = nc.dram_tensor("dbg_comb", [128, NT, 4], F32, kind="ExternalOutput").ap()
        dbg_o["h2T"] = nc.dram_tensor("dbg_h2T", [128, 8, S], BF16, kind="ExternalOutput").ap()

    win_v = win_d.rearrange("(k p) n -> p k n", p=128)
    adaw_v = adaw_d.rearrange("(k p) n -> p k n", p=128)

    P = Prog(nc)
    A = Arena(nc, 206 * 1024)
    banks = [nc.alloc_psum_tensor(f"bank{i}", [128, 512], F32) for i in range(8)]

    def bank_bf(i):
        return banks[i][:, :].bitcast(BF16)

    ohg_all = A.alloc([128, NT, 4], F32)
    cw_all = A.alloc([128, NT, 4], F32)
    s1 = A.alloc([128, 8], F32)
    b1 = A.alloc([128, 8], F32)
    s2 = A.alloc([128, 8], F32)
    b2 = A.alloc([128, 8], F32)
    n1g = A.alloc([128, 8], F32)
    n2g = A.alloc([128, 8], F32)
    modT = A.alloc([128, 48], F32)
    small = A.alloc([128, 64], F32)
    identb = A.alloc([128, 128], BF16)
    identf = A.alloc([128, 128], F32)
    onesf = A.alloc([128, 128], F32)
    A.mark("pre_hT")
    hT = A.alloc([128, 8, S], BF16)
    A.mark("phase")
    wq = A.alloc_top([128, 8, 576], BF16)
    wk = A.alloc_top([128, 8, 576], BF16)
    wv = A.alloc_top([128, 8, 512], BF16)
    wfa = A.alloc_top([128, 8, 8], BF16)

    P.pool(MEMSET(onesf, 1.0), writes=["onesf"])
    P.pool(lambda e: e.affine_select(out=identf, in_=onesf, pattern=[[-1, 128]], compare_op=ALU.is_equal,
                                     fill=0.0, base=0, channel_multiplier=1), reads=["onesf"], writes=["identf"])
    P.pool(CP(identb, identf), reads=["identf"], writes=["identb"])
    P.pool(MEMSET(modT, 0.0), writes=["modT_a", "modT_b"])
    P.dma("sp", DMA(n1g, n1g_d), writes=["n1g"])
    P.dma("sp", DMA(n2g, n2g_d), writes=["n2g"])

    gate1_b = A.alloc([128, D], F32)
    gate2_b = A.alloc([128, D], F32)
    cT = A.alloc([128, 8], F32)
    c_bf = A.alloc([128, 8], BF16)
    c_rep = A.alloc([128, 8, 128], BF16)
    adabT = A.alloc([128, 48], F32)
    adab_g = A.alloc([128, 4, D], F32)
    n2g_b = A.alloc([128, D], F32)
    sb2_b = [A.alloc([128, D], F32) for _ in range(2)]
    adaw_sb = [A.alloc([128, 8, 512], BF16) for _ in range(3)]
    P.dma("sp", DMA(cT, cT_d), writes=["cT"])
    P.dma("sp", DMA(adabT, adabT_d), writes=["adabT"])
    P.dma("sp", DMA(adab_g[:, 0, :], adab_row_d[0:1, 2 * D:3 * D].partition_broadcast(128)), writes=["adab_g0"])
    P.dma("sp", DMA(adab_g[:, 1, :], adab_row_d[0:1, 5 * D:6 * D].partition_broadcast(128)), writes=["adab_g1"])
    P.dma("sp", DMA(adab_g[:, 2, :], adab_row_d[0:1, 3 * D:4 * D].partition_broadcast(128)), writes=["adab_g2"])
    P.dma("sp", DMA(adab_g[:, 3, :], adab_row_d[0:1, 4 * D:5 * D].partition_broadcast(128)), writes=["adab_g3"])
    P.dma("sp", DMA(n2g_b, n2g_row_d[0:1, :].partition_broadcast(128)), writes=["n2g_b"])
    P.act(ACT(c_bf, cT, AF.Silu), reads=["cT"], writes=["c_bf"])
    P.dve(CP(c_rep, c_bf[:, :].unsqueeze(2).to_broadcast([128, 8, 128])), reads=["c_bf"], writes=["c_rep"])
    order = [0, 1, 2, 3, 6, 7, 8, 9, 4, 5, 10, 11]
    for n_, q in enumerate(order):
        buf = adaw_sb[n_ % 3]
        bk = ("adaw", n_ % 3)
        P.dma("pool", DMA(buf, adaw_v[:, :, q * 512:(q + 1) * 512]), writes=[bk])
        v, half = q // 2, q % 2
        if v in (2, 5):
            gi = 0 if v == 2 else 1
            bnk = banks[gi * 2 + half]
            for k in range(8):
                P.pe(MM(bnk[:, :], c_rep[:, k, :], buf[:, k, :], k == 0, k == 7), reads=[bk, "c_rep"], writes=[("bank", gi * 2 + half)])
            dst = (gate1_b if gi == 0 else gate2_b)[:, half * 512:(half + 1) * 512]
            P.dve(TT(dst, bnk[:, :], adab_g[:, gi, half * 512:(half + 1) * 512], ALU.add),
                  reads=[("bank", gi * 2 + half), f"adab_g{gi}"], writes=[("gate", gi, half)])
            if half == 1:
                P.dma("sp", DMA(gsc[gi], gate1_b if gi == 0 else gate2_b), reads=[("gate", gi, 0), ("gate", gi, 1)], writes=[("gsc", gi)])
        else:
            if v in (3, 4):
                gi = v - 1
                bi = (v - 3) * 2 + half
                for k in range(8):
                    P.pe(MM(banks[bi][:, :], c_rep[:, k, :], buf[:, k, :], k == 0, k == 7), reads=[bk, "c_rep"], writes=[("bank", bi)])
                dst = sb2_b[v - 3][:, half * 512:(half + 1) * 512]
                P.dve(TT(dst, banks[bi][:, :], adab_g[:, gi, half * 512:(half + 1) * 512], ALU.add),
                      reads=[("bank", bi), f"adab_g{gi}"], writes=[("sb2", v - 3, half)])
                if v == 4:
                    P.dve(STT(dst, dst, 1.0, n2g_b[:, half * 512:(half + 1) * 512], ALU.add, ALU.mult),
                          reads=[("sb2", 1, half), "n2g_b"], writes=[("sb2", 1, half)])
                if half == 1:
                    P.dma("sp", DMA(gsc[2 + v - 3], sb2_b[v - 3]), reads=[("sb2", v - 3, 0), ("sb2", v - 3, 1)], writes=[("gsc", 2 + v - 3)])
            for cc in range(4):
                j = v * 8 + half * 4 + cc
                for k in range(8):
                    mb = 4 if v < 2 else 5
                    P.pe(MM(banks[mb][:, j:j + 1], buf[:, k, cc * 128:(cc + 1) * 128], c_bf[:, k:k + 1], k == 0, k == 7),
                         reads=[bk, "c_bf"], writes=[("bank", mb)])
        if n_ == 3:
            P.dve(TT(modT[:, 0:16], banks[4][:, 0:16], adabT[:, 0:16], ALU.add),
                  reads=[("bank", 4), "adabT"], writes=["modT_a"])
            P.dve(STT(s1, modT[:, 8:16], 1.0, n1g, ALU.add, ALU.mult), reads=["modT_a", "n1g"], writes=["s1"])
            P.dve(CP(b1, modT[:, 0:8]), reads=["modT_a"], writes=["b1"])
        if n_ == 7:
            P.dve(TT(modT[:, 24:40], banks[5][:, 24:40], adabT[:, 24:40], ALU.add),
                  reads=[("bank", 5), "adabT"], writes=["modT_b"])
            P.dve(STT(s2, modT[:, 32:40], 1.0, n2g, ALU.add, ALU.mult), reads=["modT_b", "n2g"], writes=["s2"])
            P.dve(CP(b2, modT[:, 24:32]), reads=["modT_b"], writes=["b2"])
    if dbg:
        P.dma("sp", DMA(dbg_o["mod"], modT), reads=["modT_a", "modT_b"], writes=["dbg_mod"])
        P.dma("sp", DMA(dbg_o["g1"], gate1_b), reads=[("gate", 0, 0), ("gate", 0, 1)], writes=["dbg_g1"])

    if stop_after <= 0:
        P.emit()
        return nc, P
    def hkeys(tc, k=None):
        ks = range(8) if k is None else [k]
        return [("hT", tb, kk) for tb in range(tc * 4, tc * 4 + 4) for kk in ks]

    def norm_block(xb, xkey, ssq, rstd, junk, xn, xnkey, tb_tag, xkey2=None):
        xk = [xkey] if xkey2 is None else [xkey, xkey2]
        P.act(ACT(junk, xb, AF.Square, accum_out=ssq), reads=xk, writes=[("ssq", tb_tag), ("junk", tb_tag % 2)])
        P.dve(TS(rstd, ssq, 1.0 / D, EPS, ALU.mult, ALU.add), reads=[("ssq", tb_tag)], writes=[("rs", tb_tag)])
        P.act(ACT(rstd, rstd, AF.Sqrt), reads=[("rs", tb_tag)], writes=[("rs", tb_tag)])
        P.dve(RECIP(rstd, rstd), reads=[("rs", tb_tag)], writes=[("rs", tb_tag)])
        P.dve(TS(xn, xb, rstd, None, ALU.mult), reads=xk + [("rs", tb_tag)], writes=[xnkey])

    def load_branch_weights(br):
        base = 0 if br == 0 else 1544
        P.dma("pool", DMA(wq, win_v[:, :, base:base + 576]), writes=["wq"])
        P.dma("pool", DMA(wk, win_v[:, :, base + 512:base + 1088]), writes=["wk"])
        P.dma("pool", DMA(wv, win_v[:, :, base + 1024:base + 1536]), writes=["wv"])

    load_branch_weights(0)
    P.dma("pool", DMA(wfa, win_v[:, :, 1536:1544]), writes=["wfa"])
    xbuf = [A.alloc([128, D], F32) for _ in range(2)]
    junkb = [A.alloc([128, D], BF16) for _ in range(2)]
    xnb = [A.alloc([128, D], BF16) for _ in range(2)]
    ssq_t = A.alloc([128, NT], F32)
    rstd_t = A.alloc([128, NT], F32)
    import os as _os
    for tb in range(int(_os.environ.get('S1_N', NT))):
        xb = xbuf[tb % 2]
        P.dma("sp", DMA(xb, x_d[tb * 128:(tb + 1) * 128, :]), writes=[("xb", tb % 2)])
        norm_block(xb, ("xb", tb % 2), ssq_t[:, tb:tb + 1], rstd_t[:, tb:tb + 1], junkb[tb % 2], xnb[tb % 2], ("xn", tb % 2), tb)
        ba, bd = 4 + 2 * (tb % 2), 5 + 2 * (tb % 2)
        for k in range(8):
            bk_ = ba if k % 2 == 0 else bd
            P.pe(TR(bank_bf(bk_)[:, (k // 2) * 128:(k // 2 + 1) * 128], xnb[tb % 2][:, k * 128:(k + 1) * 128], identb),
                 reads=[("xn", tb % 2), "identb"], writes=[("bank", bk_)])
        for k in range(8):
            dst = hT[:, k, tb * 128:(tb + 1) * 128]
            bk_ = ba if k % 2 == 0 else bd
            src = bank_bf(bk_)[:, (k // 2) * 128:(k // 2 + 1) * 128]
            if k % 2 == 0:
                P.act(ACT(dst, src, AF.Identity, scale=s1[:, k:k + 1], bias=b1[:, k:k + 1]),
                      reads=[("bank", bk_), "s1", "b1"], writes=[("hT", tb, k)])
            else:
                P.dve(TS(dst, src, s1[:, k:k + 1], b1[:, k:k + 1], ALU.mult, ALU.add),
                      reads=[("bank", bk_), "s1", "b1"], writes=[("hT", tb, k)])
    if dbg:
        for k in range(8):
            P.dma("sp", DMA(dbg_o["hT"][:, k, 0:int(_os.environ.get('S1_N', NT)) * 128], hT[:, k, 0:int(_os.environ.get('S1_N', NT)) * 128]), reads=[("hT", tb, k) for tb in range(NT)], writes=[("dbg_hT", k)])
    if stop_after <= 1:
        P.emit()
        return nc, P

    P.barrier()
    A.reset("phase")
    onesb = A.alloc([128, 128], BF16)
    zerosb = A.alloc([128, 512], BF16)
    SLb = A.alloc([128, 128], BF16)
    mask_fox = [A.alloc([128, 512], BF16) for _ in range(4)]
    mask_sb = [A.alloc([128, 512], BF16) for _ in range(4)]
    negb = A.alloc([8, 1], F32)
    Qp = [A.alloc([128, S], BF16) for _ in range(2)]
    Kp = [A.alloc([128, S], BF16) for _ in range(2)]
    Vp = [A.alloc([128, NT, 128], BF16) for _ in range(2)]
    Pt = [A.alloc([128, 512], BF16) for _ in range(4)]
    e_sb = [A.alloc([128, 512], F32) for _ in range(2)]
    Lp = [A.alloc([128, 512], BF16) for _ in range(3)]
    accb = [A.alloc([128, 512], BF16) for _ in range(3)]
    acc32 = A.alloc([128, 512], F32)
    negonesb = A.alloc([128, 128], BF16)
    rcs = [A.alloc([128, 512], F32) for _ in range(2)]
    rc2 = [A.alloc([128, 512], F32) for _ in range(2)]
    osb = [A.alloc([128, 512], BF16) for _ in range(2)]
    f_e = [A.alloc([8, 512], F32)] * 2
    f_sp = [A.alloc([8, 512], F32)] * 2
    f_nF = [A.alloc([8, 512], F32) for _ in range(2)]
    f_r = [A.alloc([8, 512], F32)] * 2
    f_hb = [A.alloc([8, 12, 512], BF16)] * 2
    f_ones = A.alloc([8, 1], F32)

    P.dve(MEMSET(onesb, 1.0), writes=["onesb"])
    P.dve(MEMSET(negonesb, -1.0), writes=["negonesb"])
    P.dve(MEMSET(zerosb, 0.0), writes=["zerosb"])
    P.pool(lambda e: e.affine_select(out=SLb, in_=onesb, pattern=[[1, 128]], compare_op=ALU.is_ge,
                                     fill=0.0, base=-1, channel_multiplier=-1), reads=["onesb"], writes=["SLb"])
    for m in range(4):
        P.pool(lambda e, m=m: e.affine_select(out=mask_fox[m], in_=zerosb, pattern=[[1, 512]], compare_op=ALU.is_ge,
                                              fill=NEG, base=-128 * m, channel_multiplier=-1),
               reads=["zerosb"], writes=[("mfox", m)])
        P.pool(lambda e, m=m: e.affine_select(out=mask_sb[m], in_=zerosb, pattern=[[1, 512]], compare_op=ALU.is_ge,
                                              fill=NEG, base=-128 * m - 1, channel_multiplier=-1),
               reads=["zerosb"], writes=[("msb", m)])
    for i in range(2):
        P.dve(MEMSET(Qp[i][64:128, :], 0.0), writes=[("Qp", i, "augd")])
        P.dve(MEMSET(Kp[i][64:128, :], 0.0), writes=[("Kp", i, "augd")])
        P.dve(MEMSET(Vp[i][:, :, 64:128], 1.0), writes=[("Vp", i, "ones")])
    P.dve(MEMSET(f_ones, 1.0), writes=["f_ones"])
    P.dve(MEMSET(f_hb[0], 1.0), writes=[("f_hb", r) for r in (0, 9, 10, 11, "ones")])

    h2s_z = h2s_d.rearrange("r (a c) -> (r a) c", c=512)
    P.dma("sp", DMA(negb, bfg_d), writes=["negb"])
    P.dve(TS(negb, negb, -1.0, None, ALU.mult), reads=["negb"], writes=["negb"])

    def proj_part(br, h, part, buf, pbanks=(7, 7, 7)):
        tc = part
        pb = pbanks[0]
        for k in range(8):
            P.pe(MM(banks[pb][:, :], wq[:, k, h * 64:h * 64 + 128], hT[:, k, tc * 512:(tc + 1) * 512], k == 0, k == 7),
                 reads=["wq"] + hkeys(tc, k), writes=[("bank", pb)])
        P.dve(TS(Qp[buf][0:64, tc * 512:(tc + 1) * 512], banks[pb][0:64, :], 0.125, None, ALU.mult),
              reads=[("bank", pb)], writes=[("Qp", buf, tc)])
        pb2 = pbanks[1]
        for k in range(8):
            P.pe(MM(banks[pb2][:, :], wk[:, k, h * 64:h * 64 + 128], hT[:, k, tc * 512:(tc + 1) * 512], k == 0, k == 7),
                 reads=["wk"] + hkeys(tc, k), writes=[("bank", pb2)])
        P.dve(CP(Kp[buf][0:64, tc * 512:(tc + 1) * 512], banks[pb2][0:64, :]),
              reads=[("bank", pb2)], writes=[("Kp", buf, tc)])
        pb = pbanks[2]
        vps = banks[pb][:, 0:256].rearrange("p (a b) -> p a b", b=64)
        for q in range(4):
            tb = part * 4 + q
            for k in range(8):
                P.pe(MM(vps[:, q, :], hT[:, k, tb * 128:(tb + 1) * 128], wv[:, k, h * 64:(h + 1) * 64], k == 0, k == 7),
                     reads=["wv", ("hT", tb, k)], writes=[("bank", pb)])
        P.dve(CP(Vp[buf][:, part * 4:(part + 1) * 4, 0:64], vps), reads=[("bank", pb)], writes=[("Vp", buf, part)])
        if br == 0 and part == 7:
            P.dma("sp", DMA(Qp[buf][64:70, :], fsc[h, 0:6, :]), reads=[("fsc", t) for t in range(NC_)], writes=[("Qp", buf, "augd")])
            P.dma("sp", DMA(Kp[buf][64:70, :], fsc[h, 6:12, :]), reads=[("fsc", t) for t in range(NC_)], writes=[("Kp", buf, "augd")])
        if br == 1 and h < 2 and part == 0:
            P.dve(MEMSET(Qp[buf][64:96, :], 0.0), writes=[("Qp", buf, "augd")])
            P.dve(MEMSET(Kp[buf][64:96, :], 0.0), writes=[("Kp", buf, "augd")])

    def qk_reads(buf, c, j, fox):
        return [("Qp", buf, c), ("Kp", buf, j // 4), ("Qp", buf, "augd"), ("Kp", buf, "augd")]

    def store_o(br, h, c, src_key, src):
        gh = br * 8 + h
        P.dma("sp", DMA(ot_scr[gh // 2, (gh % 2) * 64:(gh % 2) * 64 + 64, c * 512:(c + 1) * 512], src),
              reads=[src_key], writes=[("ot", gh, c)])

    def fox_head(h, buf, next_proj):
        tiles = []
        for c in range(NC_):
            if c == 0:
                tl = [(c, m, 0, m) for m in range(4)]
            else:
                tl = [(c, 4 * c + m, m * 128, m) for m in range(4)] + [(c, j, 0, None) for j in range(4 * c)]
            for n_, t in enumerate(tl):
                tiles.append(t + (n_ == 0, n_ == len(tl) - 1))
        nt = len(tiles)

        def pv(i):
            c, j, cs, m, first, last = tiles[i]
            ob = 4 + (c % 2)
            P.pe(MM(banks[ob][:, cs:512], Vp[buf][:, j, :], Pt[i % 4][:, cs:512], first, last),
                 reads=[("Vp", buf, j // 4), ("Vp", buf, "ones"), ("Pt", i % 4)], writes=[("bank", ob)])
            if last:
                c2 = c % 2
                P.dve(RECIP(rcs[c2][64:128, :], banks[ob][64:128, :]), reads=[("bank", ob)], writes=[("rcs", c2)])
                P.dma("sp", DMA(rc2[c2][0:64, :], rcs[c2][64:128, :]), reads=[("rcs", c2)], writes=[("rc2", c2)])
                P.dve(TT(osb[c2][0:64, :], banks[ob][0:64, :], rc2[c2][0:64, :], ALU.mult),
                      reads=[("bank", ob), ("rc2", c2)], writes=[("osb", c2)])
                store_o(0, h, c, ("osb", c2), osb[c2][0:64, :])
                if next_proj is not None:
                    next_proj(c)

        for i, (c, j, cs, m, first, last) in enumerate(tiles):
            ab = i % 3
            diag = m is not None
            P.pe(MM(banks[ab][:, cs:512], Kp[buf][:, j * 128:(j + 1) * 128], Qp[buf][:, c * 512 + cs:(c + 1) * 512], True, not diag),
                 reads=qk_reads(buf, c, j, True), writes=[("bank", ab)])
            if diag:
                mw = 512 if c == 0 else cs + 128
                P.pe(MM(banks[ab][:, cs:mw], identb, mask_fox[m][:, cs:mw], False, True),
                     reads=["identb", ("mfox", m)], writes=[("bank", ab)])
            P.act(ACT(Pt[i % 4][:, cs:512], banks[ab][:, cs:512], AF.Exp), reads=[("bank", ab)], writes=[("Pt", i % 4)])
            if i >= 2:
                pv(i - 2)
        pv(nt - 2)
        pv(nt - 1)

    A2B = (2, 3, 6)

    def sb_head(h, buf, next_proj):
        tiles = []
        for c in range(NC_):
            for j in range(4 * c + 3, -1, -1):
                m = j - 4 * c if j >= 4 * c else None
                tiles.append((c, j, 0 if (m is None or m == 3) else m * 128, m))
        nt = len(tiles)

        def zmm(i, bank, stop_last):
            c, j, cs, m = tiles[i]
            diag = m is not None
            P.pe(MM(banks[bank][:, cs:512], Kp[buf][:, j * 128:(j + 1) * 128], Qp[buf][:, c * 512 + cs:(c + 1) * 512], True, stop_last and not diag),
                 reads=qk_reads(buf, c, j, False), writes=[("bank", bank)])
            if diag:
                mw = 512 if m == 3 else cs + 128
                P.pe(MM(banks[bank][:, cs:mw], identb, mask_sb[m][:, cs:mw], False, stop_last),
                     reads=["identb", ("msb", m)], writes=[("bank", bank)])

        def st_z(i):
            zmm(i, i % 2, True)

        def st_exp1(i):
            cs = tiles[i][2]
            P.act(ACT(e_sb[i % 2][:, cs:512], banks[i % 2][:, cs:512], AF.Exp), reads=[("bank", i % 2)], writes=[("e_sb", i % 2)])

        def st_ln(i):
            cs = tiles[i][2]
            P.act(ACT(Lp[i % 3][:, cs:512], e_sb[i % 2][:, cs:512], AF.Ln, bias=1.0), reads=[("e_sb", i % 2)], writes=[("Lp", i % 3)])

        def st_acc(i):
            c, j, cs, m = tiles[i]
            L_ = Lp[i % 3]
            if m == 3:
                P.dve(CP(acc32, L_), reads=[("Lp", i % 3)], writes=["acc32"])
            else:
                P.dve(TT(acc32[:, cs:512], acc32[:, cs:512], L_[:, cs:512], ALU.add), reads=[("Lp", i % 3), "acc32"], writes=["acc32"])
            P.dve(CP(accb[i % 3][:, cs:512], acc32[:, cs:512]), reads=["acc32"], writes=[("accb", i % 3)])

        def st_g2(i):
            cs = tiles[i][2]
            bk = A2B[i % 3]
            zmm(i, bk, False)
            P.pe(MM(banks[bk][:, cs:512], SLb, Lp[i % 3][:, cs:512], False, False), reads=["SLb", ("Lp", i % 3)], writes=[("bank", bk)])
            P.pe(MM(banks[bk][:, cs:512], negonesb, accb[i % 3][:, cs:512], False, True), reads=["negonesb", ("accb", i % 3)], writes=[("bank", bk)])

        def st_exp2(i):
            cs = tiles[i][2]
            bk = A2B[i % 3]
            P.act(ACT(Pt[i % 3][:, cs:512], banks[bk][:, cs:512], AF.Exp), reads=[("bank", bk)], writes=[("Pt", i % 3)])

        def st_pv(i):
            c, j, cs, m = tiles[i]
            ob = 4 + (c % 2)
            rd = [("Vp", buf, j // 4), ("Vp", buf, "ones"), ("Pt", i % 3)]
            P.pe(MM(banks[ob][:, cs:512], Vp[buf][:, j, :], Pt[i % 3][:, cs:512], m == 3, j == 0), reads=rd, writes=[("bank", ob)])
            if j == 0:
                c2 = c % 2
                P.dve(CP(osb[c2][0:64, :], banks[ob][0:64, :]), reads=[("bank", ob)], writes=[("osb", c2)])
                store_o(1, h, c, ("osb", c2), osb[c2][0:64, :])
                if next_proj is not None:
                    next_proj(c)

        for i in range(nt + 4):
            if i < nt:
                st_z(i)
                st_exp1(i)
            if 0 <= i - 3 < nt:
                st_exp2(i - 3)
            if i < nt:
                st_ln(i)
            if 0 <= i - 1 < nt:
                st_acc(i - 1)
            if 0 <= i - 2 < nt:
                st_g2(i - 2)
            if 0 <= i - 4 < nt:
                st_pv(i - 4)

    heads = [(0, h) for h in range(8)] + [(1, h) for h in range(8)]
    n_heads_run = len(heads) if stop_after > 2 else (stop_after - 1) * 0 + 16
    if stop_after == 2 and dbg:
        n_heads_run = 16
    for tc in range(NC_):
        i2 = tc % 2
        fb = 5
        for k in range(8):
            P.pe(MM(banks[fb][0:8, :], wfa[:, k, :], hT[:, k, tc * 512:(tc + 1) * 512], k == 0, k == 7),
                 reads=["wfa"] + hkeys(tc, k), writes=[("bank", fb)])
        P.act(ACT(f_e[i2], banks[fb][0:8, :], AF.Exp, scale=-1.0, bias=negb[:, 0:1]), reads=[("bank", fb), "negb"], writes=["f_e"])
        P.act(ACT(f_sp[i2], f_e[i2], AF.Ln, bias=1.0), reads=["f_e"], writes=["f_sp"])
        init = 0.0 if tc == 0 else f_nF[1 - i2][:, 511:512]
        P.dve(lambda e, i2=i2, init=init: e.tensor_tensor_scan(out=f_nF[i2], data0=f_ones[:, 0:1].to_broadcast([8, 512]),
                                                                data1=f_sp[i2], initial=init, op0=ALU.mult, op1=ALU.add),
              reads=["f_sp", "f_ones", ("f_nF", 1 - i2)], writes=[("f_nF", i2)])
        hb = f_hb[i2]
        P.dve(CP(hb[:, 9, :], f_nF[i2]), reads=[("f_nF", i2)], writes=[("f_hb", 9)])
        P.dve(TT(f_r[i2], f_nF[i2], hb[:, 9, :], ALU.subtract), reads=[("f_nF", i2), ("f_hb", 9)], writes=["f_r"])
        P.dve(CP(hb[:, 10, :], f_r[i2]), reads=["f_r"], writes=[("f_hb", 10)])
        P.dve(TT(f_r[i2], f_r[i2], hb[:, 10, :], ALU.subtract), reads=["f_r", ("f_hb", 10)], writes=["f_r"])
        P.dve(CP(hb[:, 11, :], f_r[i2]), reads=["f_r"], writes=[("f_hb", 11)])
        P.dve(TS(hb[:, 0:3, :], hb[:, 9:12, :], -1.0, None, ALU.mult), reads=[("f_hb", 9), ("f_hb", 10), ("f_hb", 11)],
              writes=[("f_hb", 0)])
        P.dma("sp", DMA(fsc[:, :, tc * 512:(tc + 1) * 512], hb),
              reads=[("f_hb", r) for r in (0, 9, 10, 11, "ones")], writes=[("fsc", tc)])
        proj_part(0, 0, tc, 0, (7, 6, 3))

    for idx, (br, h) in enumerate(heads):
        buf = idx % 2
        if idx + 1 < len(heads):
            nbr, nh = heads[idx + 1]

            def next_proj(c, nbr=nbr, nh=nh, nbuf=1 - buf, idx=idx, br=br):
                if nbr == 1 and nh == 0 and c == 0:
                    load_branch_weights(1)
                proj_part(nbr, nh, c, nbuf, (7, 6, 3) if br == 0 else (7, 7, 7))
        else:
            next_proj = None
        ex_ = idx
        for i in range(idx * 6, idx * 6 + 6):
            P.dma("sp", DMA(h2s_z[i * 128:(i + 1) * 128, :], zerosb), reads=["zerosb"], writes=[("h2s_z", i)])
        if idx == 0:
            P.dma("sp", DMA(cws_d.rearrange("(p a) e -> p (a e)", p=128), zerosb[:, 0:NS * 32].bitcast(F32)), reads=["zerosb"], writes=["cws_z"])
        P.dma("pool", DMA(wg2b_d[ex_ * 256:(ex_ + 1) * 256, :], wg2_d[ex_ * 256:(ex_ + 1) * 256, :]), writes=[("wconv", ex_, 0)])
        P.dma("pool", DMA(wu2b_d[ex_ * 256:(ex_ + 1) * 256, :], wu2_d[ex_ * 256:(ex_ + 1) * 256, :]), writes=[("wconv", ex_, 1)])
        P.dma("pool", DMA(wd2b_d[ex_ * 512:(ex_ + 1) * 512, :], wd2_d[ex_ * 512:(ex_ + 1) * 512, :]), writes=[("wconv", ex_, 2)])
        if br == 0:
            fox_head(h, buf, next_proj)
        else:
            sb_head(h, buf, next_proj)
    if stop_after <= 2:
        P.emit()
        return nc, P

    P.barrier()
    A.reset("phase")
    A.top = A.nbytes
    wga = A.alloc([128, 8, D], BF16)
    wgb = A.alloc([128, 8, D], BF16)
    wbf = A.alloc([128, 4, D], BF16)
    wbs = A.alloc([128, 4, D], BF16)
    wout = A.alloc([128, 8, D], BF16)
    wr = A.alloc([128, 8, 20], F32)
    wrs = A.alloc([128, 8, 20], F32)
    rbias = A.alloc([128, 20], F32)
    br_b = A.alloc([128, 20], F32)
    otc = [A.alloc([128, 8, 512], BF16) for _ in range(2)]
    sg_sb = [A.alloc([128, 512], F32) for _ in range(2)]
    m_sb = [A.alloc([128, 512], F32) for _ in range(2)]
    mg = A.alloc([128, 8, 512], BF16)
    b2rep = mg[:, 0:4, :].rearrange("p a b -> p (a b)").bitcast(F32).rearrange("p (a b) -> p a b", b=128)
    xb3 = [A.alloc([128, D], F32) for _ in range(2)]
    x1b = [A.alloc([128, D], F32) for _ in range(2)]
    junk3 = [A.alloc([128, D], BF16)] * 2
    h2t = [A.alloc([128, D], BF16) for _ in range(2)]
    s2_b = A.alloc([128, D], F32)
    b2_b = A.alloc([128, D], F32)
    xT_sb = A.alloc([128, 8, 128], F32)
    ssq3 = A.alloc([128, NT], F32)
    rstd3 = A.alloc([128, NT], F32)
    gate1_b = A.alloc([128, D], F32)
    P.dma("sp", DMA(gate1_b[:, 0:512], gsc[0][:, 0:512]), writes=[("gate", 0, 0)])
    P.dma("sp", DMA(gate1_b[:, 512:1024], gsc[0][:, 512:1024]), writes=[("gate", 0, 1)])

    P.dma("pool", DMA(wga, win_v[:, :, 3080:4104]), writes=["wga"])
    P.dma("pool", DMA(wbf, wbf_d.rearrange("(q p) n -> p q n", p=128)), writes=["wbf"])
    P.dma("pool", DMA(wgb, win_v[:, :, 4104:5128]), writes=["wgb"])
    P.dma("pool", DMA(wbs, wbs_d.rearrange("(q p) n -> p q n", p=128)), writes=["wbs"])
    P.dma("pool", DMA(wout, wout_d.rearrange("(k p) n -> p k n", p=128)), writes=["wout"])
    P.dma("sp", DMA(wr, wr_d.rearrange("(k p) n -> p k n", p=128)), writes=["wr"])
    P.dma("sp", DMA(br_b, br_row_d[0:1, :].partition_broadcast(128)), writes=["br_b"])
    P.dve(TT(wrs, wr, s2[:, :].unsqueeze(2).to_broadcast([128, 8, 20]), ALU.mult), reads=["wr", "s2"], writes=["wrs"])
    P.dve(CP(b2rep, b2[:, :].unsqueeze(2).to_broadcast([128, 8, 128])), reads=["b2"], writes=[("mg", fc) for fc in range(8)])
    for k in range(8):
        P.pe(MM(banks[4][:, 0:20], b2rep[:, k, :], wr[:, k, :], k == 0, k == 7), reads=[("mg", fc) for fc in range(8)] + ["wr"], writes=[("bank", 4)])
    P.dma("sp", DMA(b2_b, gsc[2]), writes=["b2_b"])
    P.dma("sp", DMA(s2_b, gsc[3]), writes=["s2_b"])
    P.dve(TT(rbias, banks[4][:, 0:20], br_b, ALU.add), reads=[("bank", 4), "br_b"], writes=["rbias"])

    lg_all = A.alloc([128, NT, 20], F32)
    rbig = mg.rearrange("p a b -> p (a b)").bitcast(F32).rearrange("p (a b) -> p a b", b=64)

    def router_all():
        B_ = NT
        r = rbig
        X_ = mybir.AxisListType.X
        gl = lg_all[:, :, 0:4]
        el = lg_all[:, :, 4:20].rearrange("p b (g e) -> p b g e", e=4)
        gmax, m1, m2, d12, w1, w2, ssum, gp = (r[:, :, i] for i in range(8))
        dif, sg_, ex = r[:, :, 8:12], r[:, :, 12:16], r[:, :, 16:20]
        esel, oh1, es2, oh2, tmpw = r[:, :, 20:24], r[:, :, 24:28], r[:, :, 28:32], r[:, :, 32:36], r[:, :, 36:40]
        prod = r[:, :, 44:60].rearrange("p b (g e) -> p b g e", e=4)
        lk = [("lg", tb) for tb in range(NT)]

        def bc(v):
            return v.unsqueeze(2).to_broadcast([128, B_, 4])
        K = ["rbig"] + [("mg", fc) for fc in range(8)]
        P.dve(lambda e: e.reduce_max(out=gmax, in_=gl, axis=X_), reads=lk, writes=K)
        P.dve(TT(ohg_all, gl, bc(gmax), ALU.is_equal), reads=lk + K, writes=[("ohg", tb) for tb in range(NT)])
        P.dve(TT(dif, gl, bc(gmax), ALU.subtract), reads=lk + K, writes=K)
        P.act(ACT(sg_, dif, AF.Sigmoid), reads=K, writes=K)
        P.dve(TS(ex, sg_, -1.0, 1.0, ALU.mult, ALU.add), reads=K, writes=K)
        P.dve(RECIP(ex, ex), reads=K, writes=K)
        P.dve(TT(ex, ex, sg_, ALU.mult), reads=K, writes=K)
        P.dve(lambda e: e.reduce_sum(out=ssum, in_=ex, axis=X_), reads=K, writes=K)
        P.dve(RECIP(gp, ssum), reads=K, writes=K)
        P.dve(TT(prod, el, ohg_all.unsqueeze(3).to_broadcast([128, B_, 4, 4]), ALU.mult),
              reads=lk + [("ohg", tb) for tb in range(NT)], writes=K)
        P.dve(lambda e: e.reduce_sum(out=esel, in_=prod.rearrange("p b g e -> p b e g"), axis=X_), reads=K, writes=K)
        P.dve(lambda e: e.reduce_max(out=m1, in_=esel, axis=X_), reads=K, writes=K)
        P.dve(TT(oh1, esel, bc(m1), ALU.is_equal), reads=K, writes=K)
        P.dve(STT(es2, oh1, -1.0e30, esel, ALU.mult, ALU.add), reads=K, writes=K)
        P.dve(lambda e: e.reduce_max(out=m2, in_=es2, axis=X_), reads=K, writes=K)
        P.dve(TT(oh2, es2, bc(m2), ALU.is_equal), reads=K, writes=K)
        P.dve(TT(d12, m1, m2, ALU.subtract), reads=K, writes=K)
        P.act(ACT(w1, d12, AF.Sigmoid), reads=K, writes=K)
        P.dve(TS(w2, w1, -1.0, 1.0, ALU.mult, ALU.add), reads=K, writes=K)
        P.dve(TT(w1, w1, gp, ALU.mult), reads=K, writes=K)
        P.dve(TT(w2, w2, gp, ALU.mult), reads=K, writes=K)
        P.dve(TT(tmpw, oh1, bc(w1), ALU.mult), reads=K, writes=K)
        P.dve(TT(oh2, oh2, bc(w2), ALU.mult), reads=K, writes=K)
        P.dve(TT(cw_all, tmpw, oh2, ALU.add), reads=K, writes=[("cw", tb) for tb in range(NT)])

    ot_v = ot_scr.rearrange("q p t -> p q t")

    def blk_p1(tc, q4):
        tb = tc * 4 + q4
        t2 = tb % 2
        P.dma("sp", DMA(xb3[t2], x_d[tb * 128:(tb + 1) * 128, :]), writes=[("xb3", t2)])
        for hf in range(2):
            yb_ = 4 + hf
            for fc in range(8):
                P.pe(MM(banks[yb_][:, :], mg[:, fc, q4 * 128:(q4 + 1) * 128], wout[:, fc, hf * 512:(hf + 1) * 512], fc == 0, fc == 7),
                     reads=[("mg", fc), "wout"], writes=[("bank", yb_)])
            P.dve(TT(x1b[t2][:, hf * 512:(hf + 1) * 512], banks[yb_][:, :], gate1_b[:, hf * 512:(hf + 1) * 512], ALU.mult),
                  reads=[("bank", yb_), ("gate", 0, hf)], writes=[("x1b", t2, hf)])
        xk = [("x1b", t2, 0), ("x1b", t2, 1)]
        P.pool(TT(x1b[t2], x1b[t2], xb3[t2], ALU.add), reads=xk + [("xb3", t2)], writes=xk)
        P.dma("sp", DMA(out_d[tb * 128:(tb + 1) * 128, :], x1b[t2]), reads=xk, writes=[("out", tb)])
        P.act(ACT(junk3[t2], x1b[t2], AF.Square, accum_out=ssq3[:, tb:tb + 1]), reads=xk, writes=[("ssq", 100 + tb), "junk3"])

    def blk_p2(tb):
        t2 = tb % 2
        tg = 100 + tb
        xk = [("x1b", t2, 0), ("x1b", t2, 1)]
        P.dve(TS(rstd3[:, tb:tb + 1], ssq3[:, tb:tb + 1], 1.0 / D, EPS, ALU.mult, ALU.add), reads=[("ssq", tg)], writes=[("rs", tg)])
        P.act(ACT(rstd3[:, tb:tb + 1], rstd3[:, tb:tb + 1], AF.Sqrt), reads=[("rs", tg)], writes=[("rs", tg)])
        P.dve(RECIP(rstd3[:, tb:tb + 1], rstd3[:, tb:tb + 1]), reads=[("rs", tg)], writes=[("rs", tg)])
        for k in range(8):
            bk_ = 6 + k // 4
            tv = banks[bk_][:, :].rearrange("p (a b) -> p a b", b=128)
            P.pe(TR(tv[:, k % 4, :], x1b[t2][:, k * 128:(k + 1) * 128], identf), reads=xk + ["identf"], writes=[("bank", bk_)])
        for hh in range(2):
            P.dve(CP(xT_sb[:, hh * 4:(hh + 1) * 4, :], banks[6 + hh][:, :].rearrange("p (a b) -> p a b", b=128)),
                  reads=[("bank", 6 + hh)], writes=[("xT", hh)])
        for k in range(8):
            P.pe(MM(banks[6][:, 0:20], xT_sb[:, k, :], wrs[:, k, :], k == 0, k == 7), reads=[("xT", 0), ("xT", 1), "wrs"], writes=[("bank", 6)])
        P.dve(STT(lg_all[:, tb, :], banks[6][:, 0:20], rstd3[:, tb:tb + 1], rbias, ALU.mult, ALU.add),
              reads=[("bank", 6), ("rs", tg), "rbias"], writes=[("lg", tb)])
        P.dve(STT(xb3[t2], x1b[t2], rstd3[:, tb:tb + 1], s2_b, ALU.mult, ALU.mult), reads=xk + [("rs", tg), "s2_b"], writes=[("xb3", t2)])
        P.pool(TT(h2t[t2], xb3[t2], b2_b, ALU.add), reads=[("xb3", t2), "b2_b"], writes=[("h2t", t2)])
        P.dma("sp", DMA(h2tok_d[tb * 128:(tb + 1) * 128, :], h2t[t2]), reads=[("h2t", t2)], writes=[("h2tok", tb)])

    for tc in range(NC_):
        o2 = tc % 2
        P.dma("sp", DMA(otc[o2], ot_v[:, :, tc * 512:(tc + 1) * 512]),
              reads=[("ot", gh, tc) for gh in range(16)], writes=[("otc", o2)])
        for fc in range(8):
            for side in range(2):
                wgx = wga if side == 0 else wgb
                wbx = wbf if side == 0 else wbs
                wgk = "wga" if side == 0 else "wgb"
                wbk = "wbf" if side == 0 else "wbs"
                gbk, bbk = side, 2 + side
                for k in range(8):
                    P.pe(MM(banks[gbk][:, :], wgx[:, k, fc * 128:(fc + 1) * 128], hT[:, k, tc * 512:(tc + 1) * 512], k == 0, k == 7),
                         reads=[wgk] + hkeys(tc, k), writes=[("bank", gbk)])
                P.act(ACT(sg_sb[side], banks[gbk][:, :], AF.Sigmoid), reads=[("bank", gbk)], writes=[("sg", side)])
                for q in range(4):
                    P.pe(MM(banks[bbk][:, :], wbx[:, q, fc * 128:(fc + 1) * 128], otc[o2][:, side * 4 + q, :], q == 0, q == 3),
                         reads=[wbk, ("otc", o2)], writes=[("bank", bbk)])
                P.dve(TT(m_sb[side], banks[bbk][:, :], sg_sb[side], ALU.mult), reads=[("bank", bbk), ("sg", side)], writes=[("m", side)])
            P.pool(TT(mg[:, fc, :], m_sb[0], m_sb[1], ALU.add), reads=[("m", 0), ("m", 1)], writes=[("mg", fc)])
        for q4 in range(4):
            blk_p1(tc, q4)
            if tc * 4 + q4 >= 1:
                blk_p2(tc * 4 + q4 - 1)
    blk_p2(NT - 1)
    router_all()
    if dbg:
        P.dma("sp", DMA(dbg_o["comb"], cw_all), reads=[("cw", tb) for tb in range(NT)], writes=["dbg_comb"])
    if stop_after <= 3:
        P.emit()
        return nc, P

    P.barrier()
    A.reset("pre_hT")
    U32 = mybir.dt.uint32
    rconst = A.alloc([128, 40], F32)
    SUTf = A.alloc([128, 128], F32)
    rank_sb = A.alloc([128, NT, 4], F32)
    tot_sb = A.alloc([128, NT, 4], F32)
    incl = A.alloc([128, NT, 4], F32)
    Tt = A.alloc([128, NT, 4], F32)
    posf = A.alloc([128, NT], F32)
    idx = A.alloc([128, NT], U32)
    Ng = A.alloc([128, 4], F32)
    cnt = A.alloc([128, 4], F32)
    Pg = A.alloc([128, 4], F32)
    off = A.alloc([128, 4], F32)
    endg = A.alloc([128, 4], F32)
    gsl = A.alloc([128, NS], F32)
    gk = A.alloc([128, NS], F32)
    idx1f = A.alloc([128, NS, 8], F32)
    idx1 = A.alloc([128, NS, 8], U32)
    idx2f = A.alloc([128, NS, 16], F32)
    idx2 = A.alloc([128, NS, 16], U32)
    gate2_b = A.alloc([128, D], F32)
    fg_b = A.alloc([128, D], F32)
    xrow4 = [A.alloc([128, 4, D], BF16) for _ in range(2)]
    xs = [A.alloc([128, 4, D], BF16) for _ in range(2)]
    cwS = [A.alloc([128, 4, 4], F32) for _ in range(2)]
    h2Ts = [A.alloc([128, 8, 512], BF16) for _ in range(2)]
    wgs = [A.alloc([128, 8, 512], BF16) for _ in range(2)]
    wus = [A.alloc([128, 8, 512], BF16) for _ in range(2)]
    wds = [A.alloc([128, 4, D], BF16) for _ in range(2)]
    actT = [A.alloc([128, 4, 512], BF16) for _ in range(2)]
    sgm = [A.alloc([128, 512], F32) for _ in range(2)]
    yacc = [A.alloc([128, 4, D], F32) for _ in range(2)]
    yb = [A.alloc([128, D], F32) for _ in range(2)]
    x1f = [A.alloc([128, D], F32) for _ in range(2)]
    ssq4 = A.alloc([128, NT], F32)
    rstd4 = A.alloc([128, NT], F32)
    junk4 = A.alloc([128, D], BF16)

    P.dma("sp", DMA(rconst, rc_d), writes=["rconst"])
    P.dma("sp", DMA(gate2_b, gsc[1]), writes=[("gate", 1, 0), ("gate", 1, 1)])
    P.dma("sp", DMA(fg_b, fg_row_d[0:1, :].partition_broadcast(128)), writes=["fg_b"])
    P.pool(lambda e: e.affine_select(out=SUTf, in_=onesf, pattern=[[1, 128]], compare_op=ALU.is_ge,
                                     fill=0.0, base=-1, channel_multiplier=-1), reads=["onesf"], writes=["SUTf"])
    okeys = [("ohg", tb) for tb in range(NT)]
    ohg2d = ohg_all.rearrange("p a b -> p (a b)")
    P.pe(MM(banks[0][:, 0:128], SUTf, ohg2d, True, True), reads=["SUTf"] + okeys, writes=[("bank", 0)])
    P.pe(MM(banks[1][:, 0:128], onesf, ohg2d, True, True), reads=["onesf"] + okeys, writes=[("bank", 1)])
    P.dve(CP(rank_sb.rearrange("p a b -> p (a b)"), banks[0][:, 0:128]), reads=[("bank", 0)], writes=["rank_sb"])
    P.dve(CP(tot_sb.rearrange("p a b -> p (a b)"), banks[1][:, 0:128]), reads=[("bank", 1)], writes=["tot_sb"])
    for g in range(4):
        P.dve(lambda e, g=g: e.tensor_tensor_scan(out=incl[:, :, g], data0=onesf[:, 0:NT], data1=tot_sb[:, :, g], initial=0.0,
                                                  op0=ALU.mult, op1=ALU.add), reads=["tot_sb", "onesf"], writes=[("incl", g)])
    ik = [("incl", g) for g in range(4)]
    P.dve(TT(Tt, incl, tot_sb, ALU.subtract), reads=ik + ["tot_sb"], writes=["Tt"])
    P.dve(TT(Tt, Tt, rank_sb, ALU.add), reads=["Tt", "rank_sb"], writes=["Tt"])
    P.dve(CP(Ng, incl[:, NT - 1, :]), reads=ik, writes=["Ng"])
    P.dve(MEMSET(cnt, 0.0), writes=["cnt"])
    for k in range(8):
        P.dve(STT(cnt, Ng, 512.0 * k, cnt, ALU.is_gt, ALU.add), reads=["Ng", "cnt"], writes=["cnt"])
    P.dve(TS(Pg, cnt, 512.0, None, ALU.mult), reads=["cnt"], writes=["Pg"])
    P.dve(MEMSET(off, 0.0), writes=["off"])
    for g in range(1, 4):
        P.dve(TT(off[:, g:g + 1], off[:, g - 1:g], Pg[:, g - 1:g], ALU.add), reads=["off", "Pg"], writes=["off"])
    P.dve(TT(endg, off, Pg, ALU.add), reads=["off", "Pg"], writes=["endg"])
    for g in range(4):
        P.dve(TS(Tt[:, :, g], Tt[:, :, g], off[:, g:g + 1], None, ALU.add), reads=["Tt", "off"], writes=["Tt"])
    P.dve(TT(Tt, Tt, ohg_all, ALU.mult), reads=["Tt"] + okeys, writes=["Tt"])
    P.dve(lambda e: e.reduce_sum(out=posf, in_=Tt, axis=mybir.AxisListType.X), reads=["Tt"], writes=["posf"])
    P.dve(CP(idx, posf), reads=["posf"], writes=["idx"])
    P.dve(MEMSET(gsl, 0.0), writes=["gsl"])
    for g in range(4):
        P.dve(STT(gsl, rconst[:, 24:36], endg[:, g:g + 1], gsl, ALU.is_ge, ALU.add), reads=["rconst", "endg", "gsl"], writes=["gsl"])
    P.dve(TS(gsl, gsl, 3.0, None, ALU.min), reads=["gsl"], writes=["gsl"])
    P.dve(TS(gk, gsl, 1024.0, None, ALU.mult), reads=["gsl"], writes=["gk"])
    P.dve(TT(idx1f, rconst[:, 0:8].unsqueeze(1).to_broadcast([128, NS, 8]), gk[:, :].unsqueeze(2).to_broadcast([128, NS, 8]), ALU.add),
          reads=["rconst", "gk"], writes=["idx1f"])
    P.dve(CP(idx1, idx1f), reads=["idx1f"], writes=["idx1"])
    P.dve(TS(gk, gsl, 2048.0, None, ALU.mult), reads=["gsl", "idx1f"], writes=["gk"])
    P.dve(TT(idx2f, rconst[:, 8:24].unsqueeze(1).to_broadcast([128, NS, 16]), gk[:, :].unsqueeze(2).to_broadcast([128, NS, 16]), ALU.add),
          reads=["rconst", "gk"], writes=["idx2f"])
    P.dve(CP(idx2, idx2f), reads=["idx2f"], writes=["idx2"])

    IOA = bass.IndirectOffsetOnAxis
    for g4 in range(NT // 4):
        r2 = g4 % 2
        P.dma("sp", DMA(xrow4[r2], h2tok_d[g4 * 512:(g4 + 1) * 512, :].rearrange("(a p) n -> p a n", p=128)), writes=[("xrow", r2)])
        for a_ in range(4):
            tb = g4 * 4 + a_
            P.dma("pool", lambda e, tb=tb, r2=r2, a_=a_: e.indirect_dma_start(out=h2s_d[:, :], out_offset=IOA(ap=idx[:, tb:tb + 1], axis=0),
                                                                              in_=xrow4[r2][:, a_, :], in_offset=None),
                  reads=[("xrow", r2), "idx"], writes=[("h2s_sc", tb)])
            P.dma("pool", lambda e, tb=tb: e.indirect_dma_start(out=cws_d[:, :], out_offset=IOA(ap=idx[:, tb:tb + 1], axis=0),
                                                                in_=cw_all[:, tb, :], in_offset=None),
                  reads=[("cw", tb), "idx"], writes=[("cws_sc", tb)])
    sc_all = [("h2s_sc", tb) for tb in range(NT)]
    cw_sc_all = [("cws_sc", tb) for tb in range(NT)]

    def slot_prep(sl):
        s2_ = sl % 2
        P.dma("sp", DMA(xs[s2_], h2s_d[sl * 512:(sl + 1) * 512, :].rearrange("(a p) n -> p a n", p=128)),
              reads=sc_all, writes=[("xs", s2_)])
        P.dma("sp", DMA(cwS[s2_], cws_d[sl * 512:(sl + 1) * 512, :].rearrange("(a p) e -> p a e", p=128)),
              reads=cw_sc_all, writes=[("cwS", s2_)])
        for a_ in range(4):
            tbk = 6 + (a_ % 2)
            pT = bank_bf(tbk)
            for k in range(8):
                P.pe(TR(pT[:, k * 128:(k + 1) * 128], xs[s2_][:, a_, k:D:8], identb), reads=[("xs", s2_), "identb"], writes=[("bank", tbk)])
            P.act(ACT(h2Ts[s2_][:, :, a_ * 128:(a_ + 1) * 128], pT.rearrange("p (k t) -> p k t", t=128), AF.Identity),
                  reads=[("bank", tbk)], writes=[("h2Ts", s2_, a_)])

    slot_prep(0)
    for sl in range(NS):
        s2_ = sl % 2
        hk = [("h2Ts", s2_, a_) for a_ in range(4)]
        for el in range(4):
            it = sl * 4 + el
            wb = it % 2
            for hf in range(2):
                P.dma("pool", lambda e, wb=wb, hf=hf, sl=sl, el=el: e.indirect_dma_start(
                    out=wgs[wb].rearrange("p k n -> p (k n)")[:, hf * 2048:(hf + 1) * 2048], out_offset=None, in_=wg2b_d[:, :],
                    in_offset=IOA(ap=idx1[:, sl, el * 2 + hf:el * 2 + hf + 1], axis=0)), reads=["idx1"], writes=[("wgs", wb, hf)])
                P.dma("pool", lambda e, wb=wb, hf=hf, sl=sl, el=el: e.indirect_dma_start(
                    out=wus[wb].rearrange("p k n -> p (k n)")[:, hf * 2048:(hf + 1) * 2048], out_offset=None, in_=wu2b_d[:, :],
                    in_offset=IOA(ap=idx1[:, sl, el * 2 + hf:el * 2 + hf + 1], axis=0)), reads=["idx1"], writes=[("wus", wb, hf)])
            for q in range(4):
                P.dma("pool", lambda e, wb=wb, q=q, sl=sl, el=el: e.indirect_dma_start(
                    out=wds[wb][:, q, :], out_offset=None, in_=wd2b_d[:, :],
                    in_offset=IOA(ap=idx2[:, sl, el * 4 + q:el * 4 + q + 1], axis=0)), reads=["idx2"], writes=[("wds", wb, q)])
            wdk = [("wds", wb, q) for q in range(4)]
            if el == 2 and sl + 1 < NS:
                slot_prep(sl + 1)
            a2 = it % 2
            for ffc in range(4):
                gb_, ub_ = ffc % 2, 2 + (ffc % 2)
                for k in range(8):
                    P.pe(MM(banks[gb_][:, :], wgs[wb][:, k, ffc * 128:(ffc + 1) * 128], h2Ts[s2_][:, k, :], k == 0, k == 7),
                         reads=[("wgs", wb, 0), ("wgs", wb, 1)] + hk, writes=[("bank", gb_)])
                for k in range(8):
                    P.pe(MM(banks[ub_][:, :], wus[wb][:, k, ffc * 128:(ffc + 1) * 128], h2Ts[s2_][:, k, :], k == 0, k == 7),
                         reads=[("wus", wb, 0), ("wus", wb, 1)] + hk, writes=[("bank", ub_)])
                P.act(ACT(sgm[ffc % 2], banks[gb_][:, :], AF.Silu), reads=[("bank", gb_)], writes=[("sgm", ffc % 2)])
                P.dve(TT(actT[a2][:, ffc, :], banks[ub_][:, :], sgm[ffc % 2], ALU.mult),
                      reads=[("bank", ub_), ("sgm", ffc % 2)], writes=[("actT", a2, ffc)])
            for a_ in range(4):
                for hf in range(2):
                    ybk = 4 + ((a_ * 2 + hf) % 2)
                    for ffc in range(4):
                        P.pe(MM(banks[ybk][:, :], actT[a2][:, ffc, a_ * 128:(a_ + 1) * 128], wds[wb][:, ffc, hf * 512:(hf + 1) * 512], ffc == 0, ffc == 3),
                             reads=[("actT", a2, f) for f in range(4)] + wdk, writes=[("bank", ybk)])
                    yv = yacc[s2_][:, a_, hf * 512:(hf + 1) * 512]
                    cwv = cwS[s2_][:, a_, el:el + 1]
                    if el == 0:
                        P.dve(TS(yv, banks[ybk][:, :], cwv, None, ALU.mult), reads=[("bank", ybk), ("cwS", s2_)], writes=[("yacc", s2_, a_, hf)])
                    else:
                        P.dve(STT(yv, banks[ybk][:, :], cwv, yv, ALU.mult, ALU.add),
                              reads=[("bank", ybk), ("cwS", s2_), ("yacc", s2_, a_, hf)], writes=[("yacc", s2_, a_, hf)])
        P.dma("sp", DMA(ys_d[sl * 512:(sl + 1) * 512, :].rearrange("(a p) n -> p a n", p=128), yacc[s2_]),
              reads=[("yacc", s2_, a_, hf) for a_ in range(4) for hf in range(2)], writes=[("ys", sl)])
    ys_all = [("ys", sl) for sl in range(NS)]
    for tb in range(NT):
        t2 = tb % 2
        P.dma("pool", lambda e, tb=tb, t2=t2: e.indirect_dma_start(out=yb[t2], out_offset=None, in_=ys_d[:, :],
                                                                   in_offset=IOA(ap=idx[:, tb:tb + 1], axis=0)),
              reads=["idx"] + ys_all, writes=[("yb", t2)])
        P.dma("sp", DMA(x1f[t2], out_d[tb * 128:(tb + 1) * 128, :]), writes=[("x1f", t2)])
        P.dve(TT(yb[t2], yb[t2], gate2_b, ALU.mult), reads=[("yb", t2), ("gate", 1, 0), ("gate", 1, 1)], writes=[("yb", t2)])
        P.dve(TT(x1f[t2], x1f[t2], yb[t2], ALU.add), reads=[("x1f", t2), ("yb", t2)], writes=[("x1f", t2)])
        P.act(ACT(junk4, x1f[t2], AF.Square, accum_out=ssq4[:, tb:tb + 1]), reads=[("x1f", t2)], writes=[("ssq4", tb), "junk4"])
        P.dve(TS(rstd4[:, tb:tb + 1], ssq4[:, tb:tb + 1], 1.0 / D, EPS, ALU.mult, ALU.add), reads=[("ssq4", tb)], writes=[("rs4", tb)])
        P.act(ACT(rstd4[:, tb:tb + 1], rstd4[:, tb:tb + 1], AF.Sqrt), reads=[("rs4", tb)], writes=[("rs4", tb)])
        P.dve(RECIP(rstd4[:, tb:tb + 1], rstd4[:, tb:tb + 1]), reads=[("rs4", tb)], writes=[("rs4", tb)])
        P.dve(STT(x1f[t2], x1f[t2], rstd4[:, tb:tb + 1], fg_b, ALU.mult, ALU.mult), reads=[("x1f", t2), ("rs4", tb), "fg_b"], writes=[("x1f", t2)])
        P.dma("sp", DMA(out_d[tb * 128:(tb + 1) * 128, :], x1f[t2]), reads=[("x1f", t2)], writes=[("out", tb)])
    P.emit()
    return nc, P


def _rconst():
    c = np.zeros((128, 40), np.float32)
    p = np.arange(128, dtype=np.float32)
    for el in range(4):
        for hf in range(2):
            c[:, el * 2 + hf] = 2 * p + 256 * el + hf
        for q in range(4):
            c[:, 8 + el * 4 + q] = p + 512 * el + 128 * q
    for sl in range(12):
        c[:, 24 + sl] = 512.0 * sl
    return c


def make_in_maps(inp):
    f = lambda a: np.ascontiguousarray(np.asarray(a, dtype=np.float32))
    B = inp["x"].shape[0]
    ada_b = f(inp["ada_b"][0])
    shared = {
        "ada_w": f(inp["ada_w"][0]),
        "ada_bT": f(ada_b.reshape(48, 128).T),
        "ada_b_row": f(ada_b.reshape(1, -1)),
        "n1g": f(inp["norm1_g"][0].reshape(8, 128).T),
        "n2g": f(inp["norm2_g"][0].reshape(8, 128).T),
        "fg_row": f(inp["final_g"].reshape(1, -1)),
        "w_in": f(inp["w_in"][0]),
        "b_forget": f(inp["b_forget"][0].reshape(8, 1)),
        "w_bf": f(inp["w_branch_fox"][0]),
        "w_bs": f(inp["w_branch_sb"][0]),
        "w_out": f(inp["w_out"][0]),
        "w_r": f(np.concatenate([inp["router_group_w"][0], inp["router_expert_w"][0]], axis=1)),
        "b_r_row": f(np.concatenate([inp["router_group_b"][0], inp["router_expert_b"][0]], axis=0).reshape(1, 20)),
        "e_wg": f(inp["expert_w_gate"][0]).reshape(16 * 256, 2048),
        "e_wu": f(inp["expert_w_up"][0]).reshape(16 * 256, 2048),
        "e_wd": f(inp["expert_w_down"][0]).reshape(16 * 512, 1024),
        "n2g_row": f(inp["norm2_g"][0].reshape(1, -1)),
        "rconst": _rconst(),
    }
    maps = []
    for b in range(B):
        m = dict(shared)
        m["x"] = f(inp["x"][b])
        m["cT"] = f(np.asarray(inp["c"][b]).reshape(8, 128).T)
        maps.append(m)
    return maps


_CACHE = {}


def kernel(**inputs):
    if "nc" not in _CACHE:
        _CACHE["nc"] = build_program()[0]
    nc = _CACHE["nc"]
    in_maps = make_in_maps(inputs)
    res = run_bass_kernel_spmd(nc, in_maps, core_ids=list(range(len(in_maps))))
    return np.stack([np.asarray(r["out"], dtype=np.float32) for r in res.results], axis=0)
```
